# Optimizing a Trainium2 kernel written in Bass

```python
import jax, jax.numpy as jnp
from jax import lax
import numpy as np

D_MODEL = 2048
BATCH = 2
SEQ = 4096
DEPTH = 1

EPS = 1e-6
M_WIDTH = D_MODEL // 2
A_WIDTH = D_MODEL - M_WIDTH
MIX_WIDTH = M_WIDTH + A_WIDTH
M_HEADS = 4
M_HEAD_DIM = M_WIDTH // M_HEADS
CONV_W = 4
CHUNK = 64
A_HEADS = 8
A_HEAD_DIM = A_WIDTH // A_HEADS
DILATED = ((128, 1), (512, 4), (2048, 16))
SEG_PAD = 2048
ROPE_THETA = 10000.0
IN_COLS = 3 * M_WIDTH + 2 * M_HEADS + 3 * A_WIDTH
IN_SPLITS = (M_WIDTH, 2 * M_WIDTH, 3 * M_WIDTH, 3 * M_WIDTH + M_HEADS, 3 * M_WIDTH + 2 * M_HEADS,
             3 * M_WIDTH + 2 * M_HEADS + A_WIDTH, 3 * M_WIDTH + 2 * M_HEADS + 2 * A_WIDTH)
N_MEM = 256
X_HEADS = 4
X_HEAD_DIM = D_MODEL // X_HEADS
PEER_HEADS = 8
N_KEYS = 128
N_EXPERTS = N_KEYS * N_KEYS
PK_DIM = 256
PK_TOPK = 16
PEER_BLOCK = 128

kernel_name = "hybrid_mlstm_dilated_peer_block"


def rmsnorm(x, g):
    xf = x.astype(jnp.float32)
    y = xf * lax.rsqrt(jnp.mean(xf * xf, axis=-1, keepdims=True) + EPS)
    return (y * g.astype(jnp.float32)).astype(x.dtype)


def heads(t, n_heads):
    b, s, _ = t.shape
    return t.reshape(b, s, n_heads, -1).transpose(0, 2, 1, 3)


def rope(x, pos):
    half = x.shape[-1] // 2
    inv = ROPE_THETA ** (-jnp.arange(half, dtype=jnp.float32) / half)
    ang = pos.astype(jnp.float32)[:, None] * inv[None, :]
    cos, sin = jnp.cos(ang), jnp.sin(ang)
    x1, x2 = x[..., :half].astype(jnp.float32), x[..., half:].astype(jnp.float32)
    out = jnp.concatenate([x1 * cos - x2 * sin, x2 * cos + x1 * sin], axis=-1)
    return out.astype(x.dtype)


def causal_conv(x, w, b):
    y = lax.conv_general_dilated(x, w[:, None, :], window_strides=(1,), padding=[(CONV_W - 1, 0)],
                                 dimension_numbers=('NWC', 'WIO', 'NWC'), feature_group_count=x.shape[-1])
    return y + b


def mlstm_chunkwise(q, k, v, i_pre, f_pre):
    out_dtype = q.dtype
    B, H, S, Dh = q.shape
    nc = S // CHUNK
    q = q.astype(jnp.float32).reshape(B, H, nc, CHUNK, Dh)
    k = k.astype(jnp.float32).reshape(B, H, nc, CHUNK, Dh) * (Dh ** -0.5)
    v = v.astype(jnp.float32).reshape(B, H, nc, CHUNK, Dh)
    logi = i_pre.astype(jnp.float32).reshape(B, H, nc, CHUNK)
    logf = jax.nn.log_sigmoid(f_pre.astype(jnp.float32)).reshape(B, H, nc, CHUNK)
    b = jnp.cumsum(logf, axis=-1)
    g = b[..., -1]
    a = logi + g[..., None] - b

    def step(carry, inp):
        C, n, m = carry
        k_c, v_c, a_c, g_c = inp
        m_new = jnp.maximum(g_c + m, jnp.max(a_c, axis=-1))
        decay = jnp.exp(g_c + m - m_new)
        w = jnp.exp(a_c - m_new[..., None])
        C_new = decay[..., None, None] * C + jnp.einsum('bhl,bhld,bhle->bhde', w, k_c, v_c)
        n_new = decay[..., None] * n + jnp.einsum('bhl,bhld->bhd', w, k_c)
        return (C_new, n_new, m_new), (C, n, m)

    init = (jnp.zeros((B, H, Dh, Dh), jnp.float32), jnp.zeros((B, H, Dh), jnp.float32),
            jnp.zeros((B, H), jnp.float32))
    xs = (jnp.moveaxis(k, 2, 0), jnp.moveaxis(v, 2, 0), jnp.moveaxis(a, 2, 0), jnp.moveaxis(g, 2, 0))
    _, (C_prev, n_prev, m_prev) = lax.scan(step, init, xs)
    C_prev = jnp.moveaxis(C_prev, 0, 2)
    n_prev = jnp.moveaxis(n_prev, 0, 2)
    m_prev = jnp.moveaxis(m_prev, 0, 2)

    causal = jnp.tril(jnp.ones((CHUNK, CHUNK), dtype=bool))
    D = jnp.where(causal, b[..., :, None] - b[..., None, :] + logi[..., None, :], -jnp.inf)
    m_inter = b + m_prev[..., None]
    m_t = jnp.maximum(m_inter, jnp.max(D, axis=-1))
    s = jnp.einsum('bhcld,bhcsd->bhcls', q, k) * jnp.exp(D - m_t[..., None])
    inter = jnp.exp(m_inter - m_t)
    num = jnp.einsum('bhcls,bhcse->bhcle', s, v) + inter[..., None] * jnp.einsum('bhcld,bhcde->bhcle', q, C_prev)
    den = jnp.sum(s, axis=-1) + inter * jnp.einsum('bhcld,bhcd->bhcl', q, n_prev)
    h = num / jnp.maximum(jnp.abs(den), jnp.exp(-m_t))[..., None]
    return h.reshape(B, H, S, Dh).astype(out_dtype)


def dilated_branch(q, k, v, window, dil):
    B, H, Sp, Dh = q.shape
    blk = window // dil
    M = Sp // dil
    nb = M // blk

    def to_stream(t):
        return t.reshape(B, H, M, dil, Dh).transpose(0, 1, 3, 2, 4).reshape(B, H, dil, nb, blk, Dh)

    def from_stream(t):
        rest = t.shape[5:]
        return t.reshape((B, H, dil, M) + rest).swapaxes(2, 3).reshape((B, H, Sp) + rest)

    def with_prev(t):
        prev = jnp.pad(t, ((0, 0), (0, 0), (0, 0), (1, 0), (0, 0), (0, 0)))[:, :, :, :-1]
        return jnp.concatenate([prev, t], axis=-2)

    qs = to_stream(q)
    kw = with_prev(to_stream(k))
    vw = with_prev(to_stream(v))
    scores = jnp.einsum('bhrnqd,bhrnkd->bhrnqk', qs, kw).astype(jnp.float32) * (Dh ** -0.5)
    qi = jnp.arange(blk)[:, None]
    kj = jnp.arange(2 * blk)[None, :]
    dist = qi + blk - kj
    band = (dist >= 0) & (dist <= blk)
    valid = band[None] & ((jnp.arange(nb)[:, None, None] > 0) | (kj >= blk)[None])
    scores = jnp.where(valid, scores, -jnp.inf)
    m = jnp.max(scores, axis=-1)
    p = jnp.exp(scores - m[..., None])
    s = jnp.sum(p, axis=-1)
    o = jnp.einsum('bhrnqk,bhrnkd->bhrnqd', p.astype(v.dtype), vw).astype(jnp.float32) / s[..., None]
    return from_stream(o), from_stream(m), from_stream(s)


def dilated_mixture(q, k, v):
    B, H, S, Dh = q.shape
    Sp = -(-S // SEG_PAD) * SEG_PAD
    pad = ((0, 0), (0, 0), (0, Sp - S), (0, 0))
    q, k, v = jnp.pad(q, pad), jnp.pad(k, pad), jnp.pad(v, pad)
    outs, maxes, sums = [], [], []
    for window, dil in DILATED:
        o, m, s = dilated_branch(q, k, v, window, dil)
        outs.append(o)
        maxes.append(m)
        sums.append(s)
    o_all, m_all, s_all = jnp.stack(outs), jnp.stack(maxes), jnp.stack(sums)
    w = s_all * jnp.exp(m_all - jnp.max(m_all, axis=0, keepdims=True))
    out = jnp.sum(w[..., None] * o_all, axis=0) / jnp.sum(w, axis=0)[..., None]
    return out[:, :, :S].astype(q.dtype)


def memory_cross_attention(h, mn, w_xq, w_xk, w_xv, w_xo):
    B, S, D = h.shape
    q = (h @ w_xq).reshape(B, S, X_HEADS, X_HEAD_DIM)
    km = (mn @ w_xk).reshape(B, N_MEM, X_HEADS, X_HEAD_DIM)
    vm = (mn @ w_xv).reshape(B, N_MEM, X_HEADS, X_HEAD_DIM)
    sc = jnp.einsum('bshd,bmhd->bhsm', q, km).astype(jnp.float32) * (X_HEAD_DIM ** -0.5)
    p = jax.nn.softmax(sc, axis=-1).astype(h.dtype)
    o = jnp.einsum('bhsm,bmhd->bshd', p, vm).reshape(B, S, D)
    return o @ w_xo


def peer(h, w_pq, sub_keys, u_tab, v_tab):
    B, S, D = h.shape
    T = B * S
    xt = h.reshape(T, D)
    q = (xt @ w_pq).reshape(T, PEER_HEADS, 2, PK_DIM // 2)
    s_half = jnp.einsum('thpc,hpkc->thpk', q, sub_keys).astype(jnp.float32)
    top_s, top_i = lax.top_k(s_half, PK_TOPK)
    cand_s = top_s[:, :, 0, :, None] + top_s[:, :, 1, None, :]
    cand_i = top_i[:, :, 0, :, None] * N_KEYS + top_i[:, :, 1, None, :]
    best_s, best_pos = lax.top_k(cand_s.reshape(T, PEER_HEADS, PK_TOPK * PK_TOPK), PK_TOPK)
    ids = jnp.take_along_axis(cand_i.reshape(T, PEER_HEADS, PK_TOPK * PK_TOPK), best_pos, axis=-1)
    gate = jax.nn.softmax(best_s, axis=-1)
    nblk = T // PEER_BLOCK

    def block(args):
        xb, idb, gb = args
        u = u_tab[idb]
        act = jax.nn.gelu(jnp.einsum('td,thkd->thk', xb, u).astype(jnp.float32), approximate=False)
        coef = (gb * act).astype(xb.dtype)
        return jnp.einsum('thk,thkd->td', coef, v_tab[idb])

    out = lax.map(block, (xt.reshape(nblk, PEER_BLOCK, D),
                          ids.reshape(nblk, PEER_BLOCK, PEER_HEADS, PK_TOPK),
                          gate.reshape(nblk, PEER_BLOCK, PEER_HEADS, PK_TOPK)))
    return out.reshape(B, S, D)


def setup_inputs(seed: int = 0) -> dict:
    key = jax.random.key(seed)
    ks = jax.random.split(key, 26)
    L = DEPTH

    def nrm(k, shape, scale):
        return jax.random.normal(k, shape, jnp.float32) * scale

    def gain(k, shape):
        return 1.0 + 0.02 * jax.random.normal(k, shape, jnp.float32)

    return {
        "x": nrm(ks[0], (BATCH, SEQ, D_MODEL), 1.0),
        "mem": nrm(ks[1], (BATCH, N_MEM, D_MODEL), 1.0),
        "g_mix": gain(ks[2], (L, D_MODEL)),
        "w_in": nrm(ks[3], (L, D_MODEL, IN_COLS), D_MODEL ** -0.5),
        "conv_w": nrm(ks[4], (L, CONV_W, M_WIDTH), CONV_W ** -0.5),
        "conv_b": nrm(ks[5], (L, M_WIDTH), 0.02),
        "w_mq": nrm(ks[6], (L, M_HEADS, M_HEAD_DIM, M_HEAD_DIM), M_HEAD_DIM ** -0.5),
        "w_mk": nrm(ks[7], (L, M_HEADS, M_HEAD_DIM, M_HEAD_DIM), M_HEAD_DIM ** -0.5),
        "b_mi": nrm(ks[8], (L, M_HEADS), 0.1),
        "b_mf": jnp.linspace(3.0, 6.0, M_HEADS, dtype=jnp.float32)[None, :] + nrm(ks[9], (L, M_HEADS), 0.1),
        "g_mhead": gain(ks[10], (L, M_HEADS, M_HEAD_DIM)),
        "g_ahead": gain(ks[11], (L, A_HEADS, A_HEAD_DIM)),
        "w_out": nrm(ks[12], (L, MIX_WIDTH, D_MODEL), MIX_WIDTH ** -0.5),
        "g_cross": gain(ks[13], (L, D_MODEL)),
        "g_mem": gain(ks[14], (L, D_MODEL)),
        "w_xq": nrm(ks[15], (L, D_MODEL, D_MODEL), D_MODEL ** -0.5),
        "w_xk": nrm(ks[16], (L, D_MODEL, D_MODEL), D_MODEL ** -0.5),
        "w_xv": nrm(ks[17], (L, D_MODEL, D_MODEL), D_MODEL ** -0.5),
        "w_xo": nrm(ks[18], (L, D_MODEL, D_MODEL), D_MODEL ** -0.5),
        "g_ffn": gain(ks[19], (L, D_MODEL)),
        "w_pq": nrm(ks[20], (L, D_MODEL, PEER_HEADS * PK_DIM), D_MODEL ** -0.5),
        "sub_keys": nrm(ks[21], (L, PEER_HEADS, 2, N_KEYS, PK_DIM // 2), (PK_DIM // 2) ** -0.5),
        "u_tab": nrm(ks[22], (L, N_EXPERTS, D_MODEL), D_MODEL ** -0.5),
        "v_tab": nrm(ks[23], (L, N_EXPERTS, D_MODEL), PEER_HEADS ** -0.5),
        "g_final": gain(ks[24], (D_MODEL,)),
    }


def reference(x, mem, g_mix, w_in, conv_w, conv_b, w_mq, w_mk, b_mi, b_mf, g_mhead, g_ahead, w_out,
              g_cross, g_mem, w_xq, w_xk, w_xv, w_xo, g_ffn, w_pq, sub_keys, u_tab, v_tab, g_final):
    B, S, _ = x.shape
    pos = jnp.arange(S)
    for l in range(DEPTH):
        h = rmsnorm(x, g_mix[l])
        proj = h @ w_in[l]
        m_in, m_v, m_o, m_i, m_f, a_q, a_k, a_v = jnp.split(proj, IN_SPLITS, axis=-1)
        c = jax.nn.silu(causal_conv(m_in, conv_w[l], conv_b[l])).reshape(B, S, M_HEADS, M_HEAD_DIM)
        mq = jnp.einsum('bshd,hde->bhse', c, w_mq[l])
        mk = jnp.einsum('bshd,hde->bhse', c, w_mk[l])
        mv = heads(m_v, M_HEADS)
        hm = mlstm_chunkwise(mq, mk, mv, (m_i + b_mi[l]).transpose(0, 2, 1), (m_f + b_mf[l]).transpose(0, 2, 1))
        hm = rmsnorm(hm.transpose(0, 2, 1, 3), g_mhead[l]).reshape(B, S, M_WIDTH) * jax.nn.sigmoid(m_o)
        qa = rope(heads(a_q, A_HEADS), pos)
        ka = rope(heads(a_k, A_HEADS), pos)
        va = heads(a_v, A_HEADS)
        ha = dilated_mixture(qa, ka, va)
        ha = rmsnorm(ha.transpose(0, 2, 1, 3), g_ahead[l]).reshape(B, S, A_WIDTH)
        x = x + jnp.concatenate([hm, ha], axis=-1) @ w_out[l]
        x = x + memory_cross_attention(rmsnorm(x, g_cross[l]), rmsnorm(mem, g_mem[l]),
                                       w_xq[l], w_xk[l], w_xv[l], w_xo[l])
        x = x + peer(rmsnorm(x, g_ffn[l]), w_pq[l], sub_keys[l], u_tab[l], v_tab[l])
    return rmsnorm(x, g_final)
```

```python
import numpy as np
import ml_dtypes
from contextlib import ExitStack
import concourse.bass as bass
import concourse.mybir as mybir
from concourse.bass_utils import run_bass_kernel_spmd

F32 = mybir.dt.float32
BF16 = mybir.dt.bfloat16
I32 = mybir.dt.int32
U32 = mybir.dt.uint32
ALU = mybir.AluOpType
AF = mybir.ActivationFunctionType
AX = mybir.AxisListType

D = 2048
NCH = 16
WIN = 4096
OWN = 1024
NPRE = WIN - OWN
EPS = 1e-6
IN_COLS = 6152
NDS = 40


class Sched:
    LIM = 3500
    DLIM = 240

    def __init__(self, nc, es):
        self.nc = nc
        self.es = es
        self.engs = {"pe": nc.tensor, "act": nc.scalar, "dve": nc.vector, "pool": nc.gpsimd, "sp": nc.sync}
        self.nsem = 0
        self.h = {}
        self.epoch = {k: 0 for k in self.engs}
        self.cnt = {k: 0 for k in self.engs}
        for k in self.engs:
            self.h[(k, 0)] = self._new()
        self.seen = {k: {} for k in self.engs}
        self.dver = [0] * NDS
        self.dcnt = [0] * NDS
        for i in range(NDS):
            self.h[("d", i, 0)] = self._new()
        self.dnext = {"sp": 0, "pool": NDS // 2, "act": 0}
        self.lastw = {}
        self.rd = {}
        self.nwaits = 0
        self.nins = 0

    def _new(self):
        self.nsem += 1
        return self.es.enter_context(self.nc.semaphore(f"s{self.nsem}"))

    def _wait(self, eng, sk, val):
        if val <= 0:
            return
        if self.seen[eng].get(sk, 0) >= val:
            return
        self.seen[eng][sk] = val
        self.engs[eng].wait_ge(self.h[sk], val)
        self.nwaits += 1

    def _deps(self, eng, reads, writes):
        need = {}

        def add(t, war=False):
            if t is None:
                return
            sk, val = t
            if sk[0] == eng and eng == "pe":
                return
            if need.get(sk, 0) < val:
                need[sk] = val

        for k in reads:
            add(self.lastw.get(k))
        for k in writes:
            add(self.lastw.get(k))
            for t in self.rd.get(k, ()):
                add(t, war=True)
        for sk, val in need.items():
            self._wait(eng, sk, val)

    def _commit(self, ticket, reads, writes):
        for k in reads:
            self.rd.setdefault(k, []).append(ticket)
        for k in writes:
            self.lastw[k] = ticket
            self.rd[k] = []

    def op(self, eng, fn, reads=(), writes=()):
        self._deps(eng, reads, writes)
        if self.cnt[eng] >= self.LIM:
            self.epoch[eng] += 1
            self.cnt[eng] = 0
            self.h[(eng, self.epoch[eng])] = self._new()
        sk = (eng, self.epoch[eng])
        ins = fn(self.engs[eng])
        ins.then_inc(self.h[sk], 1)
        self.cnt[eng] += 1
        self.nins += 1
        self._commit((sk, self.cnt[eng]), reads, writes)

    def dma(self, q, out, in_, reads=(), writes=(), fn=None):
        i = self.dnext[q]
        half = NDS // 2
        base = half if q == "pool" else 0
        self.dnext[q] = base + (i - base + 1) % half
        sk = ("d", i, self.dver[i])
        self._wait(q, sk, 16 * self.dcnt[i])
        if self.dcnt[i] >= self.DLIM:
            self.dver[i] += 1
            self.dcnt[i] = 0
            sk = ("d", i, self.dver[i])
            self.h[sk] = self._new()
        self._deps(q, reads, writes)
        if fn is None:
            ins = self.engs[q].dma_start(out=out, in_=in_)
        else:
            ins = fn(self.engs[q])
        ins.then_inc(self.h[sk], 16)
        self.dcnt[i] += 1
        self.nins += 1
        self._commit((sk, 16 * self.dcnt[i]), reads, writes)

    def barrier(self):
        for e in self.engs:
            for e2 in self.engs:
                if e2 != e:
                    if self.cnt[e2] > 0:
                        self._wait(e, (e2, self.epoch[e2]), self.cnt[e2])
                    elif self.epoch[e2] > 0:
                        self._wait(e, (e2, self.epoch[e2] - 1), self.LIM)
            for i in range(NDS):
                if self.dcnt[i] > 0:
                    self._wait(e, ("d", i, self.dver[i]), 16 * self.dcnt[i])
                elif self.dver[i] > 0:
                    self._wait(e, ("d", i, self.dver[i] - 1), 16 * self.DLIM)
        self.lastw = {}
        self.rd = {}


def bcast_free(ap_col, n):
    return ap_col.to_broadcast([ap_col.shape[0], n])


class K:
    pass


def load_weight_bf16(S, nc, es, w_dram, col0, ncols, name, stage, stage_key):
    wt = es.enter_context(nc.sbuf_tensor(name, [128, NCH, ncols], BF16))
    wv = w_dram.rearrange("(c p) n -> p c n", p=128)
    step = max(1, 2048 // ncols)
    c = 0
    i = 0
    while c < NCH:
        nn = min(step, NCH - c)
        sl = i % 2
        st = stage[sl]
        sv = st[:, 0:nn * ncols].rearrange("p (c n) -> p c n", n=ncols)
        S.dma("sp", sv, wv[:, c:c + nn, col0:col0 + ncols], writes=[(stage_key, sl)])
        eng = "pool" if i % 2 == 0 else "dve"
        S.op(eng, lambda e, c=c, nn=nn, sv=sv: e.tensor_copy(wt[:, c:c + nn, :], sv),
             reads=[(stage_key, sl)], writes=[(name, c + q) for q in range(nn)])
        c += nn
        i += 1
    return wt


def build(dbg=None):
    nc = bass.Bass("TRN2", target_bir_lowering=False)
    try:
        nc.allow_low_precision("bf16 matmuls with fp32 accumulation")
    except Exception:
        pass

    def din(name, shape, dt=F32):
        return nc.dram_tensor(name, list(shape), dt, kind="ExternalInput").ap()

    def dint(name, shape, dt=BF16):
        return nc.dram_tensor(name, list(shape), dt, kind="Internal").ap()

    x_win = din("x_win", [WIN, D])
    w_in = din("w_in", [D, IN_COLS])
    g_mix = din("g_mix", [1, D])
    cosT = din("cosT", [128, WIN])
    sinT = din("sinT", [128, WIN])
    ident_d = din("ident", [128, 128], BF16)
    rot_d = din("rotT", [128, 128], BF16)

    cw_d = din("cw_h", [128, 8, 4])
    cb_d = din("cb_h", [128, 8])
    gm_d = din("gm_h", [128, 8])
    gb_d = din("gb_h", [1, 8])
    pm_d = din("pm_h", [128, 32])
    tri_d = din("tri_f", [128, 128])
    w_mq_d = din("w_mq", [4, 256, 256])
    w_mk_d = din("w_mk", [4, 256, 256])

    kbA_d = din("kbA", [128, 48])
    ga_d = din("ga_h", [128, 8])
    trl_d = din("trl_f", [128, 128])
    w_out_d = din("w_out", [D, D])
    mem_d = din("mem_b", [256, D])
    g_ffn_d = din("g_ffn", [1, D])
    iota_d = din("iota256", [1, 256])
    w_pq_d = din("w_pq", [D, D])
    skT_d = din("skT_h", [128, 16, 128])
    u_tab_d = din("u_tab", [16384, D])
    v_tab_d = din("v_tab", [16384, D])
    g_mem_d = din("g_mem", [1, D])
    g_cross_d = din("g_cross", [1, D])
    w_xq_d = din("w_xq", [D, D]); w_xk_d = din("w_xk", [D, D]); w_xv_d = din("w_xv", [D, D]); w_xo_d = din("w_xo", [D, D])
    g_final_d = din("g_final", [1, D])
    out_d = nc.dram_tensor("out", [OWN, D], F32, kind="ExternalOutput").ap()
    dbg_out = None
    if dbg is not None:
        dbg_out = nc.dram_tensor("dbg", list(dbg["shape"]), dbg.get("dt", F32), kind="ExternalOutput").ap()

    uv_d = dint("uv_tab", [16384, 2 * D])
    s_minT = dint("s_minT", [1024, WIN])
    s_mv = dint("s_mv", [WIN, 1024])
    s_gates = dint("s_gates", [WIN, 8], F32)
    s_akT = dint("s_akT", [1024, WIN])
    s_av = dint("s_av", [WIN, 1024])

    with ExitStack() as es:
        es.enter_context(nc.allow_low_precision(reason="bf16 operands, fp32 accumulation"))
        S = Sched(nc, es)
        ident = es.enter_context(nc.sbuf_tensor("identb", [128, 128], BF16))
        rotT = es.enter_context(nc.sbuf_tensor("rotTb", [128, 128], BF16))
        S.dma("sp", ident[:], ident_d[:, :], writes=["ident"])
        S.dma("sp", rotT[:], rot_d[:, :], writes=["rotT"])
        es_mix = es
        es_mix2 = es
        moT = es_mix.enter_context(nc.sbuf_tensor("moT", [128, 8, OWN], BF16))
        aqT = es_mix.enter_context(nc.sbuf_tensor("aqT", [128, 8, OWN], BF16))


        def norm_tile(pes, x_src_ap, gbc, hT, col0, xkey, part="both"):
            xt, xb, ss, rstd, psT = pes["xt"], pes["xb"], pes["ss"], pes["rstd"], pes["psT"]
            if part in ("both", "pre"):
                norm_tile_pre(pes, x_src_ap, gbc)
            if part in ("both", "post"):
                norm_tile_post(pes, hT, col0, xkey)

        def norm_tile_pre(pes, x_src_ap, gbc):
            xt, xb, ss, rstd, psT = pes["xt"], pes["xb"], pes["ss"], pes["rstd"], pes["psT"]
            S.dma("sp", xt[:], x_src_ap, writes=["xt"])
            S.op("act", lambda e: e.activation(out=xb[:], in_=xt[:], func=AF.Square, scale=float(D) ** -0.5,
                                               accum_out=ss[:]),
                 reads=["xt"], writes=["xb", "ss"])
            S.op("dve", lambda e: e.tensor_scalar_add(rstd[:], ss[:], EPS), reads=["ss"], writes=["rstd"])
            S.op("act", lambda e: e.sqrt(rstd[:], rstd[:]), reads=["rstd"], writes=["rstd"])
            S.op("dve", lambda e: e.reciprocal(rstd[:], rstd[:]), reads=["rstd"], writes=["rstd"])
            S.op("dve", lambda e: e.scalar_tensor_tensor(out=xb[:], in0=xt[:], scalar=rstd[:, 0:1], in1=gbc[:],
                                                         op0=ALU.mult, op1=ALU.mult),
                 reads=["xt", "rstd", "gbc"], writes=["xb"])

        def norm_tile_post(pes, hT, col0, xkey):
            xt, xb, ss, rstd, psT = pes["xt"], pes["xb"], pes["ss"], pes["rstd"], pes["psT"]
            for half in range(2):
                for j in range(8):
                    c = half * 8 + j
                    S.op("pe", lambda e, c=c, j=j, half=half: e.transpose(
                        psT[half][:, j * 128:(j + 1) * 128], xb[:, c * 128:(c + 1) * 128], ident[:]),
                        reads=["xb", "ident"], writes=[("psT", half)])
                eng = "act" if half == 0 else "dve"
                if eng == "act":
                    S.op("act", lambda e, half=half: e.copy(
                        out=hT[:, half * 8:(half + 1) * 8, col0:col0 + 128],
                        in_=psT[half][:].rearrange("p (c t) -> p c t", c=8)),
                        reads=[("psT", half)], writes=[xkey])
                else:
                    S.op("dve", lambda e, half=half: e.tensor_copy(
                        hT[:, half * 8:(half + 1) * 8, col0:col0 + 128],
                        psT[half][:].rearrange("p (c t) -> p c t", c=8)),
                        reads=[("psT", half)], writes=[xkey])

        def phase_proj(name, st_list, specs, gain_d):
            with ExitStack() as pes_:
                pes = {}
                pes["xt"] = pes_.enter_context(nc.sbuf_tensor(name + "xt", [128, D], F32))
                pes["xb"] = pes_.enter_context(nc.sbuf_tensor(name + "xb", [128, D], BF16))
                pes["ss"] = pes_.enter_context(nc.sbuf_tensor(name + "ss", [128, 1], F32))
                pes["rstd"] = pes_.enter_context(nc.sbuf_tensor(name + "rstd", [128, 1], F32))
                pes["psT"] = [pes_.enter_context(nc.psum_tensor(name + f"psT{i}", [128, 1024], BF16)) for i in range(2)]
                gbc = pes_.enter_context(nc.sbuf_tensor(name + "gbc", [128, D], F32))
                S.dma("sp", gbc[:], gain_d.partition_broadcast(128), writes=["gbc"])
                hT = [pes_.enter_context(nc.sbuf_tensor(name + f"hT{i}", [128, NCH, 512], BF16)) for i in range(2)]
                stage = [pes_.enter_context(nc.sbuf_tensor(name + f"wst{i}", [128, 2048], F32)) for i in range(2)]
                psM = [pes_.enter_context(nc.psum_tensor(name + f"psM{i}", [128, 512], F32)) for i in range(4)]
                for sub in range(4):
                    t0_ = st_list[0] * 512 + sub * 128
                    norm_tile(pes, x_win[t0_:t0_ + 128, :], gbc, hT[0], sub * 128, ("hT", 0))
                ws = []
                for si, sp in enumerate(specs):
                    ws.append(load_weight_bf16(S, nc, pes_, w_in, sp["col0"], sp["ncols"], f"{name}w{si}", stage,
                                               name + "wst"))
                env = dict(pes_=pes_, psM=psM)
                for sp in specs:
                    if "setup" in sp:
                        sp["setup"](env)
                pmc = [0]

                def emit_norm(sti, sub, part="both"):
                    t0 = st_list[sti] * 512 + sub * 128
                    norm_tile(pes, x_win[t0:t0 + 128, :], gbc, hT[sti % 2], sub * 128, ("hT", sti % 2), part=part)

                def grp_f(st, h, hkey, sp, w, wkeys, cc):
                    ps = psM[pmc[0] % 4]
                    pk = ("psM", pmc[0] % 4)
                    pmc[0] += 1
                    for c in range(NCH):
                        S.op("pe", lambda e, c=c: e.matmul(
                            ps[:, :], lhsT=w[:, c, cc * 128:(cc + 1) * 128], rhs=h[:, c, :],
                            start=(c == 0), stop=(c == NCH - 1)), reads=[hkey] + wkeys, writes=[pk])
                    sp["evac"](env, st, cc, ps, pk)

                def grp_t(st, h, hkey, sp, w, wkeys, sub, nb):
                    n0 = nb * 512
                    nn = min(512, sp["ncols"] - n0)
                    ps = psM[pmc[0] % 4]
                    pk = ("psM", pmc[0] % 4)
                    pmc[0] += 1
                    for c in range(NCH):
                        S.op("pe", lambda e, c=c: e.matmul(
                            ps[:, 0:nn], lhsT=h[:, c, sub * 128:(sub + 1) * 128],
                            rhs=w[:, c, n0:n0 + nn], start=(c == 0), stop=(c == NCH - 1)),
                            reads=[hkey] + wkeys, writes=[pk])
                    sp["evac"](env, st, sub, nb, ps, pk, nn)

                for sti, st in enumerate(st_list):
                    h = hT[sti % 2]
                    hkey = ("hT", sti % 2)
                    groups = []
                    for si, sp in enumerate(specs):
                        w = ws[si]
                        wkeys = [(f"{name}w{si}", c) for c in range(NCH)]
                        if sp["kind"] == "f":
                            for cc in range(sp["ncols"] // 128):
                                groups.append(lambda st=st, h=h, hkey=hkey, sp=sp, w=w, wkeys=wkeys, cc=cc:
                                              grp_f(st, h, hkey, sp, w, wkeys, cc))
                        else:
                            for sub in range(4):
                                for nb in range((sp["ncols"] + 511) // 512):
                                    groups.append(lambda st=st, h=h, hkey=hkey, sp=sp, w=w, wkeys=wkeys, sub=sub, nb=nb:
                                                  grp_t(st, h, hkey, sp, w, wkeys, sub, nb))
                    per = (len(groups) + 3) // 4
                    for k in range(4):
                        if sti + 1 < len(st_list):
                            emit_norm(sti + 1, k, "pre")
                        for g in groups[k * per:(k + 1) * per]:
                            g()
                        if sti + 1 < len(st_list):
                            emit_norm(sti + 1, k, "post")
                    for sp in specs:
                        if "flush" in sp:
                            sp["flush"](env, st)
                S.barrier()

        def mk_f_to_dram(dst, nchunks, tag, rope=False, sb_dst=None, st0=0, func=None):
            st_ = {}

            def setup(env):
                if sb_dst is None:
                    st_["stg"] = [env["pes_"].enter_context(nc.sbuf_tensor(f"{tag}stg{i}", [128, nchunks, 512], BF16))
                                  for i in range(2)]
                st_["n"] = 0
                if func == "sigexp":
                    st_["sg"] = env["pes_"].enter_context(nc.sbuf_tensor(f"{tag}sg", [128, 512], F32))
                if rope:
                    st_["kb"] = env["pes_"].enter_context(nc.sbuf_tensor(f"{tag}kb", [128, 512], BF16))
                    st_["t1"] = env["pes_"].enter_context(nc.sbuf_tensor(f"{tag}t1", [128, 512], F32))
                    st_["t2"] = env["pes_"].enter_context(nc.sbuf_tensor(f"{tag}t2", [128, 512], F32))
                    st_["psR"] = env["pes_"].enter_context(nc.psum_tensor(f"{tag}psR", [128, 512], F32))
                    st_["cos"] = env["pes_"].enter_context(nc.sbuf_tensor(f"{tag}cos", [128, 512], F32))
                    st_["sin"] = env["pes_"].enter_context(nc.sbuf_tensor(f"{tag}sin", [128, 512], F32))

            def evac(env, st, cc, ps, pk):
                if sb_dst is None:
                    sl = st_["n"] % 2
                    oap = st_["stg"][sl][:, cc, :]
                    okey = (tag + "stg", sl)
                else:
                    oap = sb_dst[:, cc, (st - st0) * 512:(st - st0 + 1) * 512]
                    okey = (tag + "sb", cc, st)
                if not rope:
                    if func == "sigexp":
                        sg = st_["sg"]
                        S.op("act", lambda e: e.activation(out=sg[:], in_=ps[:, :], func=AF.Exp, scale=-1.0),
                             reads=[pk], writes=[tag + "sg"])
                        S.op("dve", lambda e: e.tensor_scalar_add(sg[:], sg[:], 1.0), reads=[tag + "sg"], writes=[tag + "sg"])
                        S.op("dve", lambda e: e.reciprocal(oap, sg[:]), reads=[tag + "sg"], writes=[okey])
                    elif func is not None:
                        S.op("act", lambda e: e.activation(out=oap, in_=ps[:, :], func=func), reads=[pk], writes=[okey])
                    elif cc % 2 == 0:
                        S.op("act", lambda e: e.copy(out=oap, in_=ps[:, :]), reads=[pk], writes=[okey])
                    else:
                        S.op("dve", lambda e: e.tensor_copy(oap, ps[:, :]), reads=[pk], writes=[okey])
                else:
                    kb, t1, t2, psR = st_["kb"], st_["t1"], st_["t2"], st_["psR"]
                    p0 = 0
                    if cc == 0:
                        S.dma("sp", st_["cos"][:], cosT[:, st * 512:(st + 1) * 512], writes=[tag + "cos"])
                        S.dma("sp", st_["sin"][:], sinT[:, st * 512:(st + 1) * 512], writes=[tag + "sin"])
                    S.op("act", lambda e: e.copy(out=kb[:], in_=ps[:, :]), reads=[pk], writes=[tag + "kb"])
                    S.op("pe", lambda e: e.matmul(psR[:, :], lhsT=rotT[:], rhs=kb[:], start=True, stop=True),
                         reads=[tag + "kb", "rotT"], writes=[tag + "psR"])
                    S.op("dve", lambda e: e.tensor_tensor(out=t1[:], in0=kb[:], in1=st_["cos"][:, 0:512], op=ALU.mult),
                         reads=[tag + "kb", tag + "cos"], writes=[tag + "t1"])
                    S.op("act", lambda e: e.copy(out=t2[:], in_=psR[:, :]), reads=[tag + "psR"], writes=[tag + "t2"])
                    S.op("dve", lambda e: e.tensor_tensor(out=t2[:], in0=t2[:], in1=st_["sin"][:, 0:512], op=ALU.mult),
                         reads=[tag + "t2", tag + "sin"], writes=[tag + "t2"])
                    S.op("dve", lambda e: e.tensor_tensor(out=oap, in0=t1[:], in1=t2[:], op=ALU.add),
                         reads=[tag + "t1", tag + "t2"], writes=[okey])

            def flush(env, st):
                if sb_dst is None:
                    sl = st_["n"] % 2
                    stg = st_["stg"][sl]
                    S.dma("pool", dst.rearrange("(c p) t -> p c t", p=128)[:, :, st * 512:(st + 1) * 512], stg[:],
                          reads=[(tag + "stg", sl)], writes=[(tag + "dram", st)])
                st_["n"] += 1

            return dict(setup=setup, evac=evac, flush=flush)

        def mk_t_to_dram(dst, ncols, tag, dt=BF16):
            st_ = {}

            def setup(env):
                st_["stg"] = [env["pes_"].enter_context(nc.sbuf_tensor(f"{tag}stg{i}", [128, 4, ncols], dt))
                              for i in range(2)]
                st_["n"] = 0

            def evac(env, st, sub, nb, ps, pk, nn):
                sl = st_["n"] % 2
                stg = st_["stg"][sl]
                skey = (tag + "stg", sl)
                if (sub + nb) % 2 == 0:
                    S.op("act", lambda e: e.copy(out=stg[:, sub, nb * 512:nb * 512 + nn], in_=ps[:, 0:nn]),
                         reads=[pk], writes=[skey])
                else:
                    S.op("dve", lambda e: e.tensor_copy(stg[:, sub, nb * 512:nb * 512 + nn], ps[:, 0:nn]),
                         reads=[pk], writes=[skey])

            def flush(env, st):
                sl = st_["n"] % 2
                stg = st_["stg"][sl]
                S.dma("pool", dst[st * 512:(st + 1) * 512, :].rearrange("(s p) n -> p s n", p=128), stg[:],
                      reads=[(tag + "stg", sl)], writes=[(tag + "dram", st)])
                st_["n"] += 1

            return dict(setup=setup, evac=evac, flush=flush)

        spA = [dict(kind="f", col0=0, ncols=1024, **mk_f_to_dram(s_minT, 8, "Amin")),
               dict(kind="t", col0=1024, ncols=1024, **mk_t_to_dram(s_mv, 1024, "Amv")),
               dict(kind="t", col0=3072, ncols=8, **mk_t_to_dram(s_gates, 8, "Ag", F32))]
        phase_proj("A", list(range(8)), spA, g_mix[0:1, :])

        if dbg is not None and dbg["name"] == "stopA":
            S.barrier()
            return nc
        spB = [dict(kind="f", col0=4104, ncols=1024, **mk_f_to_dram(s_akT, 8, "Bak", rope=True)),
               dict(kind="t", col0=5128, ncols=1024, **mk_t_to_dram(s_av, 1024, "Bav"))]
        phase_proj("B", list(range(2, 8)), spB, g_mix[0:1, :])
        if dbg is not None and dbg["name"] == "stopB":
            S.barrier()
            return nc
        spC = [dict(kind="f", col0=2048, ncols=1024, **mk_f_to_dram(None, 8, "Cmo", sb_dst=moT, st0=6, func="sigexp")),
               dict(kind="f", col0=3080, ncols=1024, **mk_f_to_dram(None, 8, "Caq", rope=True, sb_dst=aqT, st0=6))]
        phase_proj("C", [6, 7], spC, g_mix[0:1, :])

        mixT = es_mix2.enter_context(nc.sbuf_tensor("mixT", [128, NCH, OWN], BF16))
        print("after C", S.cnt, S.epoch, S.nsem)
        if dbg is not None and dbg["name"] == "aqT":
            S.dma("sp", dbg_out[0:1024, :].rearrange("(c p) t -> p c t", p=128), aqT[:], writes=["dbgo"])
            S.dma("sp", dbg_out[1024:2048, :].rearrange("(c p) t -> p c t", p=128), moT[:], writes=["dbgo2"])
            S.barrier()
            return nc
        with ExitStack() as ds:
            def sb(name, shape, dt=F32):
                return ds.enter_context(nc.sbuf_tensor("D" + name, shape, dt))

            def pst(name):
                return ds.enter_context(nc.psum_tensor("Dps" + name, [128, 512], F32))

            cw = sb("cw", [128, 8, 4]); cbias = sb("cbias", [128, 8]); gm = sb("gm", [128, 8]); gb = sb("gb", [128, 8])
            pm = sb("pm", [128, 32]); tri_f = sb("trif", [128, 128]); ones_f = sb("onesf", [128, 128])
            S.dma("sp", cw[:], cw_d[:, :, :], writes=["cw"])
            S.dma("sp", cbias[:], cb_d[:, :], writes=["cbias"])
            S.dma("sp", gm[:], gm_d[:, :], writes=["gm"])
            S.dma("sp", gb[:], gb_d[0:1, :].partition_broadcast(128), writes=["gb"])
            S.dma("sp", pm[:], pm_d[:, :], writes=["pm"])
            S.dma("sp", tri_f[:], tri_d[:, :], writes=["tri_f"])
            S.op("pool", lambda e: e.memset(ones_f[:], 1.0), writes=["ones_f"])
            wstg = sb("wstg", [128, 4, 2, 256])
            wq = sb("wq", [128, 4, 2, 256], BF16); wk = sb("wk", [128, 4, 2, 256], BF16)
            S.dma("sp", wstg[:], w_mq_d.rearrange("h (c p) e -> p h c e", p=128), writes=["wstg"])
            S.op("dve", lambda e: e.tensor_copy(wq[:], wstg[:]), reads=["wstg"], writes=["wq"])
            S.dma("sp", wstg[:], w_mk_d.rearrange("h (c p) e -> p h c e", p=128), reads=[], writes=["wstg"])
            S.op("dve", lambda e: e.tensor_copy(wk[:], wstg[:]), reads=["wstg"], writes=["wk"])
            Sst = [sb(f"Sst{h}", [128, 2, 384]) for h in range(4)]
            Sbf = [sb(f"Sbf{h}", [128, 2, 384], BF16) for h in range(4)]
            for h in range(4):
                S.op("pool", lambda e, h=h: e.memset(Sst[h][:], 0.0), writes=[("Sst", h, 0), ("Sst", h, 1)])
                S.op("pool", lambda e, h=h: e.memset(Sbf[h][:], 0.0), writes=[("Sbf", h)])
            xin = [sb(f"xin{i}", [128, 8, 131], BF16) for i in range(2)]
            vaug = [sb(f"vaug{i}", [128, 4, 384], BF16) for i in range(2)]
            gt = [sb(f"gt{i}", [128, 8]) for i in range(2)]
            cT = [sb(f"cT{i}", [128, 8, 128], BF16) for i in range(2)]
            for i in range(2):
                S.op("pool", lambda e, i=i: e.memset(vaug[i][:, :, 256:384], 1.0), writes=[("vaug", i)])
            acc = [sb(f"acc{i}", [128, 128]) for i in range(2)]
            g2 = sb("g2", [128, 8]); sp4 = sb("sp4", [128, 4]); w4 = sb("w4", [128, 4]); spb = sb("spb", [128, 4, 128])
            two = lambda nm, shape, dt=F32: [sb(f"{nm}{i}", shape, dt) for i in range(2)]
            kp = two("kp", [128, 256], BF16); kTb = two("kTb", [128, 2, 128], BF16); qTb = two("qTb", [128, 2, 128], BF16)
            clampT = two("clampT", [128, 128]); PT = two("PT", [128, 128], BF16); dd = two("dd", [128, 128])
            hn = two("hn", [128, 2, 128]); sq = two("sq", [128, 2, 128]); rr = two("rr", [128, 128]); tmpo = two("tmpo", [128, 128])
            dec = two("dec", [128, 1]); dSs = two("dSs", [128, 2, 384])
            psk = pst("k"); pskT = pst("kT"); psqT = pst("qT"); psS = pst("S"); psO = pst("O"); psMisc = pst("M")
            psdS = [pst("dS0"), pst("dS1")]
            minT_v = s_minT.rearrange("(c p) t -> p c t", p=128)

            NB0 = 3
            uin = [sb(f"cvu{i}", [128, D]) for i in range(NB0)]
            vin = [sb(f"cvv{i}", [128, D]) for i in range(NB0)]
            uvo = [sb(f"cvo{i}", [128, 2 * D], BF16) for i in range(NB0)]

            def conv_load(et):
                p = et % NB0
                S.dma("sp", uin[p][:], u_tab_d[et * 128:(et + 1) * 128, :], writes=[("cvu", p)])
                S.dma("sp", vin[p][:], v_tab_d[et * 128:(et + 1) * 128, :], writes=[("cvv", p)])

            def conv_cast_store(et):
                p = et % NB0
                S.op("act", lambda e: e.copy(out=uvo[p][:, 0:D], in_=uin[p][:]), reads=[("cvu", p)], writes=[("cvo", p, 0)])
                S.op("dve", lambda e: e.tensor_copy(uvo[p][:, D:2 * D], vin[p][:]), reads=[("cvv", p)], writes=[("cvo", p, 1)])
                S.dma("pool", uv_d[et * 128:(et + 1) * 128, :], uvo[p][:], reads=[("cvo", p, 0), ("cvo", p, 1)],
                      writes=[("uvd", et)])

            conv_load(0)
            conv_load(1)

            def d_loads(ck):
                par = ck % 2
                t0 = ck * 128
                xin_, vaug_, gt_ = xin[par], vaug[par], gt[par]
                if ck == 0:
                    S.op("pool", lambda e: e.memset(xin_[:, :, 0:3], 0.0), writes=[("xin", par)])
                    S.dma("sp", xin_[:, :, 3:131], minT_v[:, :, 0:128], writes=[("xin", par)])
                else:
                    S.dma("sp", xin_[:, :, 0:131], minT_v[:, :, t0 - 3:t0 + 128], writes=[("xin", par)])
                S.dma("sp", vaug_[:, :, 0:256], s_mv[t0:t0 + 128, :].rearrange("p (h e) -> p h e", h=4),
                      writes=[("vaug", par)])
                S.dma("sp", gt_[:], s_gates[t0:t0 + 128, :], writes=[("gt", par)])

            d_loads(0)
            for ck in range(32):
                own = ck >= 24
                par = ck % 2
                t0 = ck * 128
                xin_, vaug_, gt_, cT_ = xin[par], vaug[par], gt[par], cT[par]
                if ck + 1 < 32:
                    d_loads(ck + 1)
                for et in range(ck * 4, ck * 4 + 4):
                    if et + 2 < 128:
                        conv_load(et + 2)
                    conv_cast_store(et)
                S.op("dve", lambda e: e.tensor_tensor(out=g2[:], in0=gt_[:], in1=gb[:], op=ALU.add),
                     reads=[("gt", par), "gb"], writes=["g2"])
                S.op("dve", lambda e: e.tensor_scalar(out=g2[:, 0:4], in0=g2[:, 0:4], scalar1=pm[:, ck:ck + 1],
                                                      scalar2=None, op0=ALU.add), reads=["g2", "pm"], writes=["g2"])
                S.op("act", lambda e: e.activation(out=sp4[:], in_=g2[:, 4:8], func=AF.Exp, scale=-1.0),
                     reads=["g2"], writes=["sp4"])
                S.op("dve", lambda e: e.tensor_scalar_add(sp4[:], sp4[:], 1.0), reads=["sp4"], writes=["sp4"])
                S.op("act", lambda e: e.activation(out=sp4[:], in_=sp4[:], func=AF.Ln), reads=["sp4"], writes=["sp4"])
                S.op("pe", lambda e: e.matmul(psMisc[:, 256:260], lhsT=tri_f[:], rhs=sp4[:], start=True, stop=True),
                     reads=["tri_f", "sp4"], writes=["ps_csp"])
                S.op("act", lambda e: e.copy(out=w4[:], in_=psMisc[:, 256:260]), reads=["ps_csp"], writes=["w4"])
                S.op("dve", lambda e: e.tensor_tensor(out=w4[:], in0=g2[:, 0:4], in1=w4[:], op=ALU.add),
                     reads=["g2", "w4"], writes=["w4"])
                S.op("act", lambda e: e.activation(out=w4[:], in_=w4[:], func=AF.Exp), reads=["w4"], writes=["w4"])
                S.op("dve", lambda e: e.tensor_scalar_mul(w4[:], w4[:], 0.0625), reads=["w4"], writes=["w4"])
                S.op("dve", lambda e: e.tensor_copy(spb[:], sp4[:].unsqueeze(2).to_broadcast([128, 4, 128])),
                     reads=["sp4"], writes=["spb"])
                for c8 in range(8):
                    a_ = acc[c8 % 2]
                    ak = ("acc", c8 % 2)
                    S.op("dve", lambda e, c8=c8, a_=a_: e.tensor_scalar(out=a_[:], in0=xin_[:, c8, 0:128],
                                                                        scalar1=cw[:, c8, 0:1], scalar2=None,
                                                                        op0=ALU.mult),
                         reads=[("xin", par), "cw"], writes=[ak])
                    for jj in range(1, 4):
                        S.op("dve", lambda e, c8=c8, a_=a_, jj=jj: e.scalar_tensor_tensor(
                            out=a_[:], in0=xin_[:, c8, jj:jj + 128], scalar=cw[:, c8, jj:jj + 1], in1=a_[:],
                            op0=ALU.mult, op1=ALU.add), reads=[("xin", par), "cw", ak], writes=[ak])
                    S.op("act", lambda e, c8=c8, a_=a_: e.activation(out=cT_[:, c8, :], in_=a_[:], func=AF.Silu,
                                                                     bias=cbias[:, c8:c8 + 1]),
                         reads=[ak, "cbias"], writes=[("cT", par, c8)])
                def head_gen(h):
                    hp = h % 2
                    kp_, kTb_, qTb_, clampT_, PT_, dd_ = kp[hp], kTb[hp], qTb[hp], clampT[hp], PT[hp], dd[hp]
                    hn_, sq_, rr_, tmpo_, dec_, dSs_ = hn[hp], sq[hp], rr[hp], tmpo[hp], dec[hp], dSs[hp]
                    ckeys = [("cT", par, 2 * h), ("cT", par, 2 * h + 1)]
                    for dc in range(2):
                        S.op("pe", lambda e, dc=dc: e.matmul(psk[:, 0:256], lhsT=cT_[:, 2 * h + dc, :], rhs=wk[:, h, dc, :],
                                                             start=(dc == 0), stop=(dc == 1)),
                             reads=ckeys + ["wk"], writes=["psk"])
                    S.op("dve", lambda e: e.tensor_scalar(out=kp_[:], in0=psk[:, 0:256], scalar1=w4[:, h:h + 1],
                                                          scalar2=None, op0=ALU.mult), reads=["psk", "w4"], writes=[("kp", hp)])
                    yield
                    S.op("pe", lambda e: e.matmul(psMisc[:, 0:128], lhsT=spb[:, h, :], rhs=tri_f[:], start=True, stop=True),
                         reads=["spb", "tri_f"], writes=["ps_cb"])
                    S.op("act", lambda e: e.activation(out=dec_[:], in_=psMisc[:, 127:128], func=AF.Exp, scale=-1.0),
                         reads=["ps_cb"], writes=[("dec", hp)])
                    if own:
                        o0 = (ck - 24) * 128
                        S.op("act", lambda e: e.activation(out=clampT_[:], in_=psMisc[:, 0:128], func=AF.Exp),
                             reads=["ps_cb"], writes=[("clampT", hp)])
                        yield
                        for ec in range(2):
                            for dc in range(2):
                                S.op("pe", lambda e, ec=ec, dc=dc: e.matmul(
                                    pskT[:, ec * 128:(ec + 1) * 128], lhsT=wk[:, h, dc, ec * 128:(ec + 1) * 128],
                                    rhs=cT_[:, 2 * h + dc, :], start=(dc == 0), stop=(dc == 1)),
                                    reads=ckeys + ["wk"], writes=["pskT"])
                        for ec in range(2):
                            for dc in range(2):
                                S.op("pe", lambda e, ec=ec, dc=dc: e.matmul(
                                    psqT[:, ec * 128:(ec + 1) * 128], lhsT=wq[:, h, dc, ec * 128:(ec + 1) * 128],
                                    rhs=cT_[:, 2 * h + dc, :], start=(dc == 0), stop=(dc == 1)),
                                    reads=ckeys + ["wq"], writes=["psqT"])
                        S.op("act", lambda e: e.copy(out=kTb_[:], in_=pskT[:, 0:256].rearrange("p (c t) -> p c t", c=2)),
                             reads=["pskT"], writes=[("kTb", hp)])
                        S.op("dve", lambda e: e.tensor_copy(qTb_[:], psqT[:, 0:256].rearrange("p (c t) -> p c t", c=2)),
                             reads=["psqT"], writes=[("qTb", hp)])
                        yield
                        for ec in range(2):
                            S.op("pe", lambda e, ec=ec: e.matmul(psS[:, 0:128], lhsT=kTb_[:, ec, :], rhs=qTb_[:, ec, :],
                                                                 start=(ec == 0), stop=(ec == 1)),
                                 reads=[("kTb", hp), ("qTb", hp)], writes=["psS"])
                        S.op("act", lambda e: e.copy(out=dd_[:], in_=psS[:, 0:128]), reads=["psS"], writes=[("dd", hp)])
                        S.op("dve", lambda e: e.scalar_tensor_tensor(out=PT_[:], in0=dd_[:], scalar=w4[:, h:h + 1],
                                                                     in1=tri_f[:], op0=ALU.mult, op1=ALU.mult),
                             reads=[("dd", hp), "w4", "tri_f"], writes=[("PT", hp)])
                        yield
                        for j in range(3):
                            S.op("pe", lambda e, j=j: e.matmul(psO[:, j * 128:(j + 1) * 128],
                                                               lhsT=vaug_[:, h, j * 128:(j + 1) * 128], rhs=PT_[:],
                                                               start=True, stop=False),
                                 reads=[("vaug", par), ("PT", hp)], writes=["psO"])
                            for dc in range(2):
                                S.op("pe", lambda e, j=j, dc=dc: e.matmul(
                                    psO[:, j * 128:(j + 1) * 128], lhsT=Sbf[h][:, dc, j * 128:(j + 1) * 128],
                                    rhs=qTb_[:, dc, :], start=False, stop=(dc == 1)),
                                    reads=[("Sbf", h), ("qTb", hp)], writes=["psO"])
                        S.op("act", lambda e: e.activation(out=dd_[:], in_=psO[:, 256:384], func=AF.Abs),
                             reads=["psO"], writes=[("dd", hp)])
                        S.op("act", lambda e: e.copy(out=hn_[:], in_=psO[:, 0:256].rearrange("p (c t) -> p c t", c=2)),
                             reads=["psO"], writes=[("hn", hp)])
                        yield
                        S.op("dve", lambda e: e.tensor_tensor(out=dd_[:], in0=dd_[:], in1=clampT_[:], op=ALU.max),
                             reads=[("dd", hp), ("clampT", hp)], writes=[("dd", hp)])
                        S.op("dve", lambda e: e.reciprocal(dd_[:], dd_[:]), reads=[("dd", hp)], writes=[("dd", hp)])
                        S.op("dve", lambda e: e.tensor_tensor(
                            out=hn_[:], in0=hn_[:], in1=dd_[:].unsqueeze(1).to_broadcast([128, 2, 128]), op=ALU.mult),
                            reads=[("hn", hp), ("dd", hp)], writes=[("hn", hp)])
                        S.op("act", lambda e: e.activation(out=sq_[:], in_=hn_[:], func=AF.Square), reads=[("hn", hp)], writes=[("sq", hp)])
                        for j in range(2):
                            S.op("pe", lambda e, j=j: e.matmul(psMisc[:, 128:256], lhsT=ones_f[:], rhs=sq_[:, j, :],
                                                               start=(j == 0), stop=(j == 1)),
                                 reads=[("sq", hp), "ones_f"], writes=["ps_n"])
                        S.op("dve", lambda e: e.tensor_scalar(out=rr_[:], in0=psMisc[:, 128:256], scalar1=1.0 / 256,
                                                              scalar2=EPS, op0=ALU.mult, op1=ALU.add),
                             reads=["ps_n"], writes=[("rr", hp)])
                        yield
                        S.op("act", lambda e: e.sqrt(rr_[:], rr_[:]), reads=[("rr", hp)], writes=[("rr", hp)])
                        S.op("dve", lambda e: e.reciprocal(rr_[:], rr_[:]), reads=[("rr", hp)], writes=[("rr", hp)])
                        for j in range(2):
                            S.op("dve", lambda e, j=j: e.scalar_tensor_tensor(
                                out=tmpo_[:], in0=hn_[:, j, :], scalar=gm[:, 2 * h + j:2 * h + j + 1], in1=rr_[:],
                                op0=ALU.mult, op1=ALU.mult), reads=[("hn", hp), "gm", ("rr", hp)], writes=[("tmpo", hp)])
                            S.op("pool", lambda e, j=j: e.tensor_tensor(
                                out=mixT[:, 2 * h + j, o0:o0 + 128], in0=tmpo_[:], in1=moT[:, 2 * h + j, o0:o0 + 128],
                                op=ALU.mult), reads=[("tmpo", hp)], writes=[("mixT", 2 * h + j, ck)])
                    yield
                    for dc in range(2):
                        S.op("pe", lambda e, dc=dc: e.matmul(psdS[dc][:, 0:384], lhsT=kp_[:, dc * 128:(dc + 1) * 128],
                                                             rhs=vaug_[:, h, :], start=True, stop=True),
                             reads=[("kp", hp), ("vaug", par)], writes=[("psdS", dc)])
                    for dc in range(2):
                        S.op("dve", lambda e, dc=dc: e.tensor_scalar(out=Sst[h][:, dc, :], in0=Sst[h][:, dc, :],
                                                                     scalar1=dec_[:, 0:1], scalar2=None, op0=ALU.mult),
                             reads=[("Sst", h, dc), ("dec", hp)], writes=[("Sst", h, dc)])
                        S.op("act", lambda e, dc=dc: e.copy(out=dSs_[:, dc, :], in_=psdS[dc][:, 0:384]),
                             reads=[("psdS", dc)], writes=[(("dSs", hp), dc)])
                        S.op("dve", lambda e, dc=dc: e.scalar_tensor_tensor(
                            out=Sst[h][:, dc, :], in0=dSs_[:, dc, :], scalar=dec_[:, 0:1], in1=Sst[h][:, dc, :],
                            op0=ALU.mult, op1=ALU.add), reads=[(("dSs", hp), dc), ("dec", hp), ("Sst", h, dc)],
                            writes=[("Sst", h, dc)])
                    S.op("act", lambda e: e.copy(out=Sbf[h][:], in_=Sst[h][:]),
                         reads=[("Sst", h, 0), ("Sst", h, 1)], writes=[("Sbf", h)])
                for pair in ((0, 1), (2, 3)):
                    gens = [head_gen(h) for h in pair]
                    while gens:
                        for g in list(gens):
                            try:
                                next(g)
                            except StopIteration:
                                gens.remove(g)
            S.barrier()

        with ExitStack() as ds:
            def sb(name, shape, dt=F32):
                return ds.enter_context(nc.sbuf_tensor("E" + name, shape, dt))

            def pst(name):
                return ds.enter_context(nc.psum_tensor("Eps" + name, [128, 512], F32))

            kT = sb("kT", [128, 8, NPRE], BF16)
            S.dma("sp", kT[:], s_akT.rearrange("(c p) t -> p c t", p=128)[:, :, 1024:WIN], writes=["kT"])
            accN = sb("accN", [128, 8, OWN]); accD = sb("accD", [128, 8, OWN])
            VA = [sb(f"VA{i}", [128, 1024], BF16) for i in range(2)]
            VB = [sb(f"VB{i}", [128, 1024], BF16) for i in range(2)]
            two = lambda nm, shape, dt=F32: [sb(f"{nm}{i}", shape, dt) for i in range(2)]
            PA = two("PA", [128, 128], BF16); PB = two("PB", [128, 128], BF16)
            eA = two("eA", [128, 128]); eB = two("eB", [128, 128]); tO = two("tO", [128, 128]); tD = two("tD", [128, 128])
            kbA = sb("kbA", [128, 48]); ga = sb("ga", [128, 8])
            trl = sb("trl", [128, 128]); tru = sb("tru", [128, 128]); ones_b = sb("onesb", [128, 128], BF16)
            ones_f2 = sb("onesf2", [128, 128])
            S.dma("sp", kbA[:], kbA_d[:, :], writes=["kbA"])
            S.dma("sp", ga[:], ga_d[:, :], writes=["ga"])
            S.dma("sp", trl[:], trl_d[:, :], writes=["trl"])
            S.dma("sp", tru[:], tri_d[:, :], writes=["tru"])
            S.op("pool", lambda e: e.memset(ones_b[:], 1.0), writes=["ones_b"])
            S.op("pool", lambda e: e.memset(ones_f2[:], 1.0), writes=["ones_f2"])
            psA = [pst("A0"), pst("A1")]; psB = [pst("B0"), pst("B1")]
            psO = [pst("O0"), pst("O1")]; psD = [pst("D0"), pst("D1")]
            psN = psA[0]
            SC = 128.0 ** -0.5
            gi = 0
            for Q in range(2):
                glist = [(1, 0, sub) for sub in range(4)] + [(4, r, 0) for r in range(4)] + [(16, r, 0) for r in range(16)]
                for (d, r, sub) in glist:
                    nq = 128 if d < 16 else 32
                    u0 = 2048 + 512 * Q + r + 128 * sub
                    i0 = 512 * Q + r + 128 * sub
                    uA = u0 - 128 * d
                    par = gi % 2
                    va, vb = VA[par], VB[par]
                    S.dma("sp", va[:], bass.AP(tensor=s_av.tensor, offset=(1024 + uA) * 1024, ap=[[d * 1024, 128], [1, 1024]]),
                          writes=[("VA", par)])
                    S.dma("sp", vb[0:nq, :], bass.AP(tensor=s_av.tensor, offset=(1024 + u0) * 1024, ap=[[d * 1024, nq], [1, 1024]]),
                          writes=[("VB", par)])
                    qsl = slice(i0, i0 + (nq - 1) * d + 1, d)
                    def st1(h):
                        p2 = h % 2
                        q_ap = aqT[:, h, qsl]
                        S.op("pe", lambda e: e.matmul(psA[p2][:, 0:nq], lhsT=kT[:, h, uA:uA + 127 * d + 1:d], rhs=q_ap,
                                                      start=True, stop=True), reads=["kT"], writes=[("psA", p2)])
                        S.op("pe", lambda e: e.matmul(psB[p2][0:nq, 0:nq], lhsT=kT[:, h, u0:u0 + (nq - 1) * d + 1:d], rhs=q_ap,
                                                      start=True, stop=True), reads=["kT"], writes=[("psB", p2)])

                    def st2(h):
                        p2 = h % 2
                        S.op("act", lambda e: e.activation(out=eA[p2][:, 0:nq], in_=psA[p2][:, 0:nq], func=AF.Exp, scale=SC,
                                                           bias=kbA[:, gi:gi + 1]), reads=[("psA", p2), "kbA"], writes=[("eA", p2)])
                        S.op("act", lambda e: e.activation(out=eB[p2][0:nq, 0:nq], in_=psB[p2][0:nq, 0:nq], func=AF.Exp, scale=SC),
                             reads=[("psB", p2)], writes=[("eB", p2)])
                        S.op("dve", lambda e: e.tensor_tensor(out=PA[p2][:, 0:nq], in0=eA[p2][:, 0:nq], in1=trl[:, 0:nq], op=ALU.mult),
                             reads=[("eA", p2), "trl"], writes=[("PA", p2)])
                        S.op("dve", lambda e: e.tensor_tensor(out=PB[p2][0:nq, 0:nq], in0=eB[p2][0:nq, 0:nq], in1=tru[0:nq, 0:nq],
                                                              op=ALU.mult), reads=[("eB", p2), "tru"], writes=[("PB", p2)])

                    def st3(h):
                        p2 = h % 2
                        S.op("pe", lambda e: e.matmul(psO[p2][:, 0:nq], lhsT=va[:, h * 128:(h + 1) * 128], rhs=PA[p2][:, 0:nq],
                                                      start=True, stop=False), reads=[("VA", par), ("PA", p2)], writes=[("psO", p2)])
                        S.op("pe", lambda e: e.matmul(psO[p2][:, 0:nq], lhsT=vb[0:nq, h * 128:(h + 1) * 128], rhs=PB[p2][0:nq, 0:nq],
                                                      start=False, stop=True), reads=[("VB", par), ("PB", p2)], writes=[("psO", p2)])
                        S.op("pe", lambda e: e.matmul(psD[p2][:, 0:nq], lhsT=ones_b[:, :], rhs=PA[p2][:, 0:nq],
                                                      start=True, stop=False), reads=["ones_b", ("PA", p2)], writes=[("psD", p2)])
                        S.op("pe", lambda e: e.matmul(psD[p2][:, 0:nq], lhsT=ones_b[0:nq, :], rhs=PB[p2][0:nq, 0:nq],
                                                      start=False, stop=True), reads=["ones_b", ("PB", p2)], writes=[("psD", p2)])

                    def st4(h):
                        p2 = h % 2
                        akey = ("acc", h, Q)
                        if d == 1:
                            S.op("act", lambda e: e.copy(out=accN[:, h, qsl], in_=psO[p2][:, 0:nq]), reads=[("psO", p2)], writes=[akey])
                            S.op("act", lambda e: e.copy(out=accD[:, h, qsl], in_=psD[p2][:, 0:nq]), reads=[("psD", p2)], writes=[akey])
                        else:
                            S.op("act", lambda e: e.copy(out=tO[p2][:, 0:nq], in_=psO[p2][:, 0:nq]), reads=[("psO", p2)], writes=[("tO", p2)])
                            S.op("act", lambda e: e.copy(out=tD[p2][:, 0:nq], in_=psD[p2][:, 0:nq]), reads=[("psD", p2)], writes=[("tD", p2)])
                            S.op("dve", lambda e: e.tensor_tensor(out=accN[:, h, qsl], in0=accN[:, h, qsl], in1=tO[p2][:, 0:nq],
                                                                  op=ALU.add), reads=[("tO", p2), akey], writes=[akey])
                            S.op("dve", lambda e: e.tensor_tensor(out=accD[:, h, qsl], in0=accD[:, h, qsl], in1=tD[p2][:, 0:nq],
                                                                  op=ALU.add), reads=[("tD", p2), akey], writes=[akey])

                    for i_ in range(9):
                        if i_ < 8:
                            st1(i_)
                            st2(i_)
                        if i_ >= 1:
                            st3(i_ - 1)
                            st4(i_ - 1)
                    gi += 1
            o5 = sb("o5", [128, 512]); sq5 = sb("sq5", [128, 512]); r5 = sb("r5", [128, 512])
            for h in range(8):
                for Q in range(2):
                    cs = slice(Q * 512, (Q + 1) * 512)
                    akey = ("acc", h, Q)
                    S.op("dve", lambda e: e.reciprocal(r5[:], accD[:, h, cs]), reads=[akey], writes=["r5"])
                    S.op("dve", lambda e: e.tensor_tensor(out=o5[:], in0=accN[:, h, cs], in1=r5[:], op=ALU.mult),
                         reads=[akey, "r5"], writes=["o5"])
                    S.op("act", lambda e: e.activation(out=sq5[:], in_=o5[:], func=AF.Square), reads=["o5"], writes=["sq5"])
                    S.op("pe", lambda e: e.matmul(psN[:, :], lhsT=ones_f2[:], rhs=sq5[:], start=True, stop=True),
                         reads=["ones_f2", "sq5"], writes=[("psA", 0)])
                    S.op("dve", lambda e: e.tensor_scalar(out=r5[:], in0=psN[:, :], scalar1=1.0 / 128, scalar2=EPS,
                                                          op0=ALU.mult, op1=ALU.add), reads=[("psA", 0)], writes=["r5"])
                    S.op("act", lambda e: e.sqrt(r5[:], r5[:]), reads=["r5"], writes=["r5"])
                    S.op("dve", lambda e: e.reciprocal(r5[:], r5[:]), reads=["r5"], writes=["r5"])
                    S.op("dve", lambda e: e.scalar_tensor_tensor(out=mixT[:, 8 + h, cs], in0=o5[:], scalar=ga[:, h:h + 1],
                                                                 in1=r5[:], op0=ALU.mult, op1=ALU.mult),
                         reads=["o5", "ga", "r5"], writes=[("mixT", 8 + h, Q)])
            S.barrier()

        if dbg is not None and dbg["name"] == "mixT":
            S.dma("sp", dbg_out.rearrange("(c p) t -> p c t", p=128), mixT[:], writes=["dbgo"])
            S.barrier()

        def norm_sb(P, x_ap, xkeys, gbc, hT, col0, hkey, xn=None):
            t = P["tag"]
            xb, ss, psT = P["xb"], P["ss"], P["psT"]
            S.op("act", lambda e: e.activation(out=xb[:], in_=x_ap, func=AF.Square, scale=float(D) ** -0.5,
                                               accum_out=ss[:]), reads=xkeys, writes=[t + "xb", t + "ss"])
            S.op("dve", lambda e: e.tensor_scalar_add(ss[:], ss[:], EPS), reads=[t + "ss"], writes=[t + "ss"])
            S.op("act", lambda e: e.sqrt(ss[:], ss[:]), reads=[t + "ss"], writes=[t + "ss"])
            S.op("dve", lambda e: e.reciprocal(ss[:], ss[:]), reads=[t + "ss"], writes=[t + "ss"])
            if xn is not None:
                S.op("dve", lambda e: e.scalar_tensor_tensor(out=xn[:], in0=x_ap, scalar=ss[:, 0:1], in1=gbc[:],
                                                             op0=ALU.mult, op1=ALU.mult),
                     reads=list(xkeys) + [t + "ss", t + "gbc"], writes=[t + "xn"])
                if hT is not None:
                    S.op("pool", lambda e: e.tensor_copy(xb[:], xn[:]), reads=[t + "xn"], writes=[t + "xb"])
            else:
                S.op("dve", lambda e: e.scalar_tensor_tensor(out=xb[:], in0=x_ap, scalar=ss[:, 0:1], in1=gbc[:],
                                                             op0=ALU.mult, op1=ALU.mult),
                     reads=list(xkeys) + [t + "ss", t + "gbc"], writes=[t + "xb"])
            if hT is None:
                return
            for half in range(2):
                for j in range(8):
                    c = half * 8 + j
                    S.op("pe", lambda e, c=c, j=j, half=half: e.transpose(
                        psT[half][:, j * 128:(j + 1) * 128], xb[:, c * 128:(c + 1) * 128], ident[:]),
                        reads=[t + "xb"], writes=[(t + "psT", half)])
                if half == 0:
                    S.op("act", lambda e, half=half: e.copy(
                        out=hT[:, half * 8:(half + 1) * 8, col0:col0 + 128],
                        in_=psT[half][:].rearrange("p (c t) -> p c t", c=8)),
                        reads=[(t + "psT", half)], writes=[hkey])
                else:
                    S.op("dve", lambda e, half=half: e.tensor_copy(
                        hT[:, half * 8:(half + 1) * 8, col0:col0 + 128],
                        psT[half][:].rearrange("p (c t) -> p c t", c=8)),
                        reads=[(t + "psT", half)], writes=[hkey])

        def load_wblk2(t, w_dram, b2, stages, wbs, kc):
            wv = w_dram.rearrange("(c p) n -> p c n", p=128)
            wb = wbs[b2 % 2]
            for c4 in range(4):
                k = kc[0] % 2
                kc[0] += 1
                st = stages[k]
                S.dma("sp", st, wv[:, c4 * 4:(c4 + 1) * 4, b2 * 256:(b2 + 1) * 256], writes=[(t + "stage", k)])
                S.op("pool" if c4 % 2 == 0 else "dve",
                     lambda e, c4=c4, st=st: e.tensor_copy(wb[:, c4 * 4:(c4 + 1) * 4, :], st),
                     reads=[(t + "stage", k)], writes=[(t + "wb", b2 % 2, c4)])
            return wb, [(t + "wb", b2 % 2, c4) for c4 in range(4)]

        def load_wblk(t, w_dram, nb, stage, wb):
            wv = w_dram.rearrange("(c p) n -> p c n", p=128)
            for c4 in range(4):
                S.dma("sp", stage[:], wv[:, c4 * 4:(c4 + 1) * 4, nb * 512:(nb + 1) * 512], writes=[t + "stage"])
                S.op("pool" if c4 % 2 == 0 else "dve",
                     lambda e, c4=c4: e.tensor_copy(wb[:, c4 * 4:(c4 + 1) * 4, :], stage[:]),
                     reads=[t + "stage"], writes=[(t + "wb", c4)])
            return [(t + "wb", c4) for c4 in range(4)]

        xres = es.enter_context(nc.sbuf_tensor("xres", [128, 8, D], F32))
        for tt in range(8):
            S.dma("sp", xres[:, tt, :], x_win[NPRE + tt * 128:NPRE + (tt + 1) * 128, :], writes=[("xres", tt)])

        def add_proj(tagp, actT, w_dram):
            with ExitStack() as fs:
                stage = [fs.enter_context(nc.sbuf_tensor(f"{tagp}st{i}", [128, 4, 512], F32)) for i in range(2)]
                wblk = [fs.enter_context(nc.sbuf_tensor(f"{tagp}wb{i}", [128, NCH, 512], BF16)) for i in range(2)]
                tmp = [fs.enter_context(nc.sbuf_tensor(f"{tagp}tmp{i}", [128, 512], F32)) for i in range(2)]
                psF = [fs.enter_context(nc.psum_tensor(f"{tagp}ps{i}", [128, 512], F32)) for i in range(2)]
                wv = w_dram.rearrange("(c p) n -> p c n", p=128)
                k = 0
                n = 0
                for nb in range(4):
                    wb = wblk[nb % 2]
                    for c4 in range(4):
                        st = stage[k % 2]
                        S.dma("sp", st[:], wv[:, c4 * 4:(c4 + 1) * 4, nb * 512:(nb + 1) * 512], writes=[(tagp + "st", k % 2)])
                        S.op("pool" if k % 2 == 0 else "dve",
                             lambda e, st=st, wb=wb, c4=c4: e.tensor_copy(wb[:, c4 * 4:(c4 + 1) * 4, :], st[:]),
                             reads=[(tagp + "st", k % 2)], writes=[(tagp + "wb", nb % 2, c4)])
                        k += 1
                    for tt in range(8):
                        ps = psF[n % 2]
                        tm = tmp[n % 2]
                        for c in range(NCH):
                            S.op("pe", lambda e, c=c, ps=ps, wb=wb, tt=tt: e.matmul(
                                ps[:, :], lhsT=actT[:, c, tt * 128:(tt + 1) * 128], rhs=wb[:, c, :],
                                start=(c == 0), stop=(c == NCH - 1)),
                                reads=[(tagp + "wb", nb % 2, c // 4), (tagp + "act", c)], writes=[(tagp + "ps", n % 2)])
                        S.op("act", lambda e, ps=ps, tm=tm: e.copy(out=tm[:], in_=ps[:, :]),
                             reads=[(tagp + "ps", n % 2)], writes=[(tagp + "tmp", n % 2)])
                        S.op("dve", lambda e, tm=tm, tt=tt, nb=nb: e.tensor_tensor(
                            out=xres[:, tt, nb * 512:(nb + 1) * 512], in0=xres[:, tt, nb * 512:(nb + 1) * 512],
                            in1=tm[:], op=ALU.add), reads=[(tagp + "tmp", n % 2), ("xres", tt)], writes=[("xres", tt)])
                        n += 1
                S.barrier()

        add_proj("F", mixT, w_out_d)
        with ExitStack() as gs:
            def sb(name, shape, dt=F32):
                return gs.enter_context(nc.sbuf_tensor("G" + name, shape, dt))

            def pst(name, dt=F32, n=512):
                return gs.enter_context(nc.psum_tensor("Gps" + name, [128, n], dt))

            P = dict(tag="G", xb=sb("xb", [128, D], BF16), ss=sb("ss", [128, 1]),
                     psT=[pst(f"T{i}", BF16, 1024) for i in range(2)])
            gbc = sb("gbc", [128, D])
            mnT = sb("mnT", [128, NCH, 256], BF16); kmT = sb("kmT", [128, NCH, 256], BF16)
            vm = sb("vm", [128, 2, D], BF16)
            hT = moT[:].rearrange("p a b -> p (a b)").rearrange("p (c t) -> p c t", c=NCH)
            qT = aqT[:].rearrange("p a b -> p (a b)").rearrange("p (c t) -> p c t", c=NCH)
            stage_all = sb("stage", [128, 2, 1024])
            stages = [stage_all[:, i, :].rearrange("p (c n) -> p c n", c=4) for i in range(2)]
            wbs = [sb(f"wbk{i}", [128, NCH, 256], BF16) for i in range(2)]
            kc = [0]
            onesb = sb("onesb", [128, 128], BF16)
            PTm = [sb(f"PT{i}", [128, 512], BF16) for i in range(2)]
            rD = sb("rD", [128, 512]); tO = sb("tO", [128, 512])
            psQ = [pst("Q0"), pst("Q1")]; psS = [pst("S0"), pst("S1")]; psO = pst("O"); psD = pst("D")
            S.op("pool", lambda e: e.memset(onesb[:], 1.0), writes=["Gonesb"])
            S.dma("sp", gbc[:], g_mem_d[0:1, :].partition_broadcast(128), writes=["Ggbc"])
            stage_flat = stage_all[:].rearrange("p a b -> p (a b)")
            for mt in range(2):
                S.dma("sp", stage_flat, mem_d[mt * 128:(mt + 1) * 128, :], writes=[("Gstage", 0), ("Gstage", 1)])
                norm_sb(P, stage_flat, [("Gstage", 0), ("Gstage", 1)], gbc, mnT, mt * 128, ("GmnT", mt))
            mnkeys = [("GmnT", 0), ("GmnT", 1)]
            nq_ = 0
            for b2 in range(8):
                wb, wk_ = load_wblk2("G", w_xk_d, b2, stages, wbs, kc)
                for e4 in range(2):
                    ec = b2 * 2 + e4
                    ps = psQ[nq_ % 2]; pk = ("GpsQ", nq_ % 2); nq_ += 1
                    for c in range(NCH):
                        S.op("pe", lambda e, c=c, e4=e4, ps=ps: e.matmul(ps[:, 0:256], lhsT=wb[:, c, e4 * 128:(e4 + 1) * 128],
                                                                         rhs=mnT[:, c, :], start=(c == 0), stop=(c == NCH - 1)),
                             reads=wk_ + mnkeys, writes=[pk])
                    S.op("act", lambda e, ec=ec, ps=ps: e.copy(out=kmT[:, ec, :], in_=ps[:, 0:256]), reads=[pk],
                         writes=[("GkmT", ec)])
            for b2 in range(8):
                wb, wk_ = load_wblk2("G", w_xv_d, b2, stages, wbs, kc)
                for mt in range(2):
                    ps = psQ[nq_ % 2]; pk = ("GpsQ", nq_ % 2); nq_ += 1
                    for c in range(NCH):
                        S.op("pe", lambda e, c=c, mt=mt, ps=ps: e.matmul(ps[:, 0:256], lhsT=mnT[:, c, mt * 128:(mt + 1) * 128],
                                                                         rhs=wb[:, c, :], start=(c == 0), stop=(c == NCH - 1)),
                             reads=wk_ + mnkeys, writes=[pk])
                    S.op("act", lambda e, mt=mt, b2=b2, ps=ps: e.copy(out=vm[:, mt, b2 * 256:(b2 + 1) * 256], in_=ps[:, 0:256]),
                         reads=[pk], writes=[("Gvm", mt, b2)])
            S.dma("sp", gbc[:], g_cross_d[0:1, :].partition_broadcast(128), writes=["Ggbc"])
            SCX = 512.0 ** -0.5
            kmkeys = [("GkmT", ec) for ec in range(NCH)]
            vmkeys = [("Gvm", mt, b2) for mt in range(2) for b2 in range(8)]
            for st in range(2):
                for sub in range(4):
                    tt = st * 4 + sub
                    norm_sb(P, xres[:, tt, :], [("xres", tt)], gbc, hT, sub * 128, "GhT")
                for b2 in range(8):
                    wb, wk_ = load_wblk2("G", w_xq_d, b2, stages, wbs, kc)
                    for e4 in range(2):
                        ec = b2 * 2 + e4
                        ps = psQ[nq_ % 2]; pk = ("GpsQ", nq_ % 2); nq_ += 1
                        for c in range(NCH):
                            S.op("pe", lambda e, c=c, e4=e4, ps=ps: e.matmul(ps[:, :], lhsT=wb[:, c, e4 * 128:(e4 + 1) * 128],
                                                                             rhs=hT[:, c, :], start=(c == 0), stop=(c == NCH - 1)),
                                 reads=wk_ + ["GhT"], writes=[pk])
                        if ec % 2 == 0:
                            S.op("act", lambda e, ec=ec, ps=ps: e.copy(out=qT[:, ec, :], in_=ps[:, :]), reads=[pk],
                                 writes=[("GqT", ec)])
                        else:
                            S.op("dve", lambda e, ec=ec, ps=ps: e.tensor_copy(qT[:, ec, :], ps[:, :]), reads=[pk],
                                 writes=[("GqT", ec)])
                for hd in range(4):
                    ecs = range(hd * 4, hd * 4 + 4)
                    qk = [("GqT", ec) for ec in ecs]
                    for mt in range(2):
                        for i_, ec in enumerate(ecs):
                            S.op("pe", lambda e, mt=mt, ec=ec, i_=i_: e.matmul(
                                psS[mt][:, :], lhsT=kmT[:, ec, mt * 128:(mt + 1) * 128], rhs=qT[:, ec, :],
                                start=(i_ == 0), stop=(i_ == 3)), reads=qk + kmkeys, writes=[("GpsS", mt)])
                        S.op("act", lambda e, mt=mt: e.activation(out=PTm[mt][:], in_=psS[mt][:, :], func=AF.Exp, scale=SCX),
                             reads=[("GpsS", mt)], writes=[("GPT", mt)])
                    ptk = [("GPT", 0), ("GPT", 1)]
                    for mt in range(2):
                        S.op("pe", lambda e, mt=mt: e.matmul(psD[:, :], lhsT=onesb[:], rhs=PTm[mt][:],
                                                             start=(mt == 0), stop=(mt == 1)),
                             reads=ptk + ["Gonesb"], writes=["GpsD"])
                    S.op("act", lambda e: e.copy(out=rD[:], in_=psD[:, :]), reads=["GpsD"], writes=["GrD"])
                    S.op("dve", lambda e: e.reciprocal(rD[:], rD[:]), reads=["GrD"], writes=["GrD"])
                    for ec in ecs:
                        for mt in range(2):
                            S.op("pe", lambda e, mt=mt, ec=ec: e.matmul(psO[:, :], lhsT=vm[:, mt, ec * 128:(ec + 1) * 128],
                                                                        rhs=PTm[mt][:], start=(mt == 0), stop=(mt == 1)),
                                 reads=ptk + vmkeys, writes=["GpsO"])
                        S.op("act", lambda e: e.copy(out=tO[:], in_=psO[:, :]), reads=["GpsO"], writes=["GtO"])
                        S.op("dve", lambda e, ec=ec: e.tensor_tensor(out=mixT[:, ec, st * 512:(st + 1) * 512], in0=tO[:],
                                                                     in1=rD[:], op=ALU.mult),
                             reads=["GtO", "GrD"], writes=[("Gact", ec, st)])
            S.barrier()
        add_proj("G", mixT, w_xo_d)

        if dbg is not None and dbg["name"] == "xresG":
            S.dma("sp", dbg_out.rearrange("(t p) n -> p t n", p=128), xres[:], writes=["dbgo"])
            S.barrier()
            return nc
        ids_all = es.enter_context(nc.sbuf_tensor("ids_all", [128, 8, 128], I32))
        gates_all = es.enter_context(nc.sbuf_tensor("gates_all", [128, 8, 128], F32))
        with ExitStack() as hs:
            def sb(name, shape, dt=F32):
                return hs.enter_context(nc.sbuf_tensor("H" + name, shape, dt))

            def pst(name, dt=F32, n=512):
                return hs.enter_context(nc.psum_tensor("Hps" + name, [128, n], dt))

            P = dict(tag="H", xb=sb("xb", [128, D], BF16), ss=sb("ss", [128, 1]),
                     psT=[pst(f"T{i}", BF16, 1024) for i in range(2)])
            gbc = sb("gbc", [128, D])
            S.dma("sp", gbc[:], g_ffn_d[0:1, :].partition_broadcast(128), writes=["Hgbc"])
            hT = moT[:].rearrange("p a b -> p (a b)").rearrange("p (c t) -> p c t", c=NCH)
            qT = aqT[:].rearrange("p a b -> p (a b)").rearrange("p (c t) -> p c t", c=NCH)
            stage_all = sb("stage", [128, 2, 1024])
            stages = [stage_all[:, i, :].rearrange("p (c n) -> p c n", c=4) for i in range(2)]
            wbs = [sb(f"wbk{i}", [128, NCH, 256], BF16) for i in range(2)]
            kc = [0]
            skT = sb("skT", [128, 16, 128], BF16)
            stage4 = stage_all[:].rearrange("p a b -> p (a b)").rearrange("p (a b) -> p a b", a=4)
            S.dma("sp", stage4, skT_d[:, :, :].rearrange("p (a b) k -> p a (b k)", a=4),
                  writes=[("Hstage", 0), ("Hstage", 1)])
            S.op("dve", lambda e: e.tensor_copy(skT[:].rearrange("p (a b) k -> p a (b k)", a=4), stage4),
                 reads=[("Hstage", 0), ("Hstage", 1)], writes=["HskT"])
            sc = sb("sc", [128, 16, 128]); wk1 = sb("wk1", [128, 128])
            ts = sb("ts", [128, 16, 16]); ti = sb("ti", [128, 16, 16], U32); tif = sb("tif", [128, 16, 16])
            cand = sb("cand", [128, 16, 16]); cid = sb("cid", [128, 16, 16]); wk2 = sb("wk2", [128, 256])
            junk = sb("junk", [128, 256]); bs = sb("bs", [128, 16]); negm = sb("negm", [128, 1])
            ge = sb("ge", [128, 16]); zz = sb("zz", [128, 1]); idf = sb("idf", [128, 128])
            pos = sb("pos", [128, 16], U32); posf = sb("posf", [128, 16]); iota_t = sb("iota", [128, 256])
            S.dma("sp", iota_t[:], iota_d[0:1, :].partition_broadcast(128), writes=["Hiota"])
            psQ = [pst("Q0"), pst("Q1")]; psC = [pst("C0"), pst("C1")]
            nq_ = 0
            for st in range(2):
                for sub in range(4):
                    tt = st * 4 + sub
                    norm_sb(P, xres[:, tt, :], [("xres", tt)], gbc, hT, sub * 128, "HhT")
                for b2 in range(8):
                    wb, wk_ = load_wblk2("H", w_pq_d, b2, stages, wbs, kc)
                    for e4 in range(2):
                        j = b2 * 2 + e4
                        ps = psQ[nq_ % 2]; pk = ("HpsQ", nq_ % 2); nq_ += 1
                        for c in range(NCH):
                            S.op("pe", lambda e, c=c, e4=e4, ps=ps: e.matmul(ps[:, :], lhsT=wb[:, c, e4 * 128:(e4 + 1) * 128],
                                                                             rhs=hT[:, c, :], start=(c == 0), stop=(c == NCH - 1)),
                                 reads=wk_ + ["HhT"], writes=[pk])
                        if j % 2 == 0:
                            S.op("act", lambda e, j=j, ps=ps: e.copy(out=qT[:, j, :], in_=ps[:, :]), reads=[pk],
                                 writes=[("HqT", j)])
                        else:
                            S.op("dve", lambda e, j=j, ps=ps: e.tensor_copy(qT[:, j, :], ps[:, :]), reads=[pk],
                                 writes=[("HqT", j)])
                for sub in range(4):
                    tt = st * 4 + sub
                    for jb in range(4):
                        pc = psC[jb % 2]; pck = ("HpsC", jb % 2)
                        for jj in range(4):
                            j = jb * 4 + jj
                            S.op("pe", lambda e, j=j, jj=jj, pc=pc: e.matmul(
                                pc[:, jj * 128:(jj + 1) * 128], lhsT=qT[:, j, sub * 128:(sub + 1) * 128], rhs=skT[:, j, :],
                                start=True, stop=True), reads=[("HqT", j), "HskT"], writes=[pck])
                        S.op("act", lambda e, jb=jb, pc=pc: e.copy(
                            out=sc[:, jb * 4:(jb + 1) * 4, :], in_=pc[:, :].rearrange("p (a k) -> p a k", a=4)),
                            reads=[pck], writes=[("Hsc", jb)])
                    for j in range(16):
                        sk_ = ("Hsc", j // 4)
                        S.op("dve", lambda e, j=j: e.max(out=ts[:, j, 0:8], in_=sc[:, j, :]), reads=[sk_], writes=[("Hts", j, 0)])
                        S.op("dve", lambda e, j=j: e.match_replace(out=wk1[:], in_to_replace=ts[:, j, 0:8],
                                                                   in_values=sc[:, j, :], imm_value=-1e30),
                             reads=[sk_, ("Hts", j, 0)], writes=["Hwk1"])
                        S.op("dve", lambda e, j=j: e.max(out=ts[:, j, 8:16], in_=wk1[:]), reads=["Hwk1"], writes=[("Hts", j, 1)])
                        S.op("dve", lambda e, j=j: e.max_index(out=ti[:, j, 0:8], in_max=ts[:, j, 0:8], in_values=sc[:, j, :]),
                             reads=[sk_, ("Hts", j, 0)], writes=[("Hti", j, 0)])
                        S.op("dve", lambda e, j=j: e.max_index(out=ti[:, j, 8:16], in_max=ts[:, j, 8:16], in_values=wk1[:]),
                             reads=["Hwk1", ("Hts", j, 1)], writes=[("Hti", j, 1)])
                    S.op("dve", lambda e: e.tensor_copy(tif[:], ti[:]),
                         reads=[("Hti", j, q) for j in range(16) for q in range(2)], writes=["Htif"])
                    for h in range(8):
                        j0, j1 = 2 * h, 2 * h + 1
                        tsk = [("Hts", j0, 0), ("Hts", j0, 1), ("Hts", j1, 0), ("Hts", j1, 1)]
                        S.op("dve", lambda e: e.tensor_tensor(
                            out=cand[:], in0=ts[:, j0, :].unsqueeze(2).to_broadcast([128, 16, 16]),
                            in1=ts[:, j1, :].unsqueeze(1).to_broadcast([128, 16, 16]), op=ALU.add),
                            reads=tsk, writes=["Hcand"])
                        S.op("dve", lambda e: e.scalar_tensor_tensor(
                            out=cid[:], in0=tif[:, j0, :].unsqueeze(2).to_broadcast([128, 16, 16]), scalar=128.0,
                            in1=tif[:, j1, :].unsqueeze(1).to_broadcast([128, 16, 16]), op0=ALU.mult, op1=ALU.add),
                            reads=["Htif"], writes=["Hcid"])
                        candf = cand[:].rearrange("p a b -> p (a b)")
                        cidf = cid[:].rearrange("p a b -> p (a b)")
                        S.op("dve", lambda e: e.max(out=bs[:, 0:8], in_=candf), reads=["Hcand"], writes=["Hbs0"])
                        S.op("dve", lambda e: e.match_replace(out=wk2[:], in_to_replace=bs[:, 0:8], in_values=candf,
                                                              imm_value=-1e30), reads=["Hcand", "Hbs0"], writes=["Hwk2"])
                        S.op("dve", lambda e: e.max(out=bs[:, 8:16], in_=wk2[:]), reads=["Hwk2"], writes=["Hbs1"])
                        S.op("dve", lambda e: e.max_index(out=pos[:, 0:8], in_max=bs[:, 0:8], in_values=candf),
                             reads=["Hcand", "Hbs0"], writes=["Hpos0"])
                        S.op("dve", lambda e: e.max_index(out=pos[:, 8:16], in_max=bs[:, 8:16], in_values=wk2[:]),
                             reads=["Hwk2", "Hbs1"], writes=["Hpos1"])
                        S.op("dve", lambda e: e.tensor_copy(posf[:], pos[:]), reads=["Hpos0", "Hpos1"], writes=["Hposf"])
                        for k in range(16):
                            S.op("dve", lambda e, k=k: e.scalar_tensor_tensor(
                                out=junk[:], in0=iota_t[:], scalar=posf[:, k:k + 1], in1=cidf, op0=ALU.is_equal, op1=ALU.mult,
                                accum_out=idf[:, h * 16 + k:h * 16 + k + 1]),
                                reads=["Hiota", "Hcid", "Hposf"], writes=["Hjunk", ("Hidf", h)])
                        S.op("dve", lambda e: e.tensor_scalar_mul(negm[:], bs[:, 0:1], -1.0), reads=["Hbs0"], writes=["Hnegm"])
                        S.op("act", lambda e: e.activation(out=ge[:], in_=bs[:], func=AF.Exp, bias=negm[:, 0:1], accum_out=zz[:]),
                             reads=["Hbs0", "Hbs1", "Hnegm"], writes=["Hge", "Hzz"])
                        S.op("dve", lambda e: e.reciprocal(zz[:], zz[:]), reads=["Hzz"], writes=["Hzz"])
                        S.op("dve", lambda e: e.tensor_scalar(out=gates_all[:, tt, h * 16:(h + 1) * 16], in0=ge[:],
                                                              scalar1=zz[:, 0:1], scalar2=None, op0=ALU.mult),
                             reads=["Hge", "Hzz"], writes=[("gates", tt, h)])
                    S.op("dve", lambda e: e.tensor_copy(ids_all[:, tt, :], idf[:]),
                         reads=[("Hidf", h) for h in range(8)], writes=[("ids", tt)])
            S.barrier()

        if dbg is not None and dbg["name"] == "peer_ids":
            S.dma("sp", dbg_out[0:128, :].rearrange("p (t k) -> p t k", t=8), ids_all[:].bitcast(F32), writes=["dbgo"])
            S.dma("sp", dbg_out[128:256, :].rearrange("p (t k) -> p t k", t=8), gates_all[:], writes=["dbgo2"])
            S.barrier()
            return nc

        with ExitStack() as hs:
            def sb(name, shape, dt=F32):
                return hs.enter_context(nc.sbuf_tensor("Hb" + name, shape, dt))

            P = dict(tag="Hb", xb=sb("xb", [128, D], BF16), ss=sb("ss", [128, 1]), psT=None)
            gbc = sb("gbc", [128, D])
            S.dma("sp", gbc[:], g_ffn_d[0:1, :].partition_broadcast(128), writes=["Hbgbc"])
            xn2 = [sb(f"xn{i}", [128, D]) for i in range(2)]
            actc = sb("actc", [128, 128]); cf = sb("cf", [128, 128]); tmpv = sb("tmpv", [128, D])
            dg = [sb(f"dg{i}", [128, 128], BF16) for i in range(4)]
            NG = 8
            fence_t = sb("fence", [128, 1])
            gall = mixT[:].rearrange("p a b -> p (a b)")
            gb_ = [gall[:, i * 2 * D:(i + 1) * 2 * D] for i in range(4)]
            gb_ += [moT[:].rearrange("p a b -> p (a b)")[:, i * 2 * D:(i + 1) * 2 * D] for i in range(2)]
            gb_ += [aqT[:].rearrange("p a b -> p (a b)")[:, i * 2 * D:(i + 1) * 2 * D] for i in range(2)]
            psV = [[hs.enter_context(nc.psum_tensor(f"HbpsV{q}_{n}", [128, 512], F32)) for n in range(4)] for q in range(2)]
            ng = 0
            for tt in range(8):
                xn = xn2[tt % 2]
                P["tag"] = f"Hb{tt % 2}"
                S.op("act", lambda e, tt=tt: e.activation(out=P["xb"][:], in_=xres[:, tt, :], func=AF.Square,
                                                          scale=float(D) ** -0.5, accum_out=P["ss"][:]),
                     reads=[("xres", tt)], writes=["Hbxb", "Hbss"])
                S.op("dve", lambda e: e.tensor_scalar_add(P["ss"][:], P["ss"][:], EPS), reads=["Hbss"], writes=["Hbss"])
                S.op("act", lambda e: e.sqrt(P["ss"][:], P["ss"][:]), reads=["Hbss"], writes=["Hbss"])
                S.op("dve", lambda e: e.reciprocal(P["ss"][:], P["ss"][:]), reads=["Hbss"], writes=["Hbss"])
                S.op("dve", lambda e, tt=tt, xn=xn: e.scalar_tensor_tensor(out=xn[:], in0=xres[:, tt, :], scalar=P["ss"][:, 0:1],
                                                                    in1=gbc[:], op0=ALU.mult, op1=ALU.mult),
                     reads=[("xres", tt), "Hbss", "Hbgbc"], writes=[("Hbxn", tt % 2)])
                pv = psV[tt % 2]

                def chain(slot, bq):
                    b_, q_ = bq
                    S.op("act", lambda e: e.activation(out=cf[:, slot:slot + 1], in_=actc[:, slot:slot + 1], func=AF.Gelu),
                         reads=[("Hbact", slot), "Hbfence"], writes=[("Hbcf", slot)])
                    S.op("act", lambda e: e.mul(cf[:, slot:slot + 1], cf[:, slot:slot + 1], gates_all[:, tt, slot:slot + 1]),
                         reads=[("Hbcf", slot)], writes=[("Hbcf", slot)])
                    S.op("act", lambda e: e.activation(out=dg[q_][:], in_=ident[:], func=AF.Copy, scale=cf[:, slot:slot + 1]),
                         reads=[("Hbcf", slot)], writes=[("Hbdg", q_)])
                    for n in range(4):
                        S.op("pe", lambda e, n=n: e.matmul(
                            pv[n][:, :], lhsT=dg[q_][:], rhs=gb_[b_][:, D + n * 512:D + (n + 1) * 512],
                            start=(slot == 0), stop=(slot == 127)),
                            reads=[("Hbdg", q_), ("gbuf", b_)], writes=[("HbpsV", tt % 2, n)])

                prev = None
                for slot in range(128):
                    b_ = ng % NG
                    q_ = ng % 4
                    ng += 1
                    S.dma("pool", None, None, reads=[("ids", tt)], writes=[("gbuf", b_)],
                          fn=lambda e, b_=b_, slot=slot, tt=tt: e.indirect_dma_start(
                              out=gb_[b_], out_offset=None, in_=uv_d[:, :],
                              in_offset=bass.IndirectOffsetOnAxis(ap=ids_all[:, tt, slot:slot + 1], axis=0)))
                    S.op("dve", lambda e, b_=b_, slot=slot, xn=xn: e.scalar_tensor_tensor(
                        out=P["xb"][:], in0=gb_[b_][:, 0:D], scalar=1.0, in1=xn[:], op0=ALU.mult, op1=ALU.mult,
                        accum_out=actc[:, slot:slot + 1]),
                        reads=[("gbuf", b_), ("Hbxn", tt % 2)], writes=["Hbxb", ("Hbact", slot), "Hbfence"])
                    if prev is not None:
                        chain(slot - 1, prev)
                    prev = (b_, q_)
                S.op("dve", lambda e: e.tensor_copy(fence_t[:], actc[:, 0:1]), reads=[("Hbact", 0)], writes=["Hbfence"])
                chain(127, prev)
                if dbg is not None and dbg["name"] == "peer_cf":
                    S.dma("sp", dbg_out[:, tt * 128:(tt + 1) * 128], cf[:], reads=[("Hbcf", s_) for s_ in range(128)], writes=[("dbgo", tt)])
                    S.dma("sp", dbg_out[:, 1024 + tt * 128:1024 + (tt + 1) * 128], actc[:], reads=[("Hbact", s_) for s_ in range(128)], writes=[("dbgo2", tt)])
                S.op("act", lambda e, pv=pv: e.copy(out=tmpv[:, 0:512], in_=pv[0][:, :]),
                     reads=[("HbpsV", tt % 2, 0)], writes=[("Hbtmp", 0)])
                S.op("act", lambda e, pv=pv: e.copy(out=tmpv[:, 512:1024], in_=pv[1][:, :]),
                     reads=[("HbpsV", tt % 2, 1)], writes=[("Hbtmp", 1)])
                S.op("act", lambda e, pv=pv: e.copy(out=tmpv[:, 1024:1536], in_=pv[2][:, :]),
                     reads=[("HbpsV", tt % 2, 2)], writes=[("Hbtmp", 2)])
                S.op("act", lambda e, pv=pv: e.copy(out=tmpv[:, 1536:2048], in_=pv[3][:, :]),
                     reads=[("HbpsV", tt % 2, 3)], writes=[("Hbtmp", 3)])
                S.op("dve", lambda e, tt=tt: e.tensor_tensor(out=xres[:, tt, :], in0=xres[:, tt, :], in1=tmpv[:], op=ALU.add),
                     reads=[("Hbtmp", n) for n in range(4)] + [("xres", tt)], writes=[("xres", tt)])
            S.barrier()

        with ExitStack() as fs:
            gfb = fs.enter_context(nc.sbuf_tensor("gfb", [128, D], F32))
            junk = fs.enter_context(nc.sbuf_tensor("Ijunk", [128, D], BF16))
            ss = fs.enter_context(nc.sbuf_tensor("Iss", [128, 1], F32))
            ot = [fs.enter_context(nc.sbuf_tensor(f"Iot{i}", [128, D], F32)) for i in range(2)]
            S.dma("sp", gfb[:], g_final_d[0:1, :].partition_broadcast(128), writes=["gfb"])
            for tt in range(8):
                o_ = ot[tt % 2]
                S.op("act", lambda e, tt=tt: e.activation(out=junk[:], in_=xres[:, tt, :], func=AF.Square,
                                                          scale=float(D) ** -0.5, accum_out=ss[:]),
                     reads=[("xres", tt)], writes=["Ijunk", "Iss"])
                S.op("dve", lambda e: e.tensor_scalar_add(ss[:], ss[:], EPS), reads=["Iss"], writes=["Iss"])
                S.op("act", lambda e: e.sqrt(ss[:], ss[:]), reads=["Iss"], writes=["Iss"])
                S.op("dve", lambda e: e.reciprocal(ss[:], ss[:]), reads=["Iss"], writes=["Iss"])
                S.op("dve", lambda e, tt=tt, o_=o_: e.scalar_tensor_tensor(out=o_[:], in0=xres[:, tt, :], scalar=ss[:, 0:1],
                                                                    in1=gfb[:], op0=ALU.mult, op1=ALU.mult),
                     reads=[("xres", tt), "Iss", "gfb"], writes=[("Iot", tt % 2)])
                S.dma("sp", out_d[tt * 128:(tt + 1) * 128, :], o_[:], reads=[("Iot", tt % 2)], writes=[("outd", tt)])
            S.barrier()

        S.barrier()
        print("instructions", S.nins, "waits", S.nwaits, S.cnt, S.epoch, S.nsem)
    return nc


def rope_tables(j):
    half = 64
    inv = (10000.0 ** (-np.arange(half, dtype=np.float32) / half)).astype(np.float32)
    pos = (1024 * j - NPRE + np.arange(WIN)).astype(np.float32)
    ang = pos[None, :] * inv[:, None]
    cos = np.cos(ang).astype(np.float32)
    sin = np.sin(ang).astype(np.float32)
    return np.concatenate([cos, cos], 0), np.concatenate([sin, sin], 0)


def make_in_maps(inputs):
    x = np.asarray(inputs["x"], np.float32)
    ident = np.eye(128, dtype=np.float32).astype(ml_dtypes.bfloat16)
    R = np.zeros((128, 128), np.float32)
    for p in range(64):
        R[p, p + 64] = -1.0
        R[p + 64, p] = 1.0
    rotT = np.ascontiguousarray(R.T).astype(ml_dtypes.bfloat16)
    tri = np.triu(np.ones((128, 128), np.float32))
    trl = np.tril(np.ones((128, 128), np.float32))
    f32 = lambda a: np.asarray(a, np.float32)
    skT_h = np.ascontiguousarray(f32(inputs["sub_keys"])[0].reshape(16, 128, 128).transpose(2, 0, 1))
    u_tab = f32(inputs["u_tab"])[0]
    v_tab = f32(inputs["v_tab"])[0]
    maps = []
    for c in range(8):
        b, j = c // 4, c % 4
        xw = np.zeros((WIN, D), np.float32)
        lo = 1024 * j - NPRE
        src0 = max(lo, 0)
        xw[src0 - lo:] = x[b, src0:1024 * j + 1024]
        cos, sin = rope_tables(j)
        pmv = np.where(np.arange(WIN) + lo >= 0, 0.0, -30000.0).astype(np.float32)
        kbA = np.zeros((128, 48), np.float32)
        gi = 0
        for Q in range(2):
            for (d, r, sub) in [(1, 0, s_) for s_ in range(4)] + [(4, r_, 0) for r_ in range(4)] + [(16, r_, 0) for r_ in range(16)]:
                u0 = 2048 + 512 * Q + r + 128 * sub
                u = u0 - 128 * d + d * np.arange(128)
                kbA[:, gi] = np.where(1024 * j - 2048 + u >= 0, 0.0, -30000.0)
                gi += 1
        m = {
            "x_win": xw,
            "w_in": np.ascontiguousarray(np.asarray(inputs["w_in"], np.float32)[0]),
            "g_mix": np.asarray(inputs["g_mix"], np.float32).reshape(1, D),
            "cosT": cos, "sinT": sin, "ident": ident, "rotT": rotT,
            "cw_h": np.ascontiguousarray(f32(inputs["conv_w"])[0].T.reshape(8, 128, 4).transpose(1, 0, 2)),
            "cb_h": np.ascontiguousarray(f32(inputs["conv_b"])[0].reshape(8, 128).T),
            "gm_h": np.ascontiguousarray(f32(inputs["g_mhead"])[0].reshape(8, 128).T),
            "gb_h": np.concatenate([f32(inputs["b_mi"])[0], f32(inputs["b_mf"])[0]]).reshape(1, 8),
            "pm_h": np.ascontiguousarray(pmv.reshape(32, 128).T),
            "tri_f": tri,
            "w_mq": f32(inputs["w_mq"])[0], "w_mk": f32(inputs["w_mk"])[0],
            "w_out": f32(inputs["w_out"])[0], "g_final": f32(inputs["g_final"]).reshape(1, D),
            "mem_b": np.ascontiguousarray(f32(inputs["mem"])[b]),
            "g_mem": f32(inputs["g_mem"]).reshape(1, D), "g_cross": f32(inputs["g_cross"]).reshape(1, D),
            "w_xq": f32(inputs["w_xq"])[0], "w_xk": f32(inputs["w_xk"])[0], "w_xv": f32(inputs["w_xv"])[0],
            "w_xo": f32(inputs["w_xo"])[0],
            "g_ffn": f32(inputs["g_ffn"]).reshape(1, D), "w_pq": f32(inputs["w_pq"])[0],
            "skT_h": skT_h, "iota256": np.arange(256, dtype=np.float32).reshape(1, 256), "u_tab": u_tab, "v_tab": v_tab,
            "kbA": kbA, "ga_h": np.ascontiguousarray(f32(inputs["g_ahead"])[0].T), "trl_f": trl,
        }
        maps.append(m)
    return maps


def kernel(**inputs):
    nc = build()
    maps = make_in_maps(inputs)
    res = run_bass_kernel_spmd(nc, maps, core_ids=list(range(8)))
    out = np.zeros((2, 4096, D), np.float32)
    for c in range(8):
        b, j = c // 4, c % 4
        out[b, 1024 * j:1024 * j + 1024] = res.results[c]["out"]
    return out
```

```python
import numpy as np
import ml_dtypes
from contextlib import ExitStack
import concourse.bass as bass
import concourse.mybir as mybir
from concourse.bass_utils import run_bass_kernel_spmd

F32 = mybir.dt.float32
BF16 = mybir.dt.bfloat16
I32 = mybir.dt.int32
U32 = mybir.dt.uint32
ALU = mybir.AluOpType
AF = mybir.ActivationFunctionType
AX = mybir.AxisListType

D = 2048
NCH = 16
WIN = 4096
OWN = 1024
NPRE = WIN - OWN
EPS = 1e-6
IN_COLS = 6152
NDS = 40


class Sched:
    LIM = 3500
    DLIM = 240

    def __init__(self, nc, es):
        self.nc = nc
        self.es = es
        self.engs = {"pe": nc.tensor, "act": nc.scalar, "dve": nc.vector, "pool": nc.gpsimd, "sp": nc.sync}
        self.nsem = 0
        self.h = {}
        self.epoch = {k: 0 for k in self.engs}
        self.cnt = {k: 0 for k in self.engs}
        for k in self.engs:
            self.h[(k, 0)] = self._new()
        self.seen = {k: {} for k in self.engs}
        self.dver = [0] * NDS
        self.dcnt = [0] * NDS
        for i in range(NDS):
            self.h[("d", i, 0)] = self._new()
        self.dnext = {"sp": 0, "pool": NDS // 2, "act": 0}
        self.lastw = {}
        self.rd = {}
        self.nwaits = 0
        self.nins = 0

    def _new(self):
        self.nsem += 1
        return self.es.enter_context(self.nc.semaphore(f"s{self.nsem}"))

    def _wait(self, eng, sk, val):
        if val <= 0:
            return
        if self.seen[eng].get(sk, 0) >= val:
            return
        self.seen[eng][sk] = val
        self.engs[eng].wait_ge(self.h[sk], val)
        self.nwaits += 1

    def _deps(self, eng, reads, writes):
        need = {}

        def add(t, war=False):
            if t is None:
                return
            sk, val = t
            if sk[0] == eng and eng == "pe":
                return
            if need.get(sk, 0) < val:
                need[sk] = val

        for k in reads:
            add(self.lastw.get(k))
        for k in writes:
            add(self.lastw.get(k))
            for t in self.rd.get(k, ()):
                add(t, war=True)
        for sk, val in need.items():
            self._wait(eng, sk, val)

    def _commit(self, ticket, reads, writes):
        for k in reads:
            self.rd.setdefault(k, []).append(ticket)
        for k in writes:
            self.lastw[k] = ticket
            self.rd[k] = []

    def op(self, eng, fn, reads=(), writes=()):
        self._deps(eng, reads, writes)
        if self.cnt[eng] >= self.LIM:
            self.epoch[eng] += 1
            self.cnt[eng] = 0
            self.h[(eng, self.epoch[eng])] = self._new()
        sk = (eng, self.epoch[eng])
        ins = fn(self.engs[eng])
        ins.then_inc(self.h[sk], 1)
        self.cnt[eng] += 1
        self.nins += 1
        self._commit((sk, self.cnt[eng]), reads, writes)

    def dma(self, q, out, in_, reads=(), writes=(), fn=None):
        i = self.dnext[q]
        half = NDS // 2
        base = half if q == "pool" else 0
        self.dnext[q] = base + (i - base + 1) % half
        sk = ("d", i, self.dver[i])
        self._wait(q, sk, 16 * self.dcnt[i])
        if self.dcnt[i] >= self.DLIM:
            self.dver[i] += 1
            self.dcnt[i] = 0
            sk = ("d", i, self.dver[i])
            self.h[sk] = self._new()
        self._deps(q, reads, writes)
        if fn is None:
            ins = self.engs[q].dma_start(out=out, in_=in_)
        else:
            ins = fn(self.engs[q])
        ins.then_inc(self.h[sk], 16)
        self.dcnt[i] += 1
        self.nins += 1
        self._commit((sk, 16 * self.dcnt[i]), reads, writes)

    def barrier(self):
        for e in self.engs:
            for e2 in self.engs:
                if e2 != e:
                    if self.cnt[e2] > 0:
                        self._wait(e, (e2, self.epoch[e2]), self.cnt[e2])
                    elif self.epoch[e2] > 0:
                        self._wait(e, (e2, self.epoch[e2] - 1), self.LIM)
            for i in range(NDS):
                if self.dcnt[i] > 0:
                    self._wait(e, ("d", i, self.dver[i]), 16 * self.dcnt[i])
                elif self.dver[i] > 0:
                    self._wait(e, ("d", i, self.dver[i] - 1), 16 * self.DLIM)
        self.lastw = {}
        self.rd = {}


def bcast_free(ap_col, n):
    return ap_col.to_broadcast([ap_col.shape[0], n])


class K:
    pass


def load_weight_bf16(S, nc, es, w_dram, col0, ncols, name, stage, stage_key):
    wt = es.enter_context(nc.sbuf_tensor(name, [128, NCH, ncols], BF16))
    wv = w_dram.rearrange("(c p) n -> p c n", p=128)
    step = max(1, 2048 // ncols)
    c = 0
    i = 0
    while c < NCH:
        nn = min(step, NCH - c)
        sl = i % 2
        st = stage[sl]
        sv = st[:, 0:nn * ncols].rearrange("p (c n) -> p c n", n=ncols)
        S.dma("sp", sv, wv[:, c:c + nn, col0:col0 + ncols], writes=[(stage_key, sl)])
        eng = "pool" if i % 2 == 0 else "dve"
        S.op(eng, lambda e, c=c, nn=nn, sv=sv: e.tensor_copy(wt[:, c:c + nn, :], sv),
             reads=[(stage_key, sl)], writes=[(name, c + q) for q in range(nn)])
        c += nn
        i += 1
    return wt


def build(dbg=None):
    nc = bass.Bass("TRN2", target_bir_lowering=False)
    try:
        nc.allow_low_precision("bf16 matmuls with fp32 accumulation")
    except Exception:
        pass

    def din(name, shape, dt=F32):
        return nc.dram_tensor(name, list(shape), dt, kind="ExternalInput").ap()

    def dint(name, shape, dt=BF16):
        return nc.dram_tensor(name, list(shape), dt, kind="Internal").ap()

    x_win = din("x_win", [WIN, D])
    w_in = din("w_in", [D, IN_COLS])
    g_mix = din("g_mix", [1, D])
    cosT = din("cosT", [128, WIN])
    sinT = din("sinT", [128, WIN])
    ident_d = din("ident", [128, 128], BF16)
    rot_d = din("rotT", [128, 128], BF16)

    cw_d = din("cw_h", [128, 8, 4])
    cb_d = din("cb_h", [128, 8])
    gm_d = din("gm_h", [128, 8])
    gb_d = din("gb_h", [1, 8])
    pm_d = din("pm_h", [128, 32])
    tri_d = din("tri_f", [128, 128])
    w_mq_d = din("w_mq", [4, 256, 256])
    w_mk_d = din("w_mk", [4, 256, 256])

    kbA_d = din("kbA", [128, 48])
    ga_d = din("ga_h", [128, 8])
    trl_d = din("trl_f", [128, 128])
    w_out_d = din("w_out", [D, D])
    mem_d = din("mem_b", [256, D])
    g_ffn_d = din("g_ffn", [1, D])
    iota_d = din("iota256", [1, 256])
    w_pq_d = din("w_pq", [D, D])
    skT_d = din("skT_h", [128, 16, 128])
    u_tab_d = din("u_tab", [16384, D])
    v_tab_d = din("v_tab", [16384, D])
    g_mem_d = din("g_mem", [1, D])
    g_cross_d = din("g_cross", [1, D])
    w_xq_d = din("w_xq", [D, D]); w_xk_d = din("w_xk", [D, D]); w_xv_d = din("w_xv", [D, D]); w_xo_d = din("w_xo", [D, D])
    g_final_d = din("g_final", [1, D])
    out_d = nc.dram_tensor("out", [OWN, D], F32, kind="ExternalOutput").ap()
    dbg_out = None
    if dbg is not None:
        dbg_out = nc.dram_tensor("dbg", list(dbg["shape"]), dbg.get("dt", F32), kind="ExternalOutput").ap()

    uv_d = dint("uv_tab", [16384, 2 * D])
    s_minT = dint("s_minT", [1024, WIN])
    s_mv = dint("s_mv", [WIN, 1024])
    s_gates = dint("s_gates", [WIN, 8], F32)
    s_akT = dint("s_akT", [1024, WIN])
    s_av = dint("s_av", [WIN, 1024])

    with ExitStack() as es:
        es.enter_context(nc.allow_low_precision(reason="bf16 operands, fp32 accumulation"))
        S = Sched(nc, es)
        ident = es.enter_context(nc.sbuf_tensor("identb", [128, 128], BF16))
        rotT = es.enter_context(nc.sbuf_tensor("rotTb", [128, 128], BF16))
        S.dma("sp", ident[:], ident_d[:, :], writes=["ident"])
        S.dma("sp", rotT[:], rot_d[:, :], writes=["rotT"])
        es_mix = es
        es_mix2 = es
        moT = es_mix.enter_context(nc.sbuf_tensor("moT", [128, 8, OWN], BF16))
        aqT = es_mix.enter_context(nc.sbuf_tensor("aqT", [128, 8, OWN], BF16))


        def norm_tile(pes, x_src_ap, gbc, hT, col0, xkey, part="both"):
            xt, xb, ss, rstd, psT = pes["xt"], pes["xb"], pes["ss"], pes["rstd"], pes["psT"]
            if part in ("both", "pre"):
                norm_tile_pre(pes, x_src_ap, gbc)
            if part in ("both", "post"):
                norm_tile_post(pes, hT, col0, xkey)

        def norm_tile_pre(pes, x_src_ap, gbc):
            xt, xb, ss, rstd, psT = pes["xt"], pes["xb"], pes["ss"], pes["rstd"], pes["psT"]
            S.dma("sp", xt[:], x_src_ap, writes=["xt"])
            S.op("act", lambda e: e.activation(out=xb[:], in_=xt[:], func=AF.Square, scale=float(D) ** -0.5,
                                               accum_out=ss[:]),
                 reads=["xt"], writes=["xb", "ss"])
            S.op("dve", lambda e: e.tensor_scalar_add(rstd[:], ss[:], EPS), reads=["ss"], writes=["rstd"])
            S.op("act", lambda e: e.sqrt(rstd[:], rstd[:]), reads=["rstd"], writes=["rstd"])
            S.op("dve", lambda e: e.reciprocal(rstd[:], rstd[:]), reads=["rstd"], writes=["rstd"])
            S.op("dve", lambda e: e.scalar_tensor_tensor(out=xb[:], in0=xt[:], scalar=rstd[:, 0:1], in1=gbc[:],
                                                         op0=ALU.mult, op1=ALU.mult),
                 reads=["xt", "rstd", "gbc"], writes=["xb"])

        def norm_tile_post(pes, hT, col0, xkey):
            xt, xb, ss, rstd, psT = pes["xt"], pes["xb"], pes["ss"], pes["rstd"], pes["psT"]
            for half in range(2):
                for j in range(8):
                    c = half * 8 + j
                    S.op("pe", lambda e, c=c, j=j, half=half: e.transpose(
                        psT[half][:, j * 128:(j + 1) * 128], xb[:, c * 128:(c + 1) * 128], ident[:]),
                        reads=["xb", "ident"], writes=[("psT", half)])
                eng = "act" if half == 0 else "dve"
                if eng == "act":
                    S.op("act", lambda e, half=half: e.copy(
                        out=hT[:, half * 8:(half + 1) * 8, col0:col0 + 128],
                        in_=psT[half][:].rearrange("p (c t) -> p c t", c=8)),
                        reads=[("psT", half)], writes=[xkey])
                else:
                    S.op("dve", lambda e, half=half: e.tensor_copy(
                        hT[:, half * 8:(half + 1) * 8, col0:col0 + 128],
                        psT[half][:].rearrange("p (c t) -> p c t", c=8)),
                        reads=[("psT", half)], writes=[xkey])

        def phase_proj(name, st_list, specs, gain_d):
            with ExitStack() as pes_:
                pes = {}
                pes["xt"] = pes_.enter_context(nc.sbuf_tensor(name + "xt", [128, D], F32))
                pes["xb"] = pes_.enter_context(nc.sbuf_tensor(name + "xb", [128, D], BF16))
                pes["ss"] = pes_.enter_context(nc.sbuf_tensor(name + "ss", [128, 1], F32))
                pes["rstd"] = pes_.enter_context(nc.sbuf_tensor(name + "rstd", [128, 1], F32))
                pes["psT"] = [pes_.enter_context(nc.psum_tensor(name + f"psT{i}", [128, 1024], BF16)) for i in range(2)]
                gbc = pes_.enter_context(nc.sbuf_tensor(name + "gbc", [128, D], F32))
                S.dma("sp", gbc[:], gain_d.partition_broadcast(128), writes=["gbc"])
                hT = [pes_.enter_context(nc.sbuf_tensor(name + f"hT{i}", [128, NCH, 512], BF16)) for i in range(2)]
                stage = [pes_.enter_context(nc.sbuf_tensor(name + f"wst{i}", [128, 2048], F32)) for i in range(2)]
                psM = [pes_.enter_context(nc.psum_tensor(name + f"psM{i}", [128, 512], F32)) for i in range(4)]
                for sub in range(4):
                    t0_ = st_list[0] * 512 + sub * 128
                    norm_tile(pes, x_win[t0_:t0_ + 128, :], gbc, hT[0], sub * 128, ("hT", 0))
                ws = []
                for si, sp in enumerate(specs):
                    ws.append(load_weight_bf16(S, nc, pes_, w_in, sp["col0"], sp["ncols"], f"{name}w{si}", stage,
                                               name + "wst"))
                env = dict(pes_=pes_, psM=psM)
                for sp in specs:
                    if "setup" in sp:
                        sp["setup"](env)
                pmc = [0]

                def emit_norm(sti, sub, part="both"):
                    t0 = st_list[sti] * 512 + sub * 128
                    norm_tile(pes, x_win[t0:t0 + 128, :], gbc, hT[sti % 2], sub * 128, ("hT", sti % 2), part=part)

                def grp_f(st, h, hkey, sp, w, wkeys, cc):
                    ps = psM[pmc[0] % 4]
                    pk = ("psM", pmc[0] % 4)
                    pmc[0] += 1
                    for c in range(NCH):
                        S.op("pe", lambda e, c=c: e.matmul(
                            ps[:, :], lhsT=w[:, c, cc * 128:(cc + 1) * 128], rhs=h[:, c, :],
                            start=(c == 0), stop=(c == NCH - 1)), reads=[hkey] + wkeys, writes=[pk])
                    sp["evac"](env, st, cc, ps, pk)

                def grp_t(st, h, hkey, sp, w, wkeys, sub, nb):
                    n0 = nb * 512
                    nn = min(512, sp["ncols"] - n0)
                    ps = psM[pmc[0] % 4]
                    pk = ("psM", pmc[0] % 4)
                    pmc[0] += 1
                    for c in range(NCH):
                        S.op("pe", lambda e, c=c: e.matmul(
                            ps[:, 0:nn], lhsT=h[:, c, sub * 128:(sub + 1) * 128],
                            rhs=w[:, c, n0:n0 + nn], start=(c == 0), stop=(c == NCH - 1)),
                            reads=[hkey] + wkeys, writes=[pk])
                    sp["evac"](env, st, sub, nb, ps, pk, nn)

                for sti, st in enumerate(st_list):
                    h = hT[sti % 2]
                    hkey = ("hT", sti % 2)
                    groups = []
                    for si, sp in enumerate(specs):
                        w = ws[si]
                        wkeys = [(f"{name}w{si}", c) for c in range(NCH)]
                        if sp["kind"] == "f":
                            for cc in range(sp["ncols"] // 128):
                                groups.append(lambda st=st, h=h, hkey=hkey, sp=sp, w=w, wkeys=wkeys, cc=cc:
                                              grp_f(st, h, hkey, sp, w, wkeys, cc))
                        else:
                            for sub in range(4):
                                for nb in range((sp["ncols"] + 511) // 512):
                                    groups.append(lambda st=st, h=h, hkey=hkey, sp=sp, w=w, wkeys=wkeys, sub=sub, nb=nb:
                                                  grp_t(st, h, hkey, sp, w, wkeys, sub, nb))
                    per = (len(groups) + 3) // 4
                    for k in range(4):
                        if sti + 1 < len(st_list):
                            emit_norm(sti + 1, k, "pre")
                        for g in groups[k * per:(k + 1) * per]:
                            g()
                        if sti + 1 < len(st_list):
                            emit_norm(sti + 1, k, "post")
                    for sp in specs:
                        if "flush" in sp:
                            sp["flush"](env, st)
                S.barrier()

        def mk_f_to_dram(dst, nchunks, tag, rope=False, sb_dst=None, st0=0, func=None):
            st_ = {}

            def setup(env):
                if sb_dst is None:
                    st_["stg"] = [env["pes_"].enter_context(nc.sbuf_tensor(f"{tag}stg{i}", [128, nchunks, 512], BF16))
                                  for i in range(2)]
                st_["n"] = 0
                if func == "sigexp":
                    st_["sg"] = env["pes_"].enter_context(nc.sbuf_tensor(f"{tag}sg", [128, 512], F32))
                if rope:
                    st_["kb"] = env["pes_"].enter_context(nc.sbuf_tensor(f"{tag}kb", [128, 512], BF16))
                    st_["t1"] = env["pes_"].enter_context(nc.sbuf_tensor(f"{tag}t1", [128, 512], F32))
                    st_["t2"] = env["pes_"].enter_context(nc.sbuf_tensor(f"{tag}t2", [128, 512], F32))
                    st_["psR"] = env["pes_"].enter_context(nc.psum_tensor(f"{tag}psR", [128, 512], F32))
                    st_["cos"] = env["pes_"].enter_context(nc.sbuf_tensor(f"{tag}cos", [128, 512], F32))
                    st_["sin"] = env["pes_"].enter_context(nc.sbuf_tensor(f"{tag}sin", [128, 512], F32))

            def evac(env, st, cc, ps, pk):
                if sb_dst is None:
                    sl = st_["n"] % 2
                    oap = st_["stg"][sl][:, cc, :]
                    okey = (tag + "stg", sl)
                else:
                    oap = sb_dst[:, cc, (st - st0) * 512:(st - st0 + 1) * 512]
                    okey = (tag + "sb", cc, st)
                if not rope:
                    if func == "sigexp":
                        sg = st_["sg"]
                        S.op("act", lambda e: e.activation(out=sg[:], in_=ps[:, :], func=AF.Exp, scale=-1.0),
                             reads=[pk], writes=[tag + "sg"])
                        S.op("dve", lambda e: e.tensor_scalar_add(sg[:], sg[:], 1.0), reads=[tag + "sg"], writes=[tag + "sg"])
                        S.op("dve", lambda e: e.reciprocal(oap, sg[:]), reads=[tag + "sg"], writes=[okey])
                    elif func is not None:
                        S.op("act", lambda e: e.activation(out=oap, in_=ps[:, :], func=func), reads=[pk], writes=[okey])
                    elif cc % 2 == 0:
                        S.op("act", lambda e: e.copy(out=oap, in_=ps[:, :]), reads=[pk], writes=[okey])
                    else:
                        S.op("dve", lambda e: e.tensor_copy(oap, ps[:, :]), reads=[pk], writes=[okey])
                else:
                    kb, t1, t2, psR = st_["kb"], st_["t1"], st_["t2"], st_["psR"]
                    p0 = 0
                    if cc == 0:
                        S.dma("sp", st_["cos"][:], cosT[:, st * 512:(st + 1) * 512], writes=[tag + "cos"])
                        S.dma("sp", st_["sin"][:], sinT[:, st * 512:(st + 1) * 512], writes=[tag + "sin"])
                    S.op("act", lambda e: e.copy(out=kb[:], in_=ps[:, :]), reads=[pk], writes=[tag + "kb"])
                    S.op("pe", lambda e: e.matmul(psR[:, :], lhsT=rotT[:], rhs=kb[:], start=True, stop=True),
                         reads=[tag + "kb", "rotT"], writes=[tag + "psR"])
                    S.op("dve", lambda e: e.tensor_tensor(out=t1[:], in0=kb[:], in1=st_["cos"][:, 0:512], op=ALU.mult),
                         reads=[tag + "kb", tag + "cos"], writes=[tag + "t1"])
                    S.op("act", lambda e: e.copy(out=t2[:], in_=psR[:, :]), reads=[tag + "psR"], writes=[tag + "t2"])
                    S.op("dve", lambda e: e.tensor_tensor(out=t2[:], in0=t2[:], in1=st_["sin"][:, 0:512], op=ALU.mult),
                         reads=[tag + "t2", tag + "sin"], writes=[tag + "t2"])
                    S.op("dve", lambda e: e.tensor_tensor(out=oap, in0=t1[:], in1=t2[:], op=ALU.add),
                         reads=[tag + "t1", tag + "t2"], writes=[okey])

            def flush(env, st):
                if sb_dst is None:
                    sl = st_["n"] % 2
                    stg = st_["stg"][sl]
                    S.dma("pool", dst.rearrange("(c p) t -> p c t", p=128)[:, :, st * 512:(st + 1) * 512], stg[:],
                          reads=[(tag + "stg", sl)], writes=[(tag + "dram", st)])
                st_["n"] += 1

            return dict(setup=setup, evac=evac, flush=flush)

        def mk_t_to_dram(dst, ncols, tag, dt=BF16):
            st_ = {}

            def setup(env):
                st_["stg"] = [env["pes_"].enter_context(nc.sbuf_tensor(f"{tag}stg{i}", [128, 4, ncols], dt))
                              for i in range(2)]
                st_["n"] = 0

            def evac(env, st, sub, nb, ps, pk, nn):
                sl = st_["n"] % 2
                stg = st_["stg"][sl]
                skey = (tag + "stg", sl)
                if (sub + nb) % 2 == 0:
                    S.op("act", lambda e: e.copy(out=stg[:, sub, nb * 512:nb * 512 + nn], in_=ps[:, 0:nn]),
                         reads=[pk], writes=[skey])
                else:
                    S.op("dve", lambda e: e.tensor_copy(stg[:, sub, nb * 512:nb * 512 + nn], ps[:, 0:nn]),
                         reads=[pk], writes=[skey])

            def flush(env, st):
                sl = st_["n"] % 2
                stg = st_["stg"][sl]
                S.dma("pool", dst[st * 512:(st + 1) * 512, :].rearrange("(s p) n -> p s n", p=128), stg[:],
                      reads=[(tag + "stg", sl)], writes=[(tag + "dram", st)])
                st_["n"] += 1

            return dict(setup=setup, evac=evac, flush=flush)

        spA = [dict(kind="f", col0=0, ncols=1024, **mk_f_to_dram(s_minT, 8, "Amin")),
               dict(kind="t", col0=1024, ncols=1024, **mk_t_to_dram(s_mv, 1024, "Amv")),
               dict(kind="t", col0=3072, ncols=8, **mk_t_to_dram(s_gates, 8, "Ag", F32))]
        phase_proj("A", list(range(8)), spA, g_mix[0:1, :])

        if dbg is not None and dbg["name"] == "stopA":
            S.barrier()
            return nc
        spB = [dict(kind="f", col0=4104, ncols=1024, **mk_f_to_dram(s_akT, 8, "Bak", rope=True)),
               dict(kind="t", col0=5128, ncols=1024, **mk_t_to_dram(s_av, 1024, "Bav"))]
        phase_proj("B", list(range(2, 8)), spB, g_mix[0:1, :])
        if dbg is not None and dbg["name"] == "stopB":
            S.barrier()
            return nc
        spC = [dict(kind="f", col0=2048, ncols=1024, **mk_f_to_dram(None, 8, "Cmo", sb_dst=moT, st0=6, func="sigexp")),
               dict(kind="f", col0=3080, ncols=1024, **mk_f_to_dram(None, 8, "Caq", rope=True, sb_dst=aqT, st0=6))]
        phase_proj("C", [6, 7], spC, g_mix[0:1, :])

        mixT = es_mix2.enter_context(nc.sbuf_tensor("mixT", [128, NCH, OWN], BF16))
        print("after C", S.cnt, S.epoch, S.nsem)
        if dbg is not None and dbg["name"] == "aqT":
            S.dma("sp", dbg_out[0:1024, :].rearrange("(c p) t -> p c t", p=128), aqT[:], writes=["dbgo"])
            S.dma("sp", dbg_out[1024:2048, :].rearrange("(c p) t -> p c t", p=128), moT[:], writes=["dbgo2"])
            S.barrier()
            return nc
        with ExitStack() as ds:
            def sb(name, shape, dt=F32):
                return ds.enter_context(nc.sbuf_tensor("D" + name, shape, dt))

            def pst(name):
                return ds.enter_context(nc.psum_tensor("Dps" + name, [128, 512], F32))

            cw = sb("cw", [128, 8, 4]); cbias = sb("cbias", [128, 8]); gm = sb("gm", [128, 8]); gb = sb("gb", [128, 8])
            pm = sb("pm", [128, 32]); tri_f = sb("trif", [128, 128]); ones_f = sb("onesf", [128, 128])
            S.dma("sp", cw[:], cw_d[:, :, :], writes=["cw"])
            S.dma("sp", cbias[:], cb_d[:, :], writes=["cbias"])
            S.dma("sp", gm[:], gm_d[:, :], writes=["gm"])
            S.dma("sp", gb[:], gb_d[0:1, :].partition_broadcast(128), writes=["gb"])
            S.dma("sp", pm[:], pm_d[:, :], writes=["pm"])
            S.dma("sp", tri_f[:], tri_d[:, :], writes=["tri_f"])
            S.op("pool", lambda e: e.memset(ones_f[:], 1.0), writes=["ones_f"])
            wstg = sb("wstg", [128, 4, 2, 256])
            wq = sb("wq", [128, 4, 2, 256], BF16); wk = sb("wk", [128, 4, 2, 256], BF16)
            S.dma("sp", wstg[:], w_mq_d.rearrange("h (c p) e -> p h c e", p=128), writes=["wstg"])
            S.op("dve", lambda e: e.tensor_copy(wq[:], wstg[:]), reads=["wstg"], writes=["wq"])
            S.dma("sp", wstg[:], w_mk_d.rearrange("h (c p) e -> p h c e", p=128), reads=[], writes=["wstg"])
            S.op("dve", lambda e: e.tensor_copy(wk[:], wstg[:]), reads=["wstg"], writes=["wk"])
            Sst = [sb(f"Sst{h}", [128, 2, 384]) for h in range(4)]
            Sbf = [sb(f"Sbf{h}", [128, 2, 384], BF16) for h in range(4)]
            for h in range(4):
                S.op("pool", lambda e, h=h: e.memset(Sst[h][:], 0.0), writes=[("Sst", h, 0), ("Sst", h, 1)])
                S.op("pool", lambda e, h=h: e.memset(Sbf[h][:], 0.0), writes=[("Sbf", h)])
            xin = [sb(f"xin{i}", [128, 8, 131], BF16) for i in range(2)]
            vaug = [sb(f"vaug{i}", [128, 4, 384], BF16) for i in range(2)]
            gt = [sb(f"gt{i}", [128, 8]) for i in range(2)]
            cT = [sb(f"cT{i}", [128, 8, 128], BF16) for i in range(2)]
            for i in range(2):
                S.op("pool", lambda e, i=i: e.memset(vaug[i][:, :, 256:384], 1.0), writes=[("vaug", i)])
            acc = [sb(f"acc{i}", [128, 128]) for i in range(2)]
            g2 = sb("g2", [128, 8]); sp4 = sb("sp4", [128, 4]); w4 = sb("w4", [128, 4]); spb = sb("spb", [128, 4, 128])
            two = lambda nm, shape, dt=F32: [sb(f"{nm}{i}", shape, dt) for i in range(2)]
            kp = two("kp", [128, 256], BF16); kTb = two("kTb", [128, 2, 128], BF16); qTb = two("qTb", [128, 2, 128], BF16)
            clampT = two("clampT", [128, 128]); PT = two("PT", [128, 128], BF16); dd = two("dd", [128, 128])
            hn = two("hn", [128, 2, 128]); sq = two("sq", [128, 2, 128]); rr = two("rr", [128, 128]); tmpo = two("tmpo", [128, 128])
            dec = two("dec", [128, 1]); dSs = two("dSs", [128, 2, 384])
            psk = pst("k"); pskT = pst("kT"); psqT = pst("qT"); psS = pst("S"); psO = pst("O"); psMisc = pst("M")
            psdS = [pst("dS0"), pst("dS1")]
            minT_v = s_minT.rearrange("(c p) t -> p c t", p=128)

            def conv_tile(et):
                rs = slice(et * 128, (et + 1) * 128)
                S.dma("pool", uv_d[rs, 0:D], u_tab_d[rs, :], writes=[("uvd", et, 0)])
                S.dma("pool", uv_d[rs, D:2 * D], v_tab_d[rs, :], writes=[("uvd", et, 1)])

            for ck in range(32):
                own = ck >= 24
                par = ck % 2
                t0 = ck * 128
                xin_, vaug_, gt_, cT_ = xin[par], vaug[par], gt[par], cT[par]
                if ck == 0:
                    S.op("pool", lambda e: e.memset(xin_[:, :, 0:3], 0.0), writes=[("xin", par)])
                    S.dma("sp", xin_[:, :, 3:131], minT_v[:, :, 0:128], writes=[("xin", par)])
                else:
                    S.dma("sp", xin_[:, :, 0:131], minT_v[:, :, t0 - 3:t0 + 128], writes=[("xin", par)])
                S.dma("sp", vaug_[:, :, 0:256], s_mv[t0:t0 + 128, :].rearrange("p (h e) -> p h e", h=4),
                      writes=[("vaug", par)])
                S.dma("sp", gt_[:], s_gates[t0:t0 + 128, :], writes=[("gt", par)])
                for et in range(ck * 4, ck * 4 + 4):
                    conv_tile(et)
                S.op("dve", lambda e: e.tensor_tensor(out=g2[:], in0=gt_[:], in1=gb[:], op=ALU.add),
                     reads=[("gt", par), "gb"], writes=["g2"])
                S.op("dve", lambda e: e.tensor_scalar(out=g2[:, 0:4], in0=g2[:, 0:4], scalar1=pm[:, ck:ck + 1],
                                                      scalar2=None, op0=ALU.add), reads=["g2", "pm"], writes=["g2"])
                S.op("act", lambda e: e.activation(out=sp4[:], in_=g2[:, 4:8], func=AF.Exp, scale=-1.0),
                     reads=["g2"], writes=["sp4"])
                S.op("dve", lambda e: e.tensor_scalar_add(sp4[:], sp4[:], 1.0), reads=["sp4"], writes=["sp4"])
                S.op("act", lambda e: e.activation(out=sp4[:], in_=sp4[:], func=AF.Ln), reads=["sp4"], writes=["sp4"])
                S.op("pe", lambda e: e.matmul(psMisc[:, 256:260], lhsT=tri_f[:], rhs=sp4[:], start=True, stop=True),
                     reads=["tri_f", "sp4"], writes=["ps_csp"])
                S.op("act", lambda e: e.copy(out=w4[:], in_=psMisc[:, 256:260]), reads=["ps_csp"], writes=["w4"])
                S.op("dve", lambda e: e.tensor_tensor(out=w4[:], in0=g2[:, 0:4], in1=w4[:], op=ALU.add),
                     reads=["g2", "w4"], writes=["w4"])
                S.op("act", lambda e: e.activation(out=w4[:], in_=w4[:], func=AF.Exp), reads=["w4"], writes=["w4"])
                S.op("dve", lambda e: e.tensor_scalar_mul(w4[:], w4[:], 0.0625), reads=["w4"], writes=["w4"])
                S.op("dve", lambda e: e.tensor_copy(spb[:], sp4[:].unsqueeze(2).to_broadcast([128, 4, 128])),
                     reads=["sp4"], writes=["spb"])
                for c8 in range(8):
                    a_ = acc[c8 % 2]
                    ak = ("acc", c8 % 2)
                    S.op("dve", lambda e, c8=c8, a_=a_: e.tensor_scalar(out=a_[:], in0=xin_[:, c8, 0:128],
                                                                        scalar1=cw[:, c8, 0:1], scalar2=None,
                                                                        op0=ALU.mult),
                         reads=[("xin", par), "cw"], writes=[ak])
                    for jj in range(1, 4):
                        S.op("dve", lambda e, c8=c8, a_=a_, jj=jj: e.scalar_tensor_tensor(
                            out=a_[:], in0=xin_[:, c8, jj:jj + 128], scalar=cw[:, c8, jj:jj + 1], in1=a_[:],
                            op0=ALU.mult, op1=ALU.add), reads=[("xin", par), "cw", ak], writes=[ak])
                    S.op("act", lambda e, c8=c8, a_=a_: e.activation(out=cT_[:, c8, :], in_=a_[:], func=AF.Silu,
                                                                     bias=cbias[:, c8:c8 + 1]),
                         reads=[ak, "cbias"], writes=[("cT", par, c8)])
                def head_gen(h):
                    hp = h % 2
                    kp_, kTb_, qTb_, clampT_, PT_, dd_ = kp[hp], kTb[hp], qTb[hp], clampT[hp], PT[hp], dd[hp]
                    hn_, sq_, rr_, tmpo_, dec_, dSs_ = hn[hp], sq[hp], rr[hp], tmpo[hp], dec[hp], dSs[hp]
                    ckeys = [("cT", par, 2 * h), ("cT", par, 2 * h + 1)]
                    for dc in range(2):
                        S.op("pe", lambda e, dc=dc: e.matmul(psk[:, 0:256], lhsT=cT_[:, 2 * h + dc, :], rhs=wk[:, h, dc, :],
                                                             start=(dc == 0), stop=(dc == 1)),
                             reads=ckeys + ["wk"], writes=["psk"])
                    S.op("dve", lambda e: e.tensor_scalar(out=kp_[:], in0=psk[:, 0:256], scalar1=w4[:, h:h + 1],
                                                          scalar2=None, op0=ALU.mult), reads=["psk", "w4"], writes=[("kp", hp)])
                    yield
                    S.op("pe", lambda e: e.matmul(psMisc[:, 0:128], lhsT=spb[:, h, :], rhs=tri_f[:], start=True, stop=True),
                         reads=["spb", "tri_f"], writes=["ps_cb"])
                    S.op("act", lambda e: e.activation(out=dec_[:], in_=psMisc[:, 127:128], func=AF.Exp, scale=-1.0),
                         reads=["ps_cb"], writes=[("dec", hp)])
                    if own:
                        o0 = (ck - 24) * 128
                        S.op("act", lambda e: e.activation(out=clampT_[:], in_=psMisc[:, 0:128], func=AF.Exp),
                             reads=["ps_cb"], writes=[("clampT", hp)])
                        yield
                        for ec in range(2):
                            for dc in range(2):
                                S.op("pe", lambda e, ec=ec, dc=dc: e.matmul(
                                    pskT[:, ec * 128:(ec + 1) * 128], lhsT=wk[:, h, dc, ec * 128:(ec + 1) * 128],
                                    rhs=cT_[:, 2 * h + dc, :], start=(dc == 0), stop=(dc == 1)),
                                    reads=ckeys + ["wk"], writes=["pskT"])
                        for ec in range(2):
                            for dc in range(2):
                                S.op("pe", lambda e, ec=ec, dc=dc: e.matmul(
                                    psqT[:, ec * 128:(ec + 1) * 128], lhsT=wq[:, h, dc, ec * 128:(ec + 1) * 128],
                                    rhs=cT_[:, 2 * h + dc, :], start=(dc == 0), stop=(dc == 1)),
                                    reads=ckeys + ["wq"], writes=["psqT"])
                        S.op("act", lambda e: e.copy(out=kTb_[:], in_=pskT[:, 0:256].rearrange("p (c t) -> p c t", c=2)),
                             reads=["pskT"], writes=[("kTb", hp)])
                        S.op("dve", lambda e: e.tensor_copy(qTb_[:], psqT[:, 0:256].rearrange("p (c t) -> p c t", c=2)),
                             reads=["psqT"], writes=[("qTb", hp)])
                        yield
                        for ec in range(2):
                            S.op("pe", lambda e, ec=ec: e.matmul(psS[:, 0:128], lhsT=kTb_[:, ec, :], rhs=qTb_[:, ec, :],
                                                                 start=(ec == 0), stop=(ec == 1)),
                                 reads=[("kTb", hp), ("qTb", hp)], writes=["psS"])
                        S.op("act", lambda e: e.copy(out=dd_[:], in_=psS[:, 0:128]), reads=["psS"], writes=[("dd", hp)])
                        S.op("dve", lambda e: e.scalar_tensor_tensor(out=PT_[:], in0=dd_[:], scalar=w4[:, h:h + 1],
                                                                     in1=tri_f[:], op0=ALU.mult, op1=ALU.mult),
                             reads=[("dd", hp), "w4", "tri_f"], writes=[("PT", hp)])
                        yield
                        for j in range(3):
                            S.op("pe", lambda e, j=j: e.matmul(psO[:, j * 128:(j + 1) * 128],
                                                               lhsT=vaug_[:, h, j * 128:(j + 1) * 128], rhs=PT_[:],
                                                               start=True, stop=False),
                                 reads=[("vaug", par), ("PT", hp)], writes=["psO"])
                            for dc in range(2):
                                S.op("pe", lambda e, j=j, dc=dc: e.matmul(
                                    psO[:, j * 128:(j + 1) * 128], lhsT=Sbf[h][:, dc, j * 128:(j + 1) * 128],
                                    rhs=qTb_[:, dc, :], start=False, stop=(dc == 1)),
                                    reads=[("Sbf", h), ("qTb", hp)], writes=["psO"])
                        S.op("act", lambda e: e.activation(out=dd_[:], in_=psO[:, 256:384], func=AF.Abs),
                             reads=["psO"], writes=[("dd", hp)])
                        S.op("act", lambda e: e.copy(out=hn_[:], in_=psO[:, 0:256].rearrange("p (c t) -> p c t", c=2)),
                             reads=["psO"], writes=[("hn", hp)])
                        yield
                        S.op("dve", lambda e: e.tensor_tensor(out=dd_[:], in0=dd_[:], in1=clampT_[:], op=ALU.max),
                             reads=[("dd", hp), ("clampT", hp)], writes=[("dd", hp)])
                        S.op("dve", lambda e: e.reciprocal(dd_[:], dd_[:]), reads=[("dd", hp)], writes=[("dd", hp)])
                        S.op("dve", lambda e: e.tensor_tensor(
                            out=hn_[:], in0=hn_[:], in1=dd_[:].unsqueeze(1).to_broadcast([128, 2, 128]), op=ALU.mult),
                            reads=[("hn", hp), ("dd", hp)], writes=[("hn", hp)])
                        S.op("act", lambda e: e.activation(out=sq_[:], in_=hn_[:], func=AF.Square), reads=[("hn", hp)], writes=[("sq", hp)])
                        for j in range(2):
                            S.op("pe", lambda e, j=j: e.matmul(psMisc[:, 128:256], lhsT=ones_f[:], rhs=sq_[:, j, :],
                                                               start=(j == 0), stop=(j == 1)),
                                 reads=[("sq", hp), "ones_f"], writes=["ps_n"])
                        S.op("dve", lambda e: e.tensor_scalar(out=rr_[:], in0=psMisc[:, 128:256], scalar1=1.0 / 256,
                                                              scalar2=EPS, op0=ALU.mult, op1=ALU.add),
                             reads=["ps_n"], writes=[("rr", hp)])
                        yield
                        S.op("act", lambda e: e.sqrt(rr_[:], rr_[:]), reads=[("rr", hp)], writes=[("rr", hp)])
                        S.op("dve", lambda e: e.reciprocal(rr_[:], rr_[:]), reads=[("rr", hp)], writes=[("rr", hp)])
                        for j in range(2):
                            S.op("dve", lambda e, j=j: e.scalar_tensor_tensor(
                                out=tmpo_[:], in0=hn_[:, j, :], scalar=gm[:, 2 * h + j:2 * h + j + 1], in1=rr_[:],
                                op0=ALU.mult, op1=ALU.mult), reads=[("hn", hp), "gm", ("rr", hp)], writes=[("tmpo", hp)])
                            S.op("pool", lambda e, j=j: e.tensor_tensor(
                                out=mixT[:, 2 * h + j, o0:o0 + 128], in0=tmpo_[:], in1=moT[:, 2 * h + j, o0:o0 + 128],
                                op=ALU.mult), reads=[("tmpo", hp)], writes=[("mixT", 2 * h + j, ck)])
                    yield
                    for dc in range(2):
                        S.op("pe", lambda e, dc=dc: e.matmul(psdS[dc][:, 0:384], lhsT=kp_[:, dc * 128:(dc + 1) * 128],
                                                             rhs=vaug_[:, h, :], start=True, stop=True),
                             reads=[("kp", hp), ("vaug", par)], writes=[("psdS", dc)])
                    for dc in range(2):
                        S.op("dve", lambda e, dc=dc: e.tensor_scalar(out=Sst[h][:, dc, :], in0=Sst[h][:, dc, :],
                                                                     scalar1=dec_[:, 0:1], scalar2=None, op0=ALU.mult),
                             reads=[("Sst", h, dc), ("dec", hp)], writes=[("Sst", h, dc)])
                        S.op("act", lambda e, dc=dc: e.copy(out=dSs_[:, dc, :], in_=psdS[dc][:, 0:384]),
                             reads=[("psdS", dc)], writes=[(("dSs", hp), dc)])
                        S.op("dve", lambda e, dc=dc: e.scalar_tensor_tensor(
                            out=Sst[h][:, dc, :], in0=dSs_[:, dc, :], scalar=dec_[:, 0:1], in1=Sst[h][:, dc, :],
                            op0=ALU.mult, op1=ALU.add), reads=[(("dSs", hp), dc), ("dec", hp), ("Sst", h, dc)],
                            writes=[("Sst", h, dc)])
                    S.op("act", lambda e: e.copy(out=Sbf[h][:], in_=Sst[h][:]),
                         reads=[("Sst", h, 0), ("Sst", h, 1)], writes=[("Sbf", h)])
                for pair in ((0, 1), (2, 3)):
                    gens = [head_gen(h) for h in pair]
                    while gens:
                        for g in list(gens):
                            try:
                                next(g)
                            except StopIteration:
                                gens.remove(g)
            S.barrier()

        with ExitStack() as ds:
            def sb(name, shape, dt=F32):
                return ds.enter_context(nc.sbuf_tensor("E" + name, shape, dt))

            def pst(name):
                return ds.enter_context(nc.psum_tensor("Eps" + name, [128, 512], F32))

            kT = sb("kT", [128, 8, NPRE], BF16)
            S.dma("sp", kT[:], s_akT.rearrange("(c p) t -> p c t", p=128)[:, :, 1024:WIN], writes=["kT"])
            accN = sb("accN", [128, 8, OWN]); accD = sb("accD", [128, 8, OWN])
            VA = [sb(f"VA{i}", [128, 1024], BF16) for i in range(2)]
            VB = [sb(f"VB{i}", [128, 1024], BF16) for i in range(2)]
            two = lambda nm, shape, dt=F32: [sb(f"{nm}{i}", shape, dt) for i in range(2)]
            PA = two("PA", [128, 128], BF16); PB = two("PB", [128, 128], BF16)
            eA = two("eA", [128, 128]); eB = two("eB", [128, 128]); tO = two("tO", [128, 128]); tD = two("tD", [128, 128])
            kbA = sb("kbA", [128, 48]); ga = sb("ga", [128, 8])
            trl = sb("trl", [128, 128]); tru = sb("tru", [128, 128]); ones_b = sb("onesb", [128, 128], BF16)
            ones_f2 = sb("onesf2", [128, 128])
            S.dma("sp", kbA[:], kbA_d[:, :], writes=["kbA"])
            S.dma("sp", ga[:], ga_d[:, :], writes=["ga"])
            S.dma("sp", trl[:], trl_d[:, :], writes=["trl"])
            S.dma("sp", tru[:], tri_d[:, :], writes=["tru"])
            S.op("pool", lambda e: e.memset(ones_b[:], 1.0), writes=["ones_b"])
            S.op("pool", lambda e: e.memset(ones_f2[:], 1.0), writes=["ones_f2"])
            psA = [pst("A0"), pst("A1")]; psB = [pst("B0"), pst("B1")]
            psO = [pst("O0"), pst("O1")]; psD = [pst("D0"), pst("D1")]
            psN = psA[0]
            SC = 128.0 ** -0.5
            gi = 0
            for Q in range(2):
                glist = [(1, 0, sub) for sub in range(4)] + [(4, r, 0) for r in range(4)] + [(16, r, 0) for r in range(16)]
                for (d, r, sub) in glist:
                    nq = 128 if d < 16 else 32
                    u0 = 2048 + 512 * Q + r + 128 * sub
                    i0 = 512 * Q + r + 128 * sub
                    uA = u0 - 128 * d
                    par = gi % 2
                    va, vb = VA[par], VB[par]
                    S.dma("sp", va[:], bass.AP(tensor=s_av.tensor, offset=(1024 + uA) * 1024, ap=[[d * 1024, 128], [1, 1024]]),
                          writes=[("VA", par)])
                    S.dma("sp", vb[0:nq, :], bass.AP(tensor=s_av.tensor, offset=(1024 + u0) * 1024, ap=[[d * 1024, nq], [1, 1024]]),
                          writes=[("VB", par)])
                    qsl = slice(i0, i0 + (nq - 1) * d + 1, d)
                    def st1(h):
                        p2 = h % 2
                        q_ap = aqT[:, h, qsl]
                        S.op("pe", lambda e: e.matmul(psA[p2][:, 0:nq], lhsT=kT[:, h, uA:uA + 127 * d + 1:d], rhs=q_ap,
                                                      start=True, stop=True), reads=["kT"], writes=[("psA", p2)])
                        S.op("pe", lambda e: e.matmul(psB[p2][0:nq, 0:nq], lhsT=kT[:, h, u0:u0 + (nq - 1) * d + 1:d], rhs=q_ap,
                                                      start=True, stop=True), reads=["kT"], writes=[("psB", p2)])

                    def st2(h):
                        p2 = h % 2
                        S.op("act", lambda e: e.activation(out=eA[p2][:, 0:nq], in_=psA[p2][:, 0:nq], func=AF.Exp, scale=SC,
                                                           bias=kbA[:, gi:gi + 1]), reads=[("psA", p2), "kbA"], writes=[("eA", p2)])
                        S.op("act", lambda e: e.activation(out=eB[p2][0:nq, 0:nq], in_=psB[p2][0:nq, 0:nq], func=AF.Exp, scale=SC),
                             reads=[("psB", p2)], writes=[("eB", p2)])
                        S.op("dve", lambda e: e.tensor_tensor(out=PA[p2][:, 0:nq], in0=eA[p2][:, 0:nq], in1=trl[:, 0:nq], op=ALU.mult),
                             reads=[("eA", p2), "trl"], writes=[("PA", p2)])
                        S.op("dve", lambda e: e.tensor_tensor(out=PB[p2][0:nq, 0:nq], in0=eB[p2][0:nq, 0:nq], in1=tru[0:nq, 0:nq],
                                                              op=ALU.mult), reads=[("eB", p2), "tru"], writes=[("PB", p2)])

                    def st3(h):
                        p2 = h % 2
                        S.op("pe", lambda e: e.matmul(psO[p2][:, 0:nq], lhsT=va[:, h * 128:(h + 1) * 128], rhs=PA[p2][:, 0:nq],
                                                      start=True, stop=False), reads=[("VA", par), ("PA", p2)], writes=[("psO", p2)])
                        S.op("pe", lambda e: e.matmul(psO[p2][:, 0:nq], lhsT=vb[0:nq, h * 128:(h + 1) * 128], rhs=PB[p2][0:nq, 0:nq],
                                                      start=False, stop=True), reads=[("VB", par), ("PB", p2)], writes=[("psO", p2)])
                        S.op("pe", lambda e: e.matmul(psD[p2][:, 0:nq], lhsT=ones_b[:, :], rhs=PA[p2][:, 0:nq],
                                                      start=True, stop=False), reads=["ones_b", ("PA", p2)], writes=[("psD", p2)])
                        S.op("pe", lambda e: e.matmul(psD[p2][:, 0:nq], lhsT=ones_b[0:nq, :], rhs=PB[p2][0:nq, 0:nq],
                                                      start=False, stop=True), reads=["ones_b", ("PB", p2)], writes=[("psD", p2)])

                    def st4(h):
                        p2 = h % 2
                        akey = ("acc", h, Q)
                        if d == 1:
                            S.op("act", lambda e: e.copy(out=accN[:, h, qsl], in_=psO[p2][:, 0:nq]), reads=[("psO", p2)], writes=[akey])
                            S.op("act", lambda e: e.copy(out=accD[:, h, qsl], in_=psD[p2][:, 0:nq]), reads=[("psD", p2)], writes=[akey])
                        else:
                            S.op("act", lambda e: e.copy(out=tO[p2][:, 0:nq], in_=psO[p2][:, 0:nq]), reads=[("psO", p2)], writes=[("tO", p2)])
                            S.op("act", lambda e: e.copy(out=tD[p2][:, 0:nq], in_=psD[p2][:, 0:nq]), reads=[("psD", p2)], writes=[("tD", p2)])
                            S.op("dve", lambda e: e.tensor_tensor(out=accN[:, h, qsl], in0=accN[:, h, qsl], in1=tO[p2][:, 0:nq],
                                                                  op=ALU.add), reads=[("tO", p2), akey], writes=[akey])
                            S.op("dve", lambda e: e.tensor_tensor(out=accD[:, h, qsl], in0=accD[:, h, qsl], in1=tD[p2][:, 0:nq],
                                                                  op=ALU.add), reads=[("tD", p2), akey], writes=[akey])

                    for i_ in range(9):
                        if i_ < 8:
                            st1(i_)
                            st2(i_)
                        if i_ >= 1:
                            st3(i_ - 1)
                            st4(i_ - 1)
                    gi += 1
            o5 = sb("o5", [128, 512]); sq5 = sb("sq5", [128, 512]); r5 = sb("r5", [128, 512])
            for h in range(8):
                for Q in range(2):
                    cs = slice(Q * 512, (Q + 1) * 512)
                    akey = ("acc", h, Q)
                    S.op("dve", lambda e: e.reciprocal(r5[:], accD[:, h, cs]), reads=[akey], writes=["r5"])
                    S.op("dve", lambda e: e.tensor_tensor(out=o5[:], in0=accN[:, h, cs], in1=r5[:], op=ALU.mult),
                         reads=[akey, "r5"], writes=["o5"])
                    S.op("act", lambda e: e.activation(out=sq5[:], in_=o5[:], func=AF.Square), reads=["o5"], writes=["sq5"])
                    S.op("pe", lambda e: e.matmul(psN[:, :], lhsT=ones_f2[:], rhs=sq5[:], start=True, stop=True),
                         reads=["ones_f2", "sq5"], writes=[("psA", 0)])
                    S.op("dve", lambda e: e.tensor_scalar(out=r5[:], in0=psN[:, :], scalar1=1.0 / 128, scalar2=EPS,
                                                          op0=ALU.mult, op1=ALU.add), reads=[("psA", 0)], writes=["r5"])
                    S.op("act", lambda e: e.sqrt(r5[:], r5[:]), reads=["r5"], writes=["r5"])
                    S.op("dve", lambda e: e.reciprocal(r5[:], r5[:]), reads=["r5"], writes=["r5"])
                    S.op("dve", lambda e: e.scalar_tensor_tensor(out=mixT[:, 8 + h, cs], in0=o5[:], scalar=ga[:, h:h + 1],
                                                                 in1=r5[:], op0=ALU.mult, op1=ALU.mult),
                         reads=["o5", "ga", "r5"], writes=[("mixT", 8 + h, Q)])
            S.barrier()

        if dbg is not None and dbg["name"] == "mixT":
            S.dma("sp", dbg_out.rearrange("(c p) t -> p c t", p=128), mixT[:], writes=["dbgo"])
            S.barrier()

        def norm_sb(P, x_ap, xkeys, gbc, hT, col0, hkey, xn=None):
            t = P["tag"]
            xb, ss, psT = P["xb"], P["ss"], P["psT"]
            S.op("act", lambda e: e.activation(out=xb[:], in_=x_ap, func=AF.Square, scale=float(D) ** -0.5,
                                               accum_out=ss[:]), reads=xkeys, writes=[t + "xb", t + "ss"])
            S.op("dve", lambda e: e.tensor_scalar_add(ss[:], ss[:], EPS), reads=[t + "ss"], writes=[t + "ss"])
            S.op("act", lambda e: e.sqrt(ss[:], ss[:]), reads=[t + "ss"], writes=[t + "ss"])
            S.op("dve", lambda e: e.reciprocal(ss[:], ss[:]), reads=[t + "ss"], writes=[t + "ss"])
            if xn is not None:
                S.op("dve", lambda e: e.scalar_tensor_tensor(out=xn[:], in0=x_ap, scalar=ss[:, 0:1], in1=gbc[:],
                                                             op0=ALU.mult, op1=ALU.mult),
                     reads=list(xkeys) + [t + "ss", t + "gbc"], writes=[t + "xn"])
                if hT is not None:
                    S.op("pool", lambda e: e.tensor_copy(xb[:], xn[:]), reads=[t + "xn"], writes=[t + "xb"])
            else:
                S.op("dve", lambda e: e.scalar_tensor_tensor(out=xb[:], in0=x_ap, scalar=ss[:, 0:1], in1=gbc[:],
                                                             op0=ALU.mult, op1=ALU.mult),
                     reads=list(xkeys) + [t + "ss", t + "gbc"], writes=[t + "xb"])
            if hT is None:
                return
            for half in range(2):
                for j in range(8):
                    c = half * 8 + j
                    S.op("pe", lambda e, c=c, j=j, half=half: e.transpose(
                        psT[half][:, j * 128:(j + 1) * 128], xb[:, c * 128:(c + 1) * 128], ident[:]),
                        reads=[t + "xb"], writes=[(t + "psT", half)])
                if half == 0:
                    S.op("act", lambda e, half=half: e.copy(
                        out=hT[:, half * 8:(half + 1) * 8, col0:col0 + 128],
                        in_=psT[half][:].rearrange("p (c t) -> p c t", c=8)),
                        reads=[(t + "psT", half)], writes=[hkey])
                else:
                    S.op("dve", lambda e, half=half: e.tensor_copy(
                        hT[:, half * 8:(half + 1) * 8, col0:col0 + 128],
                        psT[half][:].rearrange("p (c t) -> p c t", c=8)),
                        reads=[(t + "psT", half)], writes=[hkey])

        def load_wblk2(t, w_dram, b2, stages, wbs, kc):
            wv = w_dram.rearrange("(c p) n -> p c n", p=128)
            wb = wbs[b2 % 2]
            for c4 in range(4):
                k = kc[0] % 2
                kc[0] += 1
                st = stages[k]
                S.dma("sp", st, wv[:, c4 * 4:(c4 + 1) * 4, b2 * 256:(b2 + 1) * 256], writes=[(t + "stage", k)])
                S.op("pool" if c4 % 2 == 0 else "dve",
                     lambda e, c4=c4, st=st: e.tensor_copy(wb[:, c4 * 4:(c4 + 1) * 4, :], st),
                     reads=[(t + "stage", k)], writes=[(t + "wb", b2 % 2, c4)])
            return wb, [(t + "wb", b2 % 2, c4) for c4 in range(4)]

        def load_wblk(t, w_dram, nb, stage, wb):
            wv = w_dram.rearrange("(c p) n -> p c n", p=128)
            for c4 in range(4):
                S.dma("sp", stage[:], wv[:, c4 * 4:(c4 + 1) * 4, nb * 512:(nb + 1) * 512], writes=[t + "stage"])
                S.op("pool" if c4 % 2 == 0 else "dve",
                     lambda e, c4=c4: e.tensor_copy(wb[:, c4 * 4:(c4 + 1) * 4, :], stage[:]),
                     reads=[t + "stage"], writes=[(t + "wb", c4)])
            return [(t + "wb", c4) for c4 in range(4)]

        xres = es.enter_context(nc.sbuf_tensor("xres", [128, 8, D], F32))
        for tt in range(8):
            S.dma("sp", xres[:, tt, :], x_win[NPRE + tt * 128:NPRE + (tt + 1) * 128, :], writes=[("xres", tt)])

        def add_proj(tagp, actT, w_dram):
            with ExitStack() as fs:
                stage = [fs.enter_context(nc.sbuf_tensor(f"{tagp}st{i}", [128, 4, 512], F32)) for i in range(2)]
                wblk = [fs.enter_context(nc.sbuf_tensor(f"{tagp}wb{i}", [128, NCH, 512], BF16)) for i in range(2)]
                tmp = [fs.enter_context(nc.sbuf_tensor(f"{tagp}tmp{i}", [128, 512], F32)) for i in range(2)]
                psF = [fs.enter_context(nc.psum_tensor(f"{tagp}ps{i}", [128, 512], F32)) for i in range(2)]
                wv = w_dram.rearrange("(c p) n -> p c n", p=128)
                k = 0
                n = 0
                for nb in range(4):
                    wb = wblk[nb % 2]
                    for c4 in range(4):
                        st = stage[k % 2]
                        S.dma("sp", st[:], wv[:, c4 * 4:(c4 + 1) * 4, nb * 512:(nb + 1) * 512], writes=[(tagp + "st", k % 2)])
                        S.op("pool" if k % 2 == 0 else "dve",
                             lambda e, st=st, wb=wb, c4=c4: e.tensor_copy(wb[:, c4 * 4:(c4 + 1) * 4, :], st[:]),
                             reads=[(tagp + "st", k % 2)], writes=[(tagp + "wb", nb % 2, c4)])
                        k += 1
                    for tt in range(8):
                        ps = psF[n % 2]
                        tm = tmp[n % 2]
                        for c in range(NCH):
                            S.op("pe", lambda e, c=c, ps=ps, wb=wb, tt=tt: e.matmul(
                                ps[:, :], lhsT=actT[:, c, tt * 128:(tt + 1) * 128], rhs=wb[:, c, :],
                                start=(c == 0), stop=(c == NCH - 1)),
                                reads=[(tagp + "wb", nb % 2, c // 4), (tagp + "act", c)], writes=[(tagp + "ps", n % 2)])
                        S.op("act", lambda e, ps=ps, tm=tm: e.copy(out=tm[:], in_=ps[:, :]),
                             reads=[(tagp + "ps", n % 2)], writes=[(tagp + "tmp", n % 2)])
                        S.op("dve", lambda e, tm=tm, tt=tt, nb=nb: e.tensor_tensor(
                            out=xres[:, tt, nb * 512:(nb + 1) * 512], in0=xres[:, tt, nb * 512:(nb + 1) * 512],
                            in1=tm[:], op=ALU.add), reads=[(tagp + "tmp", n % 2), ("xres", tt)], writes=[("xres", tt)])
                        n += 1
                S.barrier()

        add_proj("F", mixT, w_out_d)
        with ExitStack() as gs:
            def sb(name, shape, dt=F32):
                return gs.enter_context(nc.sbuf_tensor("G" + name, shape, dt))

            def pst(name, dt=F32, n=512):
                return gs.enter_context(nc.psum_tensor("Gps" + name, [128, n], dt))

            P = dict(tag="G", xb=sb("xb", [128, D], BF16), ss=sb("ss", [128, 1]),
                     psT=[pst(f"T{i}", BF16, 1024) for i in range(2)])
            gbc = sb("gbc", [128, D])
            mnT = sb("mnT", [128, NCH, 256], BF16); kmT = sb("kmT", [128, NCH, 256], BF16)
            vm = sb("vm", [128, 2, D], BF16)
            hT = moT[:].rearrange("p a b -> p (a b)").rearrange("p (c t) -> p c t", c=NCH)
            qT = aqT[:].rearrange("p a b -> p (a b)").rearrange("p (c t) -> p c t", c=NCH)
            stage_all = sb("stage", [128, 2, 1024])
            stages = [stage_all[:, i, :].rearrange("p (c n) -> p c n", c=4) for i in range(2)]
            wbs = [sb(f"wbk{i}", [128, NCH, 256], BF16) for i in range(2)]
            kc = [0]
            onesb = sb("onesb", [128, 128], BF16)
            PTm = [sb(f"PT{i}", [128, 512], BF16) for i in range(2)]
            rD = sb("rD", [128, 512]); tO = sb("tO", [128, 512])
            psQ = [pst("Q0"), pst("Q1")]; psS = [pst("S0"), pst("S1")]; psO = pst("O"); psD = pst("D")
            S.op("pool", lambda e: e.memset(onesb[:], 1.0), writes=["Gonesb"])
            S.dma("sp", gbc[:], g_mem_d[0:1, :].partition_broadcast(128), writes=["Ggbc"])
            stage_flat = stage_all[:].rearrange("p a b -> p (a b)")
            for mt in range(2):
                S.dma("sp", stage_flat, mem_d[mt * 128:(mt + 1) * 128, :], writes=[("Gstage", 0), ("Gstage", 1)])
                norm_sb(P, stage_flat, [("Gstage", 0), ("Gstage", 1)], gbc, mnT, mt * 128, ("GmnT", mt))
            mnkeys = [("GmnT", 0), ("GmnT", 1)]
            nq_ = 0
            for b2 in range(8):
                wb, wk_ = load_wblk2("G", w_xk_d, b2, stages, wbs, kc)
                for e4 in range(2):
                    ec = b2 * 2 + e4
                    ps = psQ[nq_ % 2]; pk = ("GpsQ", nq_ % 2); nq_ += 1
                    for c in range(NCH):
                        S.op("pe", lambda e, c=c, e4=e4, ps=ps: e.matmul(ps[:, 0:256], lhsT=wb[:, c, e4 * 128:(e4 + 1) * 128],
                                                                         rhs=mnT[:, c, :], start=(c == 0), stop=(c == NCH - 1)),
                             reads=wk_ + mnkeys, writes=[pk])
                    S.op("act", lambda e, ec=ec, ps=ps: e.copy(out=kmT[:, ec, :], in_=ps[:, 0:256]), reads=[pk],
                         writes=[("GkmT", ec)])
            for b2 in range(8):
                wb, wk_ = load_wblk2("G", w_xv_d, b2, stages, wbs, kc)
                for mt in range(2):
                    ps = psQ[nq_ % 2]; pk = ("GpsQ", nq_ % 2); nq_ += 1
                    for c in range(NCH):
                        S.op("pe", lambda e, c=c, mt=mt, ps=ps: e.matmul(ps[:, 0:256], lhsT=mnT[:, c, mt * 128:(mt + 1) * 128],
                                                                         rhs=wb[:, c, :], start=(c == 0), stop=(c == NCH - 1)),
                             reads=wk_ + mnkeys, writes=[pk])
                    S.op("act", lambda e, mt=mt, b2=b2, ps=ps: e.copy(out=vm[:, mt, b2 * 256:(b2 + 1) * 256], in_=ps[:, 0:256]),
                         reads=[pk], writes=[("Gvm", mt, b2)])
            S.dma("sp", gbc[:], g_cross_d[0:1, :].partition_broadcast(128), writes=["Ggbc"])
            SCX = 512.0 ** -0.5
            kmkeys = [("GkmT", ec) for ec in range(NCH)]
            vmkeys = [("Gvm", mt, b2) for mt in range(2) for b2 in range(8)]
            for st in range(2):
                for sub in range(4):
                    tt = st * 4 + sub
                    norm_sb(P, xres[:, tt, :], [("xres", tt)], gbc, hT, sub * 128, "GhT")
                for b2 in range(8):
                    wb, wk_ = load_wblk2("G", w_xq_d, b2, stages, wbs, kc)
                    for e4 in range(2):
                        ec = b2 * 2 + e4
                        ps = psQ[nq_ % 2]; pk = ("GpsQ", nq_ % 2); nq_ += 1
                        for c in range(NCH):
                            S.op("pe", lambda e, c=c, e4=e4, ps=ps: e.matmul(ps[:, :], lhsT=wb[:, c, e4 * 128:(e4 + 1) * 128],
                                                                             rhs=hT[:, c, :], start=(c == 0), stop=(c == NCH - 1)),
                                 reads=wk_ + ["GhT"], writes=[pk])
                        if ec % 2 == 0:
                            S.op("act", lambda e, ec=ec, ps=ps: e.copy(out=qT[:, ec, :], in_=ps[:, :]), reads=[pk],
                                 writes=[("GqT", ec)])
                        else:
                            S.op("dve", lambda e, ec=ec, ps=ps: e.tensor_copy(qT[:, ec, :], ps[:, :]), reads=[pk],
                                 writes=[("GqT", ec)])
                for hd in range(4):
                    ecs = range(hd * 4, hd * 4 + 4)
                    qk = [("GqT", ec) for ec in ecs]
                    for mt in range(2):
                        for i_, ec in enumerate(ecs):
                            S.op("pe", lambda e, mt=mt, ec=ec, i_=i_: e.matmul(
                                psS[mt][:, :], lhsT=kmT[:, ec, mt * 128:(mt + 1) * 128], rhs=qT[:, ec, :],
                                start=(i_ == 0), stop=(i_ == 3)), reads=qk + kmkeys, writes=[("GpsS", mt)])
                        S.op("act", lambda e, mt=mt: e.activation(out=PTm[mt][:], in_=psS[mt][:, :], func=AF.Exp, scale=SCX),
                             reads=[("GpsS", mt)], writes=[("GPT", mt)])
                    ptk = [("GPT", 0), ("GPT", 1)]
                    for mt in range(2):
                        S.op("pe", lambda e, mt=mt: e.matmul(psD[:, :], lhsT=onesb[:], rhs=PTm[mt][:],
                                                             start=(mt == 0), stop=(mt == 1)),
                             reads=ptk + ["Gonesb"], writes=["GpsD"])
                    S.op("act", lambda e: e.copy(out=rD[:], in_=psD[:, :]), reads=["GpsD"], writes=["GrD"])
                    S.op("dve", lambda e: e.reciprocal(rD[:], rD[:]), reads=["GrD"], writes=["GrD"])
                    for ec in ecs:
                        for mt in range(2):
                            S.op("pe", lambda e, mt=mt, ec=ec: e.matmul(psO[:, :], lhsT=vm[:, mt, ec * 128:(ec + 1) * 128],
                                                                        rhs=PTm[mt][:], start=(mt == 0), stop=(mt == 1)),
                                 reads=ptk + vmkeys, writes=["GpsO"])
                        S.op("act", lambda e: e.copy(out=tO[:], in_=psO[:, :]), reads=["GpsO"], writes=["GtO"])
                        S.op("dve", lambda e, ec=ec: e.tensor_tensor(out=mixT[:, ec, st * 512:(st + 1) * 512], in0=tO[:],
                                                                     in1=rD[:], op=ALU.mult),
                             reads=["GtO", "GrD"], writes=[("Gact", ec, st)])
            S.barrier()
        add_proj("G", mixT, w_xo_d)

        if dbg is not None and dbg["name"] == "xresG":
            S.dma("sp", dbg_out.rearrange("(t p) n -> p t n", p=128), xres[:], writes=["dbgo"])
            S.barrier()
            return nc
        ids_all = es.enter_context(nc.sbuf_tensor("ids_all", [128, 8, 128], I32))
        gates_all = es.enter_context(nc.sbuf_tensor("gates_all", [128, 8, 128], F32))
        with ExitStack() as hs:
            def sb(name, shape, dt=F32):
                return hs.enter_context(nc.sbuf_tensor("H" + name, shape, dt))

            def pst(name, dt=F32, n=512):
                return hs.enter_context(nc.psum_tensor("Hps" + name, [128, n], dt))

            P = dict(tag="H", xb=sb("xb", [128, D], BF16), ss=sb("ss", [128, 1]),
                     psT=[pst(f"T{i}", BF16, 1024) for i in range(2)])
            gbc = sb("gbc", [128, D])
            S.dma("sp", gbc[:], g_ffn_d[0:1, :].partition_broadcast(128), writes=["Hgbc"])
            hT = moT[:].rearrange("p a b -> p (a b)").rearrange("p (c t) -> p c t", c=NCH)
            qT = aqT[:].rearrange("p a b -> p (a b)").rearrange("p (c t) -> p c t", c=NCH)
            stage_all = sb("stage", [128, 2, 1024])
            stages = [stage_all[:, i, :].rearrange("p (c n) -> p c n", c=4) for i in range(2)]
            wbs = [sb(f"wbk{i}", [128, NCH, 256], BF16) for i in range(2)]
            kc = [0]
            skT = sb("skT", [128, 16, 128], BF16)
            stage4 = stage_all[:].rearrange("p a b -> p (a b)").rearrange("p (a b) -> p a b", a=4)
            S.dma("sp", stage4, skT_d[:, :, :].rearrange("p (a b) k -> p a (b k)", a=4),
                  writes=[("Hstage", 0), ("Hstage", 1)])
            S.op("dve", lambda e: e.tensor_copy(skT[:].rearrange("p (a b) k -> p a (b k)", a=4), stage4),
                 reads=[("Hstage", 0), ("Hstage", 1)], writes=["HskT"])
            sc = sb("sc", [128, 16, 128]); wk1 = sb("wk1", [128, 128])
            ts = sb("ts", [128, 16, 16]); ti = sb("ti", [128, 16, 16], U32); tif = sb("tif", [128, 16, 16])
            cand = sb("cand", [128, 16, 16]); cid = sb("cid", [128, 16, 16]); wk2 = sb("wk2", [128, 256])
            junk = sb("junk", [128, 256]); bs = sb("bs", [128, 16]); negm = sb("negm", [128, 1])
            ge = sb("ge", [128, 16]); zz = sb("zz", [128, 1]); idf = sb("idf", [128, 128])
            pos = sb("pos", [128, 16], U32); posf = sb("posf", [128, 16]); iota_t = sb("iota", [128, 256])
            S.dma("sp", iota_t[:], iota_d[0:1, :].partition_broadcast(128), writes=["Hiota"])
            psQ = [pst("Q0"), pst("Q1")]; psC = [pst("C0"), pst("C1")]
            nq_ = 0
            for st in range(2):
                for sub in range(4):
                    tt = st * 4 + sub
                    norm_sb(P, xres[:, tt, :], [("xres", tt)], gbc, hT, sub * 128, "HhT")
                for b2 in range(8):
                    wb, wk_ = load_wblk2("H", w_pq_d, b2, stages, wbs, kc)
                    for e4 in range(2):
                        j = b2 * 2 + e4
                        ps = psQ[nq_ % 2]; pk = ("HpsQ", nq_ % 2); nq_ += 1
                        for c in range(NCH):
                            S.op("pe", lambda e, c=c, e4=e4, ps=ps: e.matmul(ps[:, :], lhsT=wb[:, c, e4 * 128:(e4 + 1) * 128],
                                                                             rhs=hT[:, c, :], start=(c == 0), stop=(c == NCH - 1)),
                                 reads=wk_ + ["HhT"], writes=[pk])
                        if j % 2 == 0:
                            S.op("act", lambda e, j=j, ps=ps: e.copy(out=qT[:, j, :], in_=ps[:, :]), reads=[pk],
                                 writes=[("HqT", j)])
                        else:
                            S.op("dve", lambda e, j=j, ps=ps: e.tensor_copy(qT[:, j, :], ps[:, :]), reads=[pk],
                                 writes=[("HqT", j)])
                for sub in range(4):
                    tt = st * 4 + sub
                    for jb in range(4):
                        pc = psC[jb % 2]; pck = ("HpsC", jb % 2)
                        for jj in range(4):
                            j = jb * 4 + jj
                            S.op("pe", lambda e, j=j, jj=jj, pc=pc: e.matmul(
                                pc[:, jj * 128:(jj + 1) * 128], lhsT=qT[:, j, sub * 128:(sub + 1) * 128], rhs=skT[:, j, :],
                                start=True, stop=True), reads=[("HqT", j), "HskT"], writes=[pck])
                        S.op("act", lambda e, jb=jb, pc=pc: e.copy(
                            out=sc[:, jb * 4:(jb + 1) * 4, :], in_=pc[:, :].rearrange("p (a k) -> p a k", a=4)),
                            reads=[pck], writes=[("Hsc", jb)])
                    for j in range(16):
                        sk_ = ("Hsc", j // 4)
                        S.op("dve", lambda e, j=j: e.max(out=ts[:, j, 0:8], in_=sc[:, j, :]), reads=[sk_], writes=[("Hts", j, 0)])
                        S.op("dve", lambda e, j=j: e.match_replace(out=wk1[:], in_to_replace=ts[:, j, 0:8],
                                                                   in_values=sc[:, j, :], imm_value=-1e30),
                             reads=[sk_, ("Hts", j, 0)], writes=["Hwk1"])
                        S.op("dve", lambda e, j=j: e.max(out=ts[:, j, 8:16], in_=wk1[:]), reads=["Hwk1"], writes=[("Hts", j, 1)])
                        S.op("dve", lambda e, j=j: e.max_index(out=ti[:, j, 0:8], in_max=ts[:, j, 0:8], in_values=sc[:, j, :]),
                             reads=[sk_, ("Hts", j, 0)], writes=[("Hti", j, 0)])
                        S.op("dve", lambda e, j=j: e.max_index(out=ti[:, j, 8:16], in_max=ts[:, j, 8:16], in_values=wk1[:]),
                             reads=["Hwk1", ("Hts", j, 1)], writes=[("Hti", j, 1)])
                    S.op("dve", lambda e: e.tensor_copy(tif[:], ti[:]),
                         reads=[("Hti", j, q) for j in range(16) for q in range(2)], writes=["Htif"])
                    for h in range(8):
                        j0, j1 = 2 * h, 2 * h + 1
                        tsk = [("Hts", j0, 0), ("Hts", j0, 1), ("Hts", j1, 0), ("Hts", j1, 1)]
                        S.op("dve", lambda e: e.tensor_tensor(
                            out=cand[:], in0=ts[:, j0, :].unsqueeze(2).to_broadcast([128, 16, 16]),
                            in1=ts[:, j1, :].unsqueeze(1).to_broadcast([128, 16, 16]), op=ALU.add),
                            reads=tsk, writes=["Hcand"])
                        S.op("dve", lambda e: e.scalar_tensor_tensor(
                            out=cid[:], in0=tif[:, j0, :].unsqueeze(2).to_broadcast([128, 16, 16]), scalar=128.0,
                            in1=tif[:, j1, :].unsqueeze(1).to_broadcast([128, 16, 16]), op0=ALU.mult, op1=ALU.add),
                            reads=["Htif"], writes=["Hcid"])
                        candf = cand[:].rearrange("p a b -> p (a b)")
                        cidf = cid[:].rearrange("p a b -> p (a b)")
                        S.op("dve", lambda e: e.max(out=bs[:, 0:8], in_=candf), reads=["Hcand"], writes=["Hbs0"])
                        S.op("dve", lambda e: e.match_replace(out=wk2[:], in_to_replace=bs[:, 0:8], in_values=candf,
                                                              imm_value=-1e30), reads=["Hcand", "Hbs0"], writes=["Hwk2"])
                        S.op("dve", lambda e: e.max(out=bs[:, 8:16], in_=wk2[:]), reads=["Hwk2"], writes=["Hbs1"])
                        S.op("dve", lambda e: e.max_index(out=pos[:, 0:8], in_max=bs[:, 0:8], in_values=candf),
                             reads=["Hcand", "Hbs0"], writes=["Hpos0"])
                        S.op("dve", lambda e: e.max_index(out=pos[:, 8:16], in_max=bs[:, 8:16], in_values=wk2[:]),
                             reads=["Hwk2", "Hbs1"], writes=["Hpos1"])
                        S.op("dve", lambda e: e.tensor_copy(posf[:], pos[:]), reads=["Hpos0", "Hpos1"], writes=["Hposf"])
                        for k in range(16):
                            S.op("dve", lambda e, k=k: e.scalar_tensor_tensor(
                                out=junk[:], in0=iota_t[:], scalar=posf[:, k:k + 1], in1=cidf, op0=ALU.is_equal, op1=ALU.mult,
                                accum_out=idf[:, h * 16 + k:h * 16 + k + 1]),
                                reads=["Hiota", "Hcid", "Hposf"], writes=["Hjunk", ("Hidf", h)])
                        S.op("dve", lambda e: e.tensor_scalar_mul(negm[:], bs[:, 0:1], -1.0), reads=["Hbs0"], writes=["Hnegm"])
                        S.op("act", lambda e: e.activation(out=ge[:], in_=bs[:], func=AF.Exp, bias=negm[:, 0:1], accum_out=zz[:]),
                             reads=["Hbs0", "Hbs1", "Hnegm"], writes=["Hge", "Hzz"])
                        S.op("dve", lambda e: e.reciprocal(zz[:], zz[:]), reads=["Hzz"], writes=["Hzz"])
                        S.op("dve", lambda e: e.tensor_scalar(out=gates_all[:, tt, h * 16:(h + 1) * 16], in0=ge[:],
                                                              scalar1=zz[:, 0:1], scalar2=None, op0=ALU.mult),
                             reads=["Hge", "Hzz"], writes=[("gates", tt, h)])
                    S.op("dve", lambda e: e.tensor_copy(ids_all[:, tt, :], idf[:]),
                         reads=[("Hidf", h) for h in range(8)], writes=[("ids", tt)])
            S.barrier()

        if dbg is not None and dbg["name"] == "peer_ids":
            S.dma("sp", dbg_out[0:128, :].rearrange("p (t k) -> p t k", t=8), ids_all[:].bitcast(F32), writes=["dbgo"])
            S.dma("sp", dbg_out[128:256, :].rearrange("p (t k) -> p t k", t=8), gates_all[:], writes=["dbgo2"])
            S.barrier()
            return nc

        with ExitStack() as hs:
            def sb(name, shape, dt=F32):
                return hs.enter_context(nc.sbuf_tensor("Hb" + name, shape, dt))

            P = dict(tag="Hb", xb=sb("xb", [128, D], BF16), ss=sb("ss", [128, 1]), psT=None)
            gbc = sb("gbc", [128, D])
            S.dma("sp", gbc[:], g_ffn_d[0:1, :].partition_broadcast(128), writes=["Hbgbc"])
            xn2 = [sb(f"xn{i}", [128, D]) for i in range(2)]
            actc = sb("actc", [128, 128]); cf = sb("cf", [128, 128]); tmpv = sb("tmpv", [128, D])
            dg = [sb(f"dg{i}", [128, 128], BF16) for i in range(4)]
            NG = 8
            fence_t = sb("fence", [128, 1])
            gall = mixT[:].rearrange("p a b -> p (a b)")
            gb_ = [gall[:, i * 2 * D:(i + 1) * 2 * D] for i in range(4)]
            gb_ += [moT[:].rearrange("p a b -> p (a b)")[:, i * 2 * D:(i + 1) * 2 * D] for i in range(2)]
            gb_ += [aqT[:].rearrange("p a b -> p (a b)")[:, i * 2 * D:(i + 1) * 2 * D] for i in range(2)]
            psV = [[hs.enter_context(nc.psum_tensor(f"HbpsV{q}_{n}", [128, 512], F32)) for n in range(4)] for q in range(2)]
            ng = 0
            for tt in range(8):
                xn = xn2[tt % 2]
                P["tag"] = f"Hb{tt % 2}"
                S.op("act", lambda e, tt=tt: e.activation(out=P["xb"][:], in_=xres[:, tt, :], func=AF.Square,
                                                          scale=float(D) ** -0.5, accum_out=P["ss"][:]),
                     reads=[("xres", tt)], writes=["Hbxb", "Hbss"])
                S.op("dve", lambda e: e.tensor_scalar_add(P["ss"][:], P["ss"][:], EPS), reads=["Hbss"], writes=["Hbss"])
                S.op("act", lambda e: e.sqrt(P["ss"][:], P["ss"][:]), reads=["Hbss"], writes=["Hbss"])
                S.op("dve", lambda e: e.reciprocal(P["ss"][:], P["ss"][:]), reads=["Hbss"], writes=["Hbss"])
                S.op("dve", lambda e, tt=tt, xn=xn: e.scalar_tensor_tensor(out=xn[:], in0=xres[:, tt, :], scalar=P["ss"][:, 0:1],
                                                                    in1=gbc[:], op0=ALU.mult, op1=ALU.mult),
                     reads=[("xres", tt), "Hbss", "Hbgbc"], writes=[("Hbxn", tt % 2)])
                pv = psV[tt % 2]

                def chain(slot, bq):
                    b_, q_ = bq
                    S.op("act", lambda e: e.activation(out=cf[:, slot:slot + 1], in_=actc[:, slot:slot + 1], func=AF.Gelu),
                         reads=[("Hbact", slot), "Hbfence"], writes=[("Hbcf", slot)])
                    S.op("act", lambda e: e.mul(cf[:, slot:slot + 1], cf[:, slot:slot + 1], gates_all[:, tt, slot:slot + 1]),
                         reads=[("Hbcf", slot)], writes=[("Hbcf", slot)])
                    S.op("act", lambda e: e.activation(out=dg[q_][:], in_=ident[:], func=AF.Copy, scale=cf[:, slot:slot + 1]),
                         reads=[("Hbcf", slot)], writes=[("Hbdg", q_)])
                    for n in range(4):
                        S.op("pe", lambda e, n=n: e.matmul(
                            pv[n][:, :], lhsT=dg[q_][:], rhs=gb_[b_][:, D + n * 512:D + (n + 1) * 512],
                            start=(slot == 0), stop=(slot == 127)),
                            reads=[("Hbdg", q_), ("gbuf", b_)], writes=[("HbpsV", tt % 2, n)])

                prev = None
                for slot in range(128):
                    b_ = ng % NG
                    q_ = ng % 4
                    ng += 1
                    S.dma("pool", None, None, reads=[("ids", tt)], writes=[("gbuf", b_)],
                          fn=lambda e, b_=b_, slot=slot, tt=tt: e.indirect_dma_start(
                              out=gb_[b_], out_offset=None, in_=uv_d[:, :],
                              in_offset=bass.IndirectOffsetOnAxis(ap=ids_all[:, tt, slot:slot + 1], axis=0)))
                    S.op("dve", lambda e, b_=b_, slot=slot, xn=xn: e.scalar_tensor_tensor(
                        out=P["xb"][:], in0=gb_[b_][:, 0:D], scalar=1.0, in1=xn[:], op0=ALU.mult, op1=ALU.mult,
                        accum_out=actc[:, slot:slot + 1]),
                        reads=[("gbuf", b_), ("Hbxn", tt % 2)], writes=["Hbxb", ("Hbact", slot), "Hbfence"])
                    if prev is not None:
                        chain(slot - 1, prev)
                    prev = (b_, q_)
                S.op("dve", lambda e: e.tensor_copy(fence_t[:], actc[:, 0:1]), reads=[("Hbact", 0)], writes=["Hbfence"])
                chain(127, prev)
                if dbg is not None and dbg["name"] == "peer_cf":
                    S.dma("sp", dbg_out[:, tt * 128:(tt + 1) * 128], cf[:], reads=[("Hbcf", s_) for s_ in range(128)], writes=[("dbgo", tt)])
                    S.dma("sp", dbg_out[:, 1024 + tt * 128:1024 + (tt + 1) * 128], actc[:], reads=[("Hbact", s_) for s_ in range(128)], writes=[("dbgo2", tt)])
                S.op("act", lambda e, pv=pv: e.copy(out=tmpv[:, 0:512], in_=pv[0][:, :]),
                     reads=[("HbpsV", tt % 2, 0)], writes=[("Hbtmp", 0)])
                S.op("act", lambda e, pv=pv: e.copy(out=tmpv[:, 512:1024], in_=pv[1][:, :]),
                     reads=[("HbpsV", tt % 2, 1)], writes=[("Hbtmp", 1)])
                S.op("act", lambda e, pv=pv: e.copy(out=tmpv[:, 1024:1536], in_=pv[2][:, :]),
                     reads=[("HbpsV", tt % 2, 2)], writes=[("Hbtmp", 2)])
                S.op("act", lambda e, pv=pv: e.copy(out=tmpv[:, 1536:2048], in_=pv[3][:, :]),
                     reads=[("HbpsV", tt % 2, 3)], writes=[("Hbtmp", 3)])
                S.op("dve", lambda e, tt=tt: e.tensor_tensor(out=xres[:, tt, :], in0=xres[:, tt, :], in1=tmpv[:], op=ALU.add),
                     reads=[("Hbtmp", n) for n in range(4)] + [("xres", tt)], writes=[("xres", tt)])
            S.barrier()

        with ExitStack() as fs:
            gfb = fs.enter_context(nc.sbuf_tensor("gfb", [128, D], F32))
            junk = fs.enter_context(nc.sbuf_tensor("Ijunk", [128, D], BF16))
            ss = fs.enter_context(nc.sbuf_tensor("Iss", [128, 1], F32))
            ot = [fs.enter_context(nc.sbuf_tensor(f"Iot{i}", [128, D], F32)) for i in range(2)]
            S.dma("sp", gfb[:], g_final_d[0:1, :].partition_broadcast(128), writes=["gfb"])
            for tt in range(8):
                o_ = ot[tt % 2]
                S.op("act", lambda e, tt=tt: e.activation(out=junk[:], in_=xres[:, tt, :], func=AF.Square,
                                                          scale=float(D) ** -0.5, accum_out=ss[:]),
                     reads=[("xres", tt)], writes=["Ijunk", "Iss"])
                S.op("dve", lambda e: e.tensor_scalar_add(ss[:], ss[:], EPS), reads=["Iss"], writes=["Iss"])
                S.op("act", lambda e: e.sqrt(ss[:], ss[:]), reads=["Iss"], writes=["Iss"])
                S.op("dve", lambda e: e.reciprocal(ss[:], ss[:]), reads=["Iss"], writes=["Iss"])
                S.op("dve", lambda e, tt=tt, o_=o_: e.scalar_tensor_tensor(out=o_[:], in0=xres[:, tt, :], scalar=ss[:, 0:1],
                                                                    in1=gfb[:], op0=ALU.mult, op1=ALU.mult),
                     reads=[("xres", tt), "Iss", "gfb"], writes=[("Iot", tt % 2)])
                S.dma("sp", out_d[tt * 128:(tt + 1) * 128, :], o_[:], reads=[("Iot", tt % 2)], writes=[("outd", tt)])
            S.barrier()

        S.barrier()
        print("instructions", S.nins, "waits", S.nwaits, S.cnt, S.epoch, S.nsem)
    return nc


def rope_tables(j):
    half = 64
    inv = (10000.0 ** (-np.arange(half, dtype=np.float32) / half)).astype(np.float32)
    pos = (1024 * j - NPRE + np.arange(WIN)).astype(np.float32)
    ang = pos[None, :] * inv[:, None]
    cos = np.cos(ang).astype(np.float32)
    sin = np.sin(ang).astype(np.float32)
    return np.concatenate([cos, cos], 0), np.concatenate([sin, sin], 0)


def make_in_maps(inputs):
    x = np.asarray(inputs["x"], np.float32)
    ident = np.eye(128, dtype=np.float32).astype(ml_dtypes.bfloat16)
    R = np.zeros((128, 128), np.float32)
    for p in range(64):
        R[p, p + 64] = -1.0
        R[p + 64, p] = 1.0
    rotT = np.ascontiguousarray(R.T).astype(ml_dtypes.bfloat16)
    tri = np.triu(np.ones((128, 128), np.float32))
    trl = np.tril(np.ones((128, 128), np.float32))
    f32 = lambda a: np.asarray(a, np.float32)
    skT_h = np.ascontiguousarray(f32(inputs["sub_keys"])[0].reshape(16, 128, 128).transpose(2, 0, 1))
    u_tab = f32(inputs["u_tab"])[0]
    v_tab = f32(inputs["v_tab"])[0]
    maps = []
    for c in range(8):
        b, j = c // 4, c % 4
        xw = np.zeros((WIN, D), np.float32)
        lo = 1024 * j - NPRE
        src0 = max(lo, 0)
        xw[src0 - lo:] = x[b, src0:1024 * j + 1024]
        cos, sin = rope_tables(j)
        pmv = np.where(np.arange(WIN) + lo >= 0, 0.0, -30000.0).astype(np.float32)
        kbA = np.zeros((128, 48), np.float32)
        gi = 0
        for Q in range(2):
            for (d, r, sub) in [(1, 0, s_) for s_ in range(4)] + [(4, r_, 0) for r_ in range(4)] + [(16, r_, 0) for r_ in range(16)]:
                u0 = 2048 + 512 * Q + r + 128 * sub
                u = u0 - 128 * d + d * np.arange(128)
                kbA[:, gi] = np.where(1024 * j - 2048 + u >= 0, 0.0, -30000.0)
                gi += 1
        m = {
            "x_win": xw,
            "w_in": np.ascontiguousarray(np.asarray(inputs["w_in"], np.float32)[0]),
            "g_mix": np.asarray(inputs["g_mix"], np.float32).reshape(1, D),
            "cosT": cos, "sinT": sin, "ident": ident, "rotT": rotT,
            "cw_h": np.ascontiguousarray(f32(inputs["conv_w"])[0].T.reshape(8, 128, 4).transpose(1, 0, 2)),
            "cb_h": np.ascontiguousarray(f32(inputs["conv_b"])[0].reshape(8, 128).T),
            "gm_h": np.ascontiguousarray(f32(inputs["g_mhead"])[0].reshape(8, 128).T),
            "gb_h": np.concatenate([f32(inputs["b_mi"])[0], f32(inputs["b_mf"])[0]]).reshape(1, 8),
            "pm_h": np.ascontiguousarray(pmv.reshape(32, 128).T),
            "tri_f": tri,
            "w_mq": f32(inputs["w_mq"])[0], "w_mk": f32(inputs["w_mk"])[0],
            "w_out": f32(inputs["w_out"])[0], "g_final": f32(inputs["g_final"]).reshape(1, D),
            "mem_b": np.ascontiguousarray(f32(inputs["mem"])[b]),
            "g_mem": f32(inputs["g_mem"]).reshape(1, D), "g_cross": f32(inputs["g_cross"]).reshape(1, D),
            "w_xq": f32(inputs["w_xq"])[0], "w_xk": f32(inputs["w_xk"])[0], "w_xv": f32(inputs["w_xv"])[0],
            "w_xo": f32(inputs["w_xo"])[0],
            "g_ffn": f32(inputs["g_ffn"]).reshape(1, D), "w_pq": f32(inputs["w_pq"])[0],
            "skT_h": skT_h, "iota256": np.arange(256, dtype=np.float32).reshape(1, 256), "u_tab": u_tab, "v_tab": v_tab,
            "kbA": kbA, "ga_h": np.ascontiguousarray(f32(inputs["g_ahead"])[0].T), "trl_f": trl,
        }
        maps.append(m)
    return maps


def kernel(**inputs):
    nc = build()
    maps = make_in_maps(inputs)
    res = run_bass_kernel_spmd(nc, maps, core_ids=list(range(8)))
    out = np.zeros((2, 4096, D), np.float32)
    for c in range(8):
        b, j = c // 4, c % 4
        out[b, 1024 * j:1024 * j + 1024] = res.results[c]["out"]
    return out
```

```python
import numpy as np
import ml_dtypes
from contextlib import ExitStack
import concourse.bass as bass
import concourse.mybir as mybir
from concourse.bass_utils import run_bass_kernel_spmd

F32 = mybir.dt.float32
BF16 = mybir.dt.bfloat16
I32 = mybir.dt.int32
U32 = mybir.dt.uint32
ALU = mybir.AluOpType
AF = mybir.ActivationFunctionType
AX = mybir.AxisListType

D = 2048
NCH = 16
WIN = 4096
OWN = 1024
NPRE = WIN - OWN
EPS = 1e-6
IN_COLS = 6152
NDS = 40


class Sched:
    LIM = 3500
    DLIM = 240

    def __init__(self, nc, es):
        self.nc = nc
        self.es = es
        self.engs = {"pe": nc.tensor, "act": nc.scalar, "dve": nc.vector, "pool": nc.gpsimd, "sp": nc.sync}
        self.nsem = 0
        self.h = {}
        self.epoch = {k: 0 for k in self.engs}
        self.cnt = {k: 0 for k in self.engs}
        for k in self.engs:
            self.h[(k, 0)] = self._new()
        self.seen = {k: {} for k in self.engs}
        self.dver = [0] * NDS
        self.dcnt = [0] * NDS
        for i in range(NDS):
            self.h[("d", i, 0)] = self._new()
        self.dnext = {"sp": 0, "pool": NDS // 2, "act": 0}
        self.lastw = {}
        self.rd = {}
        self.nwaits = 0
        self.nins = 0

    def _new(self):
        self.nsem += 1
        return self.es.enter_context(self.nc.semaphore(f"s{self.nsem}"))

    def _wait(self, eng, sk, val):
        if val <= 0:
            return
        if self.seen[eng].get(sk, 0) >= val:
            return
        self.seen[eng][sk] = val
        self.engs[eng].wait_ge(self.h[sk], val)
        self.nwaits += 1

    def _deps(self, eng, reads, writes):
        need = {}

        def add(t, war=False):
            if t is None:
                return
            sk, val = t
            if sk[0] == eng and eng == "pe":
                return
            if need.get(sk, 0) < val:
                need[sk] = val

        for k in reads:
            add(self.lastw.get(k))
        for k in writes:
            add(self.lastw.get(k))
            for t in self.rd.get(k, ()):
                add(t, war=True)
        for sk, val in need.items():
            self._wait(eng, sk, val)

    def _commit(self, ticket, reads, writes):
        for k in reads:
            self.rd.setdefault(k, []).append(ticket)
        for k in writes:
            self.lastw[k] = ticket
            self.rd[k] = []

    def op(self, eng, fn, reads=(), writes=()):
        self._deps(eng, reads, writes)
        if self.cnt[eng] >= self.LIM:
            self.epoch[eng] += 1
            self.cnt[eng] = 0
            self.h[(eng, self.epoch[eng])] = self._new()
        sk = (eng, self.epoch[eng])
        ins = fn(self.engs[eng])
        ins.then_inc(self.h[sk], 1)
        self.cnt[eng] += 1
        self.nins += 1
        self._commit((sk, self.cnt[eng]), reads, writes)

    def dma(self, q, out, in_, reads=(), writes=(), fn=None):
        i = self.dnext[q]
        half = NDS // 2
        base = half if q == "pool" else 0
        self.dnext[q] = base + (i - base + 1) % half
        sk = ("d", i, self.dver[i])
        self._wait(q, sk, 16 * self.dcnt[i])
        if self.dcnt[i] >= self.DLIM:
            self.dver[i] += 1
            self.dcnt[i] = 0
            sk = ("d", i, self.dver[i])
            self.h[sk] = self._new()
        self._deps(q, reads, writes)
        if fn is None:
            ins = self.engs[q].dma_start(out=out, in_=in_)
        else:
            ins = fn(self.engs[q])
        ins.then_inc(self.h[sk], 16)
        self.dcnt[i] += 1
        self.nins += 1
        self._commit((sk, 16 * self.dcnt[i]), reads, writes)

    def barrier(self):
        for e in self.engs:
            for e2 in self.engs:
                if e2 != e:
                    if self.cnt[e2] > 0:
                        self._wait(e, (e2, self.epoch[e2]), self.cnt[e2])
                    elif self.epoch[e2] > 0:
                        self._wait(e, (e2, self.epoch[e2] - 1), self.LIM)
            for i in range(NDS):
                if self.dcnt[i] > 0:
                    self._wait(e, ("d", i, self.dver[i]), 16 * self.dcnt[i])
                elif self.dver[i] > 0:
                    self._wait(e, ("d", i, self.dver[i] - 1), 16 * self.DLIM)
        self.lastw = {}
        self.rd = {}


def bcast_free(ap_col, n):
    return ap_col.to_broadcast([ap_col.shape[0], n])


class K:
    pass


def load_weight_bf16(S, nc, es, w_dram, col0, ncols, name, stage, stage_key):
    wt = es.enter_context(nc.sbuf_tensor(name, [128, NCH, ncols], BF16))
    wv = w_dram.rearrange("(c p) n -> p c n", p=128)
    step = max(1, 2048 // ncols)
    c = 0
    i = 0
    while c < NCH:
        nn = min(step, NCH - c)
        sl = i % 2
        st = stage[sl]
        sv = st[:, 0:nn * ncols].rearrange("p (c n) -> p c n", n=ncols)
        S.dma("sp", sv, wv[:, c:c + nn, col0:col0 + ncols], writes=[(stage_key, sl)])
        eng = "pool" if i % 2 == 0 else "dve"
        S.op(eng, lambda e, c=c, nn=nn, sv=sv: e.tensor_copy(wt[:, c:c + nn, :], sv),
             reads=[(stage_key, sl)], writes=[(name, c + q) for q in range(nn)])
        c += nn
        i += 1
    return wt


def build(dbg=None):
    nc = bass.Bass("TRN2", target_bir_lowering=False)
    try:
        nc.allow_low_precision("bf16 matmuls with fp32 accumulation")
    except Exception:
        pass

    def din(name, shape, dt=F32):
        return nc.dram_tensor(name, list(shape), dt, kind="ExternalInput").ap()

    def dint(name, shape, dt=BF16):
        return nc.dram_tensor(name, list(shape), dt, kind="Internal").ap()

    x_win = din("x_win", [WIN, D])
    w_in = din("w_in", [D, IN_COLS])
    g_mix = din("g_mix", [1, D])
    cosT = din("cosT", [128, WIN])
    sinT = din("sinT", [128, WIN])
    ident_d = din("ident", [128, 128], BF16)
    rot_d = din("rotT", [128, 128], BF16)

    cw_d = din("cw_h", [128, 8, 4])
    cb_d = din("cb_h", [128, 8])
    gm_d = din("gm_h", [128, 8])
    gb_d = din("gb_h", [1, 8])
    pm_d = din("pm_h", [128, 32])
    tri_d = din("tri_f", [128, 128])
    w_mq_d = din("w_mq", [4, 256, 256])
    w_mk_d = din("w_mk", [4, 256, 256])

    kbA_d = din("kbA", [128, 48])
    ga_d = din("ga_h", [128, 8])
    trl_d = din("trl_f", [128, 128])
    w_out_d = din("w_out", [D, D])
    mem_d = din("mem_b", [256, D])
    g_ffn_d = din("g_ffn", [1, D])
    iota_d = din("iota256", [1, 256])
    w_pq_d = din("w_pq", [D, D])
    skT_d = din("skT_h", [128, 16, 128])
    u_tab_d = din("u_tab", [16384, D])
    v_tab_d = din("v_tab", [16384, D])
    g_mem_d = din("g_mem", [1, D])
    g_cross_d = din("g_cross", [1, D])
    w_xq_d = din("w_xq", [D, D]); w_xk_d = din("w_xk", [D, D]); w_xv_d = din("w_xv", [D, D]); w_xo_d = din("w_xo", [D, D])
    g_final_d = din("g_final", [1, D])
    out_d = nc.dram_tensor("out", [OWN, D], F32, kind="ExternalOutput").ap()
    dbg_out = None
    if dbg is not None:
        dbg_out = nc.dram_tensor("dbg", list(dbg["shape"]), dbg.get("dt", F32), kind="ExternalOutput").ap()

    uv_d = dint("uv_tab", [16384, 2 * D])
    s_minT = dint("s_minT", [1024, WIN])
    s_mv = dint("s_mv", [WIN, 1024])
    s_gates = dint("s_gates", [WIN, 8], F32)
    s_akT = dint("s_akT", [1024, WIN])
    s_av = dint("s_av", [WIN, 1024])

    with ExitStack() as es:
        es.enter_context(nc.allow_low_precision(reason="bf16 operands, fp32 accumulation"))
        S = Sched(nc, es)
        ident = es.enter_context(nc.sbuf_tensor("identb", [128, 128], BF16))
        rotT = es.enter_context(nc.sbuf_tensor("rotTb", [128, 128], BF16))
        S.dma("sp", ident[:], ident_d[:, :], writes=["ident"])
        S.dma("sp", rotT[:], rot_d[:, :], writes=["rotT"])
        es_mix = es
        es_mix2 = es
        moT = es_mix.enter_context(nc.sbuf_tensor("moT", [128, 8, OWN], BF16))
        aqT = es_mix.enter_context(nc.sbuf_tensor("aqT", [128, 8, OWN], BF16))


        def norm_tile(pes, x_src_ap, gbc, hT, col0, xkey, part="both"):
            xt, xb, ss, rstd, psT = pes["xt"], pes["xb"], pes["ss"], pes["rstd"], pes["psT"]
            if part in ("both", "pre"):
                norm_tile_pre(pes, x_src_ap, gbc)
            if part in ("both", "post"):
                norm_tile_post(pes, hT, col0, xkey)

        def norm_tile_pre(pes, x_src_ap, gbc):
            xt, xb, ss, rstd, psT = pes["xt"], pes["xb"], pes["ss"], pes["rstd"], pes["psT"]
            S.dma("sp", xt[:], x_src_ap, writes=["xt"])
            S.op("act", lambda e: e.activation(out=xb[:], in_=xt[:], func=AF.Square, scale=float(D) ** -0.5,
                                               accum_out=ss[:]),
                 reads=["xt"], writes=["xb", "ss"])
            S.op("dve", lambda e: e.tensor_scalar_add(rstd[:], ss[:], EPS), reads=["ss"], writes=["rstd"])
            S.op("act", lambda e: e.sqrt(rstd[:], rstd[:]), reads=["rstd"], writes=["rstd"])
            S.op("dve", lambda e: e.reciprocal(rstd[:], rstd[:]), reads=["rstd"], writes=["rstd"])
            S.op("dve", lambda e: e.scalar_tensor_tensor(out=xb[:], in0=xt[:], scalar=rstd[:, 0:1], in1=gbc[:],
                                                         op0=ALU.mult, op1=ALU.mult),
                 reads=["xt", "rstd", "gbc"], writes=["xb"])

        def norm_tile_post(pes, hT, col0, xkey):
            xt, xb, ss, rstd, psT = pes["xt"], pes["xb"], pes["ss"], pes["rstd"], pes["psT"]
            for half in range(2):
                for j in range(8):
                    c = half * 8 + j
                    S.op("pe", lambda e, c=c, j=j, half=half: e.transpose(
                        psT[half][:, j * 128:(j + 1) * 128], xb[:, c * 128:(c + 1) * 128], ident[:]),
                        reads=["xb", "ident"], writes=[("psT", half)])
                eng = "act" if half == 0 else "dve"
                if eng == "act":
                    S.op("act", lambda e, half=half: e.copy(
                        out=hT[:, half * 8:(half + 1) * 8, col0:col0 + 128],
                        in_=psT[half][:].rearrange("p (c t) -> p c t", c=8)),
                        reads=[("psT", half)], writes=[xkey])
                else:
                    S.op("dve", lambda e, half=half: e.tensor_copy(
                        hT[:, half * 8:(half + 1) * 8, col0:col0 + 128],
                        psT[half][:].rearrange("p (c t) -> p c t", c=8)),
                        reads=[("psT", half)], writes=[xkey])

        def phase_proj(name, st_list, specs, gain_d):
            with ExitStack() as pes_:
                pes = {}
                pes["xt"] = pes_.enter_context(nc.sbuf_tensor(name + "xt", [128, D], F32))
                pes["xb"] = pes_.enter_context(nc.sbuf_tensor(name + "xb", [128, D], BF16))
                pes["ss"] = pes_.enter_context(nc.sbuf_tensor(name + "ss", [128, 1], F32))
                pes["rstd"] = pes_.enter_context(nc.sbuf_tensor(name + "rstd", [128, 1], F32))
                pes["psT"] = [pes_.enter_context(nc.psum_tensor(name + f"psT{i}", [128, 1024], BF16)) for i in range(2)]
                gbc = pes_.enter_context(nc.sbuf_tensor(name + "gbc", [128, D], F32))
                S.dma("sp", gbc[:], gain_d.partition_broadcast(128), writes=["gbc"])
                hT = [pes_.enter_context(nc.sbuf_tensor(name + f"hT{i}", [128, NCH, 512], BF16)) for i in range(2)]
                stage = [pes_.enter_context(nc.sbuf_tensor(name + f"wst{i}", [128, 2048], F32)) for i in range(2)]
                psM = [pes_.enter_context(nc.psum_tensor(name + f"psM{i}", [128, 512], F32)) for i in range(4)]
                for sub in range(4):
                    t0_ = st_list[0] * 512 + sub * 128
                    norm_tile(pes, x_win[t0_:t0_ + 128, :], gbc, hT[0], sub * 128, ("hT", 0))
                ws = []
                for si, sp in enumerate(specs):
                    ws.append(load_weight_bf16(S, nc, pes_, w_in, sp["col0"], sp["ncols"], f"{name}w{si}", stage,
                                               name + "wst"))
                env = dict(pes_=pes_, psM=psM)
                for sp in specs:
                    if "setup" in sp:
                        sp["setup"](env)
                pmc = [0]

                def emit_norm(sti, sub, part="both"):
                    t0 = st_list[sti] * 512 + sub * 128
                    norm_tile(pes, x_win[t0:t0 + 128, :], gbc, hT[sti % 2], sub * 128, ("hT", sti % 2), part=part)

                def grp_f(st, h, hkey, sp, w, wkeys, cc):
                    ps = psM[pmc[0] % 4]
                    pk = ("psM", pmc[0] % 4)
                    pmc[0] += 1
                    for c in range(NCH):
                        S.op("pe", lambda e, c=c: e.matmul(
                            ps[:, :], lhsT=w[:, c, cc * 128:(cc + 1) * 128], rhs=h[:, c, :],
                            start=(c == 0), stop=(c == NCH - 1)), reads=[hkey] + wkeys, writes=[pk])
                    sp["evac"](env, st, cc, ps, pk)

                def grp_t(st, h, hkey, sp, w, wkeys, sub, nb):
                    n0 = nb * 512
                    nn = min(512, sp["ncols"] - n0)
                    ps = psM[pmc[0] % 4]
                    pk = ("psM", pmc[0] % 4)
                    pmc[0] += 1
                    for c in range(NCH):
                        S.op("pe", lambda e, c=c: e.matmul(
                            ps[:, 0:nn], lhsT=h[:, c, sub * 128:(sub + 1) * 128],
                            rhs=w[:, c, n0:n0 + nn], start=(c == 0), stop=(c == NCH - 1)),
                            reads=[hkey] + wkeys, writes=[pk])
                    sp["evac"](env, st, sub, nb, ps, pk, nn)

                for sti, st in enumerate(st_list):
                    h = hT[sti % 2]
                    hkey = ("hT", sti % 2)
                    groups = []
                    for si, sp in enumerate(specs):
                        w = ws[si]
                        wkeys = [(f"{name}w{si}", c) for c in range(NCH)]
                        if sp["kind"] == "f":
                            for cc in range(sp["ncols"] // 128):
                                groups.append(lambda st=st, h=h, hkey=hkey, sp=sp, w=w, wkeys=wkeys, cc=cc:
                                              grp_f(st, h, hkey, sp, w, wkeys, cc))
                        else:
                            for sub in range(4):
                                for nb in range((sp["ncols"] + 511) // 512):
                                    groups.append(lambda st=st, h=h, hkey=hkey, sp=sp, w=w, wkeys=wkeys, sub=sub, nb=nb:
                                                  grp_t(st, h, hkey, sp, w, wkeys, sub, nb))
                    per = (len(groups) + 3) // 4
                    for k in range(4):
                        if sti + 1 < len(st_list):
                            emit_norm(sti + 1, k, "pre")
                        for g in groups[k * per:(k + 1) * per]:
                            g()
                        if sti + 1 < len(st_list):
                            emit_norm(sti + 1, k, "post")
                    for sp in specs:
                        if "flush" in sp:
                            sp["flush"](env, st)
                S.barrier()

        def mk_f_to_dram(dst, nchunks, tag, rope=False, sb_dst=None, st0=0, func=None):
            st_ = {}

            def setup(env):
                if sb_dst is None:
                    st_["stg"] = [env["pes_"].enter_context(nc.sbuf_tensor(f"{tag}stg{i}", [128, nchunks, 512], BF16))
                                  for i in range(2)]
                st_["n"] = 0
                if func == "sigexp":
                    st_["sg"] = env["pes_"].enter_context(nc.sbuf_tensor(f"{tag}sg", [128, 512], F32))
                if rope:
                    st_["kb"] = env["pes_"].enter_context(nc.sbuf_tensor(f"{tag}kb", [128, 512], BF16))
                    st_["t1"] = env["pes_"].enter_context(nc.sbuf_tensor(f"{tag}t1", [128, 512], F32))
                    st_["t2"] = env["pes_"].enter_context(nc.sbuf_tensor(f"{tag}t2", [128, 512], F32))
                    st_["psR"] = env["pes_"].enter_context(nc.psum_tensor(f"{tag}psR", [128, 512], F32))
                    st_["cos"] = env["pes_"].enter_context(nc.sbuf_tensor(f"{tag}cos", [128, 512], F32))
                    st_["sin"] = env["pes_"].enter_context(nc.sbuf_tensor(f"{tag}sin", [128, 512], F32))

            def evac(env, st, cc, ps, pk):
                if sb_dst is None:
                    sl = st_["n"] % 2
                    oap = st_["stg"][sl][:, cc, :]
                    okey = (tag + "stg", sl)
                else:
                    oap = sb_dst[:, cc, (st - st0) * 512:(st - st0 + 1) * 512]
                    okey = (tag + "sb", cc, st)
                if not rope:
                    if func == "sigexp":
                        sg = st_["sg"]
                        S.op("act", lambda e: e.activation(out=sg[:], in_=ps[:, :], func=AF.Exp, scale=-1.0),
                             reads=[pk], writes=[tag + "sg"])
                        S.op("dve", lambda e: e.tensor_scalar_add(sg[:], sg[:], 1.0), reads=[tag + "sg"], writes=[tag + "sg"])
                        S.op("dve", lambda e: e.reciprocal(oap, sg[:]), reads=[tag + "sg"], writes=[okey])
                    elif func is not None:
                        S.op("act", lambda e: e.activation(out=oap, in_=ps[:, :], func=func), reads=[pk], writes=[okey])
                    elif cc % 2 == 0:
                        S.op("act", lambda e: e.copy(out=oap, in_=ps[:, :]), reads=[pk], writes=[okey])
                    else:
                        S.op("dve", lambda e: e.tensor_copy(oap, ps[:, :]), reads=[pk], writes=[okey])
                else:
                    kb, t1, t2, psR = st_["kb"], st_["t1"], st_["t2"], st_["psR"]
                    p0 = 0
                    if cc == 0:
                        S.dma("sp", st_["cos"][:], cosT[:, st * 512:(st + 1) * 512], writes=[tag + "cos"])
                        S.dma("sp", st_["sin"][:], sinT[:, st * 512:(st + 1) * 512], writes=[tag + "sin"])
                    S.op("act", lambda e: e.copy(out=kb[:], in_=ps[:, :]), reads=[pk], writes=[tag + "kb"])
                    S.op("pe", lambda e: e.matmul(psR[:, :], lhsT=rotT[:], rhs=kb[:], start=True, stop=True),
                         reads=[tag + "kb", "rotT"], writes=[tag + "psR"])
                    S.op("dve", lambda e: e.tensor_tensor(out=t1[:], in0=kb[:], in1=st_["cos"][:, 0:512], op=ALU.mult),
                         reads=[tag + "kb", tag + "cos"], writes=[tag + "t1"])
                    S.op("act", lambda e: e.copy(out=t2[:], in_=psR[:, :]), reads=[tag + "psR"], writes=[tag + "t2"])
                    S.op("dve", lambda e: e.tensor_tensor(out=t2[:], in0=t2[:], in1=st_["sin"][:, 0:512], op=ALU.mult),
                         reads=[tag + "t2", tag + "sin"], writes=[tag + "t2"])
                    S.op("dve", lambda e: e.tensor_tensor(out=oap, in0=t1[:], in1=t2[:], op=ALU.add),
                         reads=[tag + "t1", tag + "t2"], writes=[okey])

            def flush(env, st):
                if sb_dst is None:
                    sl = st_["n"] % 2
                    stg = st_["stg"][sl]
                    S.dma("pool", dst.rearrange("(c p) t -> p c t", p=128)[:, :, st * 512:(st + 1) * 512], stg[:],
                          reads=[(tag + "stg", sl)], writes=[(tag + "dram", st)])
                st_["n"] += 1

            return dict(setup=setup, evac=evac, flush=flush)

        def mk_t_to_dram(dst, ncols, tag, dt=BF16):
            st_ = {}

            def setup(env):
                st_["stg"] = [env["pes_"].enter_context(nc.sbuf_tensor(f"{tag}stg{i}", [128, 4, ncols], dt))
                              for i in range(2)]
                st_["n"] = 0

            def evac(env, st, sub, nb, ps, pk, nn):
                sl = st_["n"] % 2
                stg = st_["stg"][sl]
                skey = (tag + "stg", sl)
                if (sub + nb) % 2 == 0:
                    S.op("act", lambda e: e.copy(out=stg[:, sub, nb * 512:nb * 512 + nn], in_=ps[:, 0:nn]),
                         reads=[pk], writes=[skey])
                else:
                    S.op("dve", lambda e: e.tensor_copy(stg[:, sub, nb * 512:nb * 512 + nn], ps[:, 0:nn]),
                         reads=[pk], writes=[skey])

            def flush(env, st):
                sl = st_["n"] % 2
                stg = st_["stg"][sl]
                S.dma("pool", dst[st * 512:(st + 1) * 512, :].rearrange("(s p) n -> p s n", p=128), stg[:],
                      reads=[(tag + "stg", sl)], writes=[(tag + "dram", st)])
                st_["n"] += 1

            return dict(setup=setup, evac=evac, flush=flush)

        spA = [dict(kind="f", col0=0, ncols=1024, **mk_f_to_dram(s_minT, 8, "Amin")),
               dict(kind="t", col0=1024, ncols=1024, **mk_t_to_dram(s_mv, 1024, "Amv")),
               dict(kind="t", col0=3072, ncols=8, **mk_t_to_dram(s_gates, 8, "Ag", F32))]
        phase_proj("A", list(range(8)), spA, g_mix[0:1, :])

        if dbg is not None and dbg["name"] == "stopA":
            S.barrier()
            return nc
        spB = [dict(kind="f", col0=4104, ncols=1024, **mk_f_to_dram(s_akT, 8, "Bak", rope=True)),
               dict(kind="t", col0=5128, ncols=1024, **mk_t_to_dram(s_av, 1024, "Bav"))]
        phase_proj("B", list(range(2, 8)), spB, g_mix[0:1, :])
        if dbg is not None and dbg["name"] == "stopB":
            S.barrier()
            return nc
        spC = [dict(kind="f", col0=2048, ncols=1024, **mk_f_to_dram(None, 8, "Cmo", sb_dst=moT, st0=6, func="sigexp")),
               dict(kind="f", col0=3080, ncols=1024, **mk_f_to_dram(None, 8, "Caq", rope=True, sb_dst=aqT, st0=6))]
        phase_proj("C", [6, 7], spC, g_mix[0:1, :])

        mixT = es_mix2.enter_context(nc.sbuf_tensor("mixT", [128, NCH, OWN], BF16))
        print("after C", S.cnt, S.epoch, S.nsem)
        if dbg is not None and dbg["name"] == "aqT":
            S.dma("sp", dbg_out[0:1024, :].rearrange("(c p) t -> p c t", p=128), aqT[:], writes=["dbgo"])
            S.dma("sp", dbg_out[1024:2048, :].rearrange("(c p) t -> p c t", p=128), moT[:], writes=["dbgo2"])
            S.barrier()
            return nc
        with ExitStack() as ds:
            def sb(name, shape, dt=F32):
                return ds.enter_context(nc.sbuf_tensor("D" + name, shape, dt))

            def pst(name):
                return ds.enter_context(nc.psum_tensor("Dps" + name, [128, 512], F32))

            cw = sb("cw", [128, 8, 4]); cbias = sb("cbias", [128, 8]); gm = sb("gm", [128, 8]); gb = sb("gb", [128, 8])
            pm = sb("pm", [128, 32]); tri_f = sb("trif", [128, 128]); ones_f = sb("onesf", [128, 128])
            S.dma("sp", cw[:], cw_d[:, :, :], writes=["cw"])
            S.dma("sp", cbias[:], cb_d[:, :], writes=["cbias"])
            S.dma("sp", gm[:], gm_d[:, :], writes=["gm"])
            S.dma("sp", gb[:], gb_d[0:1, :].partition_broadcast(128), writes=["gb"])
            S.dma("sp", pm[:], pm_d[:, :], writes=["pm"])
            S.dma("sp", tri_f[:], tri_d[:, :], writes=["tri_f"])
            S.op("pool", lambda e: e.memset(ones_f[:], 1.0), writes=["ones_f"])
            wstg = sb("wstg", [128, 4, 2, 256])
            wq = sb("wq", [128, 4, 2, 256], BF16); wk = sb("wk", [128, 4, 2, 256], BF16)
            S.dma("sp", wstg[:], w_mq_d.rearrange("h (c p) e -> p h c e", p=128), writes=["wstg"])
            S.op("dve", lambda e: e.tensor_copy(wq[:], wstg[:]), reads=["wstg"], writes=["wq"])
            S.dma("sp", wstg[:], w_mk_d.rearrange("h (c p) e -> p h c e", p=128), reads=[], writes=["wstg"])
            S.op("dve", lambda e: e.tensor_copy(wk[:], wstg[:]), reads=["wstg"], writes=["wk"])
            Sst = [sb(f"Sst{h}", [128, 2, 384]) for h in range(4)]
            Sbf = [sb(f"Sbf{h}", [128, 2, 384], BF16) for h in range(4)]
            for h in range(4):
                S.op("pool", lambda e, h=h: e.memset(Sst[h][:], 0.0), writes=[("Sst", h, 0), ("Sst", h, 1)])
                S.op("pool", lambda e, h=h: e.memset(Sbf[h][:], 0.0), writes=[("Sbf", h)])
            xin = [sb(f"xin{i}", [128, 8, 131], BF16) for i in range(2)]
            vaug = [sb(f"vaug{i}", [128, 4, 384], BF16) for i in range(2)]
            gt = [sb(f"gt{i}", [128, 8]) for i in range(2)]
            cT = [sb(f"cT{i}", [128, 8, 128], BF16) for i in range(2)]
            for i in range(2):
                S.op("pool", lambda e, i=i: e.memset(vaug[i][:, :, 256:384], 1.0), writes=[("vaug", i)])
            acc = [sb(f"acc{i}", [128, 128]) for i in range(2)]
            g2 = sb("g2", [128, 8]); sp4 = sb("sp4", [128, 4]); w4 = sb("w4", [128, 4]); spb = sb("spb", [128, 4, 128])
            two = lambda nm, shape, dt=F32: [sb(f"{nm}{i}", shape, dt) for i in range(2)]
            kp = two("kp", [128, 256], BF16); kTb = two("kTb", [128, 2, 128], BF16); qTb = two("qTb", [128, 2, 128], BF16)
            clampT = two("clampT", [128, 128]); PT = two("PT", [128, 128], BF16); dd = two("dd", [128, 128])
            hn = two("hn", [128, 2, 128]); sq = two("sq", [128, 2, 128]); rr = two("rr", [128, 128]); tmpo = two("tmpo", [128, 128])
            dec = two("dec", [128, 1]); dSs = two("dSs", [128, 2, 384])
            psk = pst("k"); pskT = pst("kT"); psqT = pst("qT"); psS = pst("S"); psO = pst("O"); psMisc = pst("M")
            psdS = [pst("dS0"), pst("dS1")]
            minT_v = s_minT.rearrange("(c p) t -> p c t", p=128)

            def conv_tile(et):
                rs = slice(et * 128, (et + 1) * 128)
                S.dma("pool", uv_d[rs, 0:D], u_tab_d[rs, :], writes=[("uvd", et, 0)])
                S.dma("pool", uv_d[rs, D:2 * D], v_tab_d[rs, :], writes=[("uvd", et, 1)])

            for ck in range(32):
                own = ck >= 24
                par = ck % 2
                t0 = ck * 128
                xin_, vaug_, gt_, cT_ = xin[par], vaug[par], gt[par], cT[par]
                if ck == 0:
                    S.op("pool", lambda e: e.memset(xin_[:, :, 0:3], 0.0), writes=[("xin", par)])
                    S.dma("sp", xin_[:, :, 3:131], minT_v[:, :, 0:128], writes=[("xin", par)])
                else:
                    S.dma("sp", xin_[:, :, 0:131], minT_v[:, :, t0 - 3:t0 + 128], writes=[("xin", par)])
                S.dma("sp", vaug_[:, :, 0:256], s_mv[t0:t0 + 128, :].rearrange("p (h e) -> p h e", h=4),
                      writes=[("vaug", par)])
                S.dma("sp", gt_[:], s_gates[t0:t0 + 128, :], writes=[("gt", par)])
                for et in range(ck * 4, ck * 4 + 4):
                    conv_tile(et)
                S.op("dve", lambda e: e.tensor_tensor(out=g2[:], in0=gt_[:], in1=gb[:], op=ALU.add),
                     reads=[("gt", par), "gb"], writes=["g2"])
                S.op("dve", lambda e: e.tensor_scalar(out=g2[:, 0:4], in0=g2[:, 0:4], scalar1=pm[:, ck:ck + 1],
                                                      scalar2=None, op0=ALU.add), reads=["g2", "pm"], writes=["g2"])
                S.op("act", lambda e: e.activation(out=sp4[:], in_=g2[:, 4:8], func=AF.Exp, scale=-1.0),
                     reads=["g2"], writes=["sp4"])
                S.op("dve", lambda e: e.tensor_scalar_add(sp4[:], sp4[:], 1.0), reads=["sp4"], writes=["sp4"])
                S.op("act", lambda e: e.activation(out=sp4[:], in_=sp4[:], func=AF.Ln), reads=["sp4"], writes=["sp4"])
                S.op("pe", lambda e: e.matmul(psMisc[:, 256:260], lhsT=tri_f[:], rhs=sp4[:], start=True, stop=True),
                     reads=["tri_f", "sp4"], writes=["ps_csp"])
                S.op("act", lambda e: e.copy(out=w4[:], in_=psMisc[:, 256:260]), reads=["ps_csp"], writes=["w4"])
                S.op("dve", lambda e: e.tensor_tensor(out=w4[:], in0=g2[:, 0:4], in1=w4[:], op=ALU.add),
                     reads=["g2", "w4"], writes=["w4"])
                S.op("act", lambda e: e.activation(out=w4[:], in_=w4[:], func=AF.Exp), reads=["w4"], writes=["w4"])
                S.op("dve", lambda e: e.tensor_scalar_mul(w4[:], w4[:], 0.0625), reads=["w4"], writes=["w4"])
                S.op("dve", lambda e: e.tensor_copy(spb[:], sp4[:].unsqueeze(2).to_broadcast([128, 4, 128])),
                     reads=["sp4"], writes=["spb"])
                for c8 in range(8):
                    a_ = acc[c8 % 2]
                    ak = ("acc", c8 % 2)
                    S.op("dve", lambda e, c8=c8, a_=a_: e.tensor_scalar(out=a_[:], in0=xin_[:, c8, 0:128],
                                                                        scalar1=cw[:, c8, 0:1], scalar2=None,
                                                                        op0=ALU.mult),
                         reads=[("xin", par), "cw"], writes=[ak])
                    for jj in range(1, 4):
                        S.op("dve", lambda e, c8=c8, a_=a_, jj=jj: e.scalar_tensor_tensor(
                            out=a_[:], in0=xin_[:, c8, jj:jj + 128], scalar=cw[:, c8, jj:jj + 1], in1=a_[:],
                            op0=ALU.mult, op1=ALU.add), reads=[("xin", par), "cw", ak], writes=[ak])
                    S.op("act", lambda e, c8=c8, a_=a_: e.activation(out=cT_[:, c8, :], in_=a_[:], func=AF.Silu,
                                                                     bias=cbias[:, c8:c8 + 1]),
                         reads=[ak, "cbias"], writes=[("cT", par, c8)])
                def head_gen(h):
                    hp = h % 2
                    kp_, kTb_, qTb_, clampT_, PT_, dd_ = kp[hp], kTb[hp], qTb[hp], clampT[hp], PT[hp], dd[hp]
                    hn_, sq_, rr_, tmpo_, dec_, dSs_ = hn[hp], sq[hp], rr[hp], tmpo[hp], dec[hp], dSs[hp]
                    ckeys = [("cT", par, 2 * h), ("cT", par, 2 * h + 1)]
                    for dc in range(2):
                        S.op("pe", lambda e, dc=dc: e.matmul(psk[:, 0:256], lhsT=cT_[:, 2 * h + dc, :], rhs=wk[:, h, dc, :],
                                                             start=(dc == 0), stop=(dc == 1)),
                             reads=ckeys + ["wk"], writes=["psk"])
                    S.op("dve", lambda e: e.tensor_scalar(out=kp_[:], in0=psk[:, 0:256], scalar1=w4[:, h:h + 1],
                                                          scalar2=None, op0=ALU.mult), reads=["psk", "w4"], writes=[("kp", hp)])
                    yield
                    S.op("pe", lambda e: e.matmul(psMisc[:, 0:128], lhsT=spb[:, h, :], rhs=tri_f[:], start=True, stop=True),
                         reads=["spb", "tri_f"], writes=["ps_cb"])
                    S.op("act", lambda e: e.activation(out=dec_[:], in_=psMisc[:, 127:128], func=AF.Exp, scale=-1.0),
                         reads=["ps_cb"], writes=[("dec", hp)])
                    if own:
                        o0 = (ck - 24) * 128
                        S.op("act", lambda e: e.activation(out=clampT_[:], in_=psMisc[:, 0:128], func=AF.Exp),
                             reads=["ps_cb"], writes=[("clampT", hp)])
                        yield
                        for ec in range(2):
                            for dc in range(2):
                                S.op("pe", lambda e, ec=ec, dc=dc: e.matmul(
                                    pskT[:, ec * 128:(ec + 1) * 128], lhsT=wk[:, h, dc, ec * 128:(ec + 1) * 128],
                                    rhs=cT_[:, 2 * h + dc, :], start=(dc == 0), stop=(dc == 1)),
                                    reads=ckeys + ["wk"], writes=["pskT"])
                        for ec in range(2):
                            for dc in range(2):
                                S.op("pe", lambda e, ec=ec, dc=dc: e.matmul(
                                    psqT[:, ec * 128:(ec + 1) * 128], lhsT=wq[:, h, dc, ec * 128:(ec + 1) * 128],
                                    rhs=cT_[:, 2 * h + dc, :], start=(dc == 0), stop=(dc == 1)),
                                    reads=ckeys + ["wq"], writes=["psqT"])
                        S.op("act", lambda e: e.copy(out=kTb_[:], in_=pskT[:, 0:256].rearrange("p (c t) -> p c t", c=2)),
                             reads=["pskT"], writes=[("kTb", hp)])
                        S.op("dve", lambda e: e.tensor_copy(qTb_[:], psqT[:, 0:256].rearrange("p (c t) -> p c t", c=2)),
                             reads=["psqT"], writes=[("qTb", hp)])
                        yield
                        for ec in range(2):
                            S.op("pe", lambda e, ec=ec: e.matmul(psS[:, 0:128], lhsT=kTb_[:, ec, :], rhs=qTb_[:, ec, :],
                                                                 start=(ec == 0), stop=(ec == 1)),
                                 reads=[("kTb", hp), ("qTb", hp)], writes=["psS"])
                        S.op("act", lambda e: e.copy(out=dd_[:], in_=psS[:, 0:128]), reads=["psS"], writes=[("dd", hp)])
                        S.op("dve", lambda e: e.scalar_tensor_tensor(out=PT_[:], in0=dd_[:], scalar=w4[:, h:h + 1],
                                                                     in1=tri_f[:], op0=ALU.mult, op1=ALU.mult),
                             reads=[("dd", hp), "w4", "tri_f"], writes=[("PT", hp)])
                        yield
                        for j in range(3):
                            S.op("pe", lambda e, j=j: e.matmul(psO[:, j * 128:(j + 1) * 128],
                                                               lhsT=vaug_[:, h, j * 128:(j + 1) * 128], rhs=PT_[:],
                                                               start=True, stop=False),
                                 reads=[("vaug", par), ("PT", hp)], writes=["psO"])
                            for dc in range(2):
                                S.op("pe", lambda e, j=j, dc=dc: e.matmul(
                                    psO[:, j * 128:(j + 1) * 128], lhsT=Sbf[h][:, dc, j * 128:(j + 1) * 128],
                                    rhs=qTb_[:, dc, :], start=False, stop=(dc == 1)),
                                    reads=[("Sbf", h), ("qTb", hp)], writes=["psO"])
                        S.op("act", lambda e: e.activation(out=dd_[:], in_=psO[:, 256:384], func=AF.Abs),
                             reads=["psO"], writes=[("dd", hp)])
                        S.op("act", lambda e: e.copy(out=hn_[:], in_=psO[:, 0:256].rearrange("p (c t) -> p c t", c=2)),
                             reads=["psO"], writes=[("hn", hp)])
                        yield
                        S.op("dve", lambda e: e.tensor_tensor(out=dd_[:], in0=dd_[:], in1=clampT_[:], op=ALU.max),
                             reads=[("dd", hp), ("clampT", hp)], writes=[("dd", hp)])
                        S.op("dve", lambda e: e.reciprocal(dd_[:], dd_[:]), reads=[("dd", hp)], writes=[("dd", hp)])
                        S.op("dve", lambda e: e.tensor_tensor(
                            out=hn_[:], in0=hn_[:], in1=dd_[:].unsqueeze(1).to_broadcast([128, 2, 128]), op=ALU.mult),
                            reads=[("hn", hp), ("dd", hp)], writes=[("hn", hp)])
                        S.op("act", lambda e: e.activation(out=sq_[:], in_=hn_[:], func=AF.Square), reads=[("hn", hp)], writes=[("sq", hp)])
                        for j in range(2):
                            S.op("pe", lambda e, j=j: e.matmul(psMisc[:, 128:256], lhsT=ones_f[:], rhs=sq_[:, j, :],
                                                               start=(j == 0), stop=(j == 1)),
                                 reads=[("sq", hp), "ones_f"], writes=["ps_n"])
                        S.op("dve", lambda e: e.tensor_scalar(out=rr_[:], in0=psMisc[:, 128:256], scalar1=1.0 / 256,
                                                              scalar2=EPS, op0=ALU.mult, op1=ALU.add),
                             reads=["ps_n"], writes=[("rr", hp)])
                        yield
                        S.op("act", lambda e: e.sqrt(rr_[:], rr_[:]), reads=[("rr", hp)], writes=[("rr", hp)])
                        S.op("dve", lambda e: e.reciprocal(rr_[:], rr_[:]), reads=[("rr", hp)], writes=[("rr", hp)])
                        for j in range(2):
                            S.op("dve", lambda e, j=j: e.scalar_tensor_tensor(
                                out=tmpo_[:], in0=hn_[:, j, :], scalar=gm[:, 2 * h + j:2 * h + j + 1], in1=rr_[:],
                                op0=ALU.mult, op1=ALU.mult), reads=[("hn", hp), "gm", ("rr", hp)], writes=[("tmpo", hp)])
                            S.op("pool", lambda e, j=j: e.tensor_tensor(
                                out=mixT[:, 2 * h + j, o0:o0 + 128], in0=tmpo_[:], in1=moT[:, 2 * h + j, o0:o0 + 128],
                                op=ALU.mult), reads=[("tmpo", hp)], writes=[("mixT", 2 * h + j, ck)])
                    yield
                    for dc in range(2):
                        S.op("pe", lambda e, dc=dc: e.matmul(psdS[dc][:, 0:384], lhsT=kp_[:, dc * 128:(dc + 1) * 128],
                                                             rhs=vaug_[:, h, :], start=True, stop=True),
                             reads=[("kp", hp), ("vaug", par)], writes=[("psdS", dc)])
                    for dc in range(2):
                        S.op("dve", lambda e, dc=dc: e.tensor_scalar(out=Sst[h][:, dc, :], in0=Sst[h][:, dc, :],
                                                                     scalar1=dec_[:, 0:1], scalar2=None, op0=ALU.mult),
                             reads=[("Sst", h, dc), ("dec", hp)], writes=[("Sst", h, dc)])
                        S.op("act", lambda e, dc=dc: e.copy(out=dSs_[:, dc, :], in_=psdS[dc][:, 0:384]),
                             reads=[("psdS", dc)], writes=[(("dSs", hp), dc)])
                        S.op("dve", lambda e, dc=dc: e.scalar_tensor_tensor(
                            out=Sst[h][:, dc, :], in0=dSs_[:, dc, :], scalar=dec_[:, 0:1], in1=Sst[h][:, dc, :],
                            op0=ALU.mult, op1=ALU.add), reads=[(("dSs", hp), dc), ("dec", hp), ("Sst", h, dc)],
                            writes=[("Sst", h, dc)])
                    S.op("act", lambda e: e.copy(out=Sbf[h][:], in_=Sst[h][:]),
                         reads=[("Sst", h, 0), ("Sst", h, 1)], writes=[("Sbf", h)])
                for pair in ((0, 1), (2, 3)):
                    gens = [head_gen(h) for h in pair]
                    while gens:
                        for g in list(gens):
                            try:
                                next(g)
                            except StopIteration:
                                gens.remove(g)
            S.barrier()

        with ExitStack() as ds:
            def sb(name, shape, dt=F32):
                return ds.enter_context(nc.sbuf_tensor("E" + name, shape, dt))

            def pst(name):
                return ds.enter_context(nc.psum_tensor("Eps" + name, [128, 512], F32))

            kT = sb("kT", [128, 8, NPRE], BF16)
            S.dma("sp", kT[:], s_akT.rearrange("(c p) t -> p c t", p=128)[:, :, 1024:WIN], writes=["kT"])
            accN = sb("accN", [128, 8, OWN]); accD = sb("accD", [128, 8, OWN])
            VA = [sb(f"VA{i}", [128, 1024], BF16) for i in range(2)]
            VB = [sb(f"VB{i}", [128, 1024], BF16) for i in range(2)]
            two = lambda nm, shape, dt=F32: [sb(f"{nm}{i}", shape, dt) for i in range(2)]
            PA = two("PA", [128, 128], BF16); PB = two("PB", [128, 128], BF16)
            eA = two("eA", [128, 128]); eB = two("eB", [128, 128]); tO = two("tO", [128, 128]); tD = two("tD", [128, 128])
            kbA = sb("kbA", [128, 48]); ga = sb("ga", [128, 8])
            trl = sb("trl", [128, 128]); tru = sb("tru", [128, 128]); ones_b = sb("onesb", [128, 128], BF16)
            ones_f2 = sb("onesf2", [128, 128])
            S.dma("sp", kbA[:], kbA_d[:, :], writes=["kbA"])
            S.dma("sp", ga[:], ga_d[:, :], writes=["ga"])
            S.dma("sp", trl[:], trl_d[:, :], writes=["trl"])
            S.dma("sp", tru[:], tri_d[:, :], writes=["tru"])
            S.op("pool", lambda e: e.memset(ones_b[:], 1.0), writes=["ones_b"])
            S.op("pool", lambda e: e.memset(ones_f2[:], 1.0), writes=["ones_f2"])
            psA = [pst("A0"), pst("A1")]; psB = [pst("B0"), pst("B1")]
            psO = [pst("O0"), pst("O1")]; psD = [pst("D0"), pst("D1")]
            psN = psA[0]
            SC = 128.0 ** -0.5
            gi = 0
            for Q in range(2):
                glist = [(1, 0, sub) for sub in range(4)] + [(4, r, 0) for r in range(4)] + [(16, r, 0) for r in range(16)]
                for (d, r, sub) in glist:
                    nq = 128 if d < 16 else 32
                    u0 = 2048 + 512 * Q + r + 128 * sub
                    i0 = 512 * Q + r + 128 * sub
                    uA = u0 - 128 * d
                    par = gi % 2
                    va, vb = VA[par], VB[par]
                    S.dma("sp", va[:], bass.AP(tensor=s_av.tensor, offset=(1024 + uA) * 1024, ap=[[d * 1024, 128], [1, 1024]]),
                          writes=[("VA", par)])
                    S.dma("sp", vb[0:nq, :], bass.AP(tensor=s_av.tensor, offset=(1024 + u0) * 1024, ap=[[d * 1024, nq], [1, 1024]]),
                          writes=[("VB", par)])
                    qsl = slice(i0, i0 + (nq - 1) * d + 1, d)
                    def st1(h):
                        p2 = h % 2
                        q_ap = aqT[:, h, qsl]
                        S.op("pe", lambda e: e.matmul(psA[p2][:, 0:nq], lhsT=kT[:, h, uA:uA + 127 * d + 1:d], rhs=q_ap,
                                                      start=True, stop=True), reads=["kT"], writes=[("psA", p2)])
                        S.op("pe", lambda e: e.matmul(psB[p2][0:nq, 0:nq], lhsT=kT[:, h, u0:u0 + (nq - 1) * d + 1:d], rhs=q_ap,
                                                      start=True, stop=True), reads=["kT"], writes=[("psB", p2)])

                    def st2(h):
                        p2 = h % 2
                        S.op("act", lambda e: e.activation(out=eA[p2][:, 0:nq], in_=psA[p2][:, 0:nq], func=AF.Exp, scale=SC,
                                                           bias=kbA[:, gi:gi + 1]), reads=[("psA", p2), "kbA"], writes=[("eA", p2)])
                        S.op("act", lambda e: e.activation(out=eB[p2][0:nq, 0:nq], in_=psB[p2][0:nq, 0:nq], func=AF.Exp, scale=SC),
                             reads=[("psB", p2)], writes=[("eB", p2)])
                        S.op("dve", lambda e: e.tensor_tensor(out=PA[p2][:, 0:nq], in0=eA[p2][:, 0:nq], in1=trl[:, 0:nq], op=ALU.mult),
                             reads=[("eA", p2), "trl"], writes=[("PA", p2)])
                        S.op("dve", lambda e: e.tensor_tensor(out=PB[p2][0:nq, 0:nq], in0=eB[p2][0:nq, 0:nq], in1=tru[0:nq, 0:nq],
                                                              op=ALU.mult), reads=[("eB", p2), "tru"], writes=[("PB", p2)])

                    def st3(h):
                        p2 = h % 2
                        S.op("pe", lambda e: e.matmul(psO[p2][:, 0:nq], lhsT=va[:, h * 128:(h + 1) * 128], rhs=PA[p2][:, 0:nq],
                                                      start=True, stop=False), reads=[("VA", par), ("PA", p2)], writes=[("psO", p2)])
                        S.op("pe", lambda e: e.matmul(psO[p2][:, 0:nq], lhsT=vb[0:nq, h * 128:(h + 1) * 128], rhs=PB[p2][0:nq, 0:nq],
                                                      start=False, stop=True), reads=[("VB", par), ("PB", p2)], writes=[("psO", p2)])
                        S.op("pe", lambda e: e.matmul(psD[p2][:, 0:nq], lhsT=ones_b[:, :], rhs=PA[p2][:, 0:nq],
                                                      start=True, stop=False), reads=["ones_b", ("PA", p2)], writes=[("psD", p2)])
                        S.op("pe", lambda e: e.matmul(psD[p2][:, 0:nq], lhsT=ones_b[0:nq, :], rhs=PB[p2][0:nq, 0:nq],
                                                      start=False, stop=True), reads=["ones_b", ("PB", p2)], writes=[("psD", p2)])

                    def st4(h):
                        p2 = h % 2
                        akey = ("acc", h, Q)
                        if d == 1:
                            S.op("act", lambda e: e.copy(out=accN[:, h, qsl], in_=psO[p2][:, 0:nq]), reads=[("psO", p2)], writes=[akey])
                            S.op("act", lambda e: e.copy(out=accD[:, h, qsl], in_=psD[p2][:, 0:nq]), reads=[("psD", p2)], writes=[akey])
                        else:
                            S.op("act", lambda e: e.copy(out=tO[p2][:, 0:nq], in_=psO[p2][:, 0:nq]), reads=[("psO", p2)], writes=[("tO", p2)])
                            S.op("act", lambda e: e.copy(out=tD[p2][:, 0:nq], in_=psD[p2][:, 0:nq]), reads=[("psD", p2)], writes=[("tD", p2)])
                            S.op("dve", lambda e: e.tensor_tensor(out=accN[:, h, qsl], in0=accN[:, h, qsl], in1=tO[p2][:, 0:nq],
                                                                  op=ALU.add), reads=[("tO", p2), akey], writes=[akey])
                            S.op("dve", lambda e: e.tensor_tensor(out=accD[:, h, qsl], in0=accD[:, h, qsl], in1=tD[p2][:, 0:nq],
                                                                  op=ALU.add), reads=[("tD", p2), akey], writes=[akey])

                    for i_ in range(9):
                        if i_ < 8:
                            st1(i_)
                            st2(i_)
                        if i_ >= 1:
                            st3(i_ - 1)
                            st4(i_ - 1)
                    gi += 1
            o5 = sb("o5", [128, 512]); sq5 = sb("sq5", [128, 512]); r5 = sb("r5", [128, 512])
            for h in range(8):
                for Q in range(2):
                    cs = slice(Q * 512, (Q + 1) * 512)
                    akey = ("acc", h, Q)
                    S.op("dve", lambda e: e.reciprocal(r5[:], accD[:, h, cs]), reads=[akey], writes=["r5"])
                    S.op("dve", lambda e: e.tensor_tensor(out=o5[:], in0=accN[:, h, cs], in1=r5[:], op=ALU.mult),
                         reads=[akey, "r5"], writes=["o5"])
                    S.op("act", lambda e: e.activation(out=sq5[:], in_=o5[:], func=AF.Square), reads=["o5"], writes=["sq5"])
                    S.op("pe", lambda e: e.matmul(psN[:, :], lhsT=ones_f2[:], rhs=sq5[:], start=True, stop=True),
                         reads=["ones_f2", "sq5"], writes=[("psA", 0)])
                    S.op("dve", lambda e: e.tensor_scalar(out=r5[:], in0=psN[:, :], scalar1=1.0 / 128, scalar2=EPS,
                                                          op0=ALU.mult, op1=ALU.add), reads=[("psA", 0)], writes=["r5"])
                    S.op("act", lambda e: e.sqrt(r5[:], r5[:]), reads=["r5"], writes=["r5"])
                    S.op("dve", lambda e: e.reciprocal(r5[:], r5[:]), reads=["r5"], writes=["r5"])
                    S.op("dve", lambda e: e.scalar_tensor_tensor(out=mixT[:, 8 + h, cs], in0=o5[:], scalar=ga[:, h:h + 1],
                                                                 in1=r5[:], op0=ALU.mult, op1=ALU.mult),
                         reads=["o5", "ga", "r5"], writes=[("mixT", 8 + h, Q)])
            S.barrier()

        if dbg is not None and dbg["name"] == "mixT":
            S.dma("sp", dbg_out.rearrange("(c p) t -> p c t", p=128), mixT[:], writes=["dbgo"])
            S.barrier()

        def norm_sb(P, x_ap, xkeys, gbc, hT, col0, hkey, xn=None):
            t = P["tag"]
            xb, ss, psT = P["xb"], P["ss"], P["psT"]
            S.op("act", lambda e: e.activation(out=xb[:], in_=x_ap, func=AF.Square, scale=float(D) ** -0.5,
                                               accum_out=ss[:]), reads=xkeys, writes=[t + "xb", t + "ss"])
            S.op("dve", lambda e: e.tensor_scalar_add(ss[:], ss[:], EPS), reads=[t + "ss"], writes=[t + "ss"])
            S.op("act", lambda e: e.sqrt(ss[:], ss[:]), reads=[t + "ss"], writes=[t + "ss"])
            S.op("dve", lambda e: e.reciprocal(ss[:], ss[:]), reads=[t + "ss"], writes=[t + "ss"])
            if xn is not None:
                S.op("dve", lambda e: e.scalar_tensor_tensor(out=xn[:], in0=x_ap, scalar=ss[:, 0:1], in1=gbc[:],
                                                             op0=ALU.mult, op1=ALU.mult),
                     reads=list(xkeys) + [t + "ss", t + "gbc"], writes=[t + "xn"])
                if hT is not None:
                    S.op("pool", lambda e: e.tensor_copy(xb[:], xn[:]), reads=[t + "xn"], writes=[t + "xb"])
            else:
                S.op("dve", lambda e: e.scalar_tensor_tensor(out=xb[:], in0=x_ap, scalar=ss[:, 0:1], in1=gbc[:],
                                                             op0=ALU.mult, op1=ALU.mult),
                     reads=list(xkeys) + [t + "ss", t + "gbc"], writes=[t + "xb"])
            if hT is None:
                return
            for half in range(2):
                for j in range(8):
                    c = half * 8 + j
                    S.op("pe", lambda e, c=c, j=j, half=half: e.transpose(
                        psT[half][:, j * 128:(j + 1) * 128], xb[:, c * 128:(c + 1) * 128], ident[:]),
                        reads=[t + "xb"], writes=[(t + "psT", half)])
                if half == 0:
                    S.op("act", lambda e, half=half: e.copy(
                        out=hT[:, half * 8:(half + 1) * 8, col0:col0 + 128],
                        in_=psT[half][:].rearrange("p (c t) -> p c t", c=8)),
                        reads=[(t + "psT", half)], writes=[hkey])
                else:
                    S.op("dve", lambda e, half=half: e.tensor_copy(
                        hT[:, half * 8:(half + 1) * 8, col0:col0 + 128],
                        psT[half][:].rearrange("p (c t) -> p c t", c=8)),
                        reads=[(t + "psT", half)], writes=[hkey])

        def load_wblk2(t, w_dram, b2, stages, wbs, kc):
            wv = w_dram.rearrange("(c p) n -> p c n", p=128)
            wb = wbs[b2 % 2]
            for c4 in range(4):
                S.dma("pool", wb[:, c4 * 4:(c4 + 1) * 4, :], wv[:, c4 * 4:(c4 + 1) * 4, b2 * 256:(b2 + 1) * 256],
                      writes=[(t + "wb", b2 % 2, c4)])
            return wb, [(t + "wb", b2 % 2, c4) for c4 in range(4)]

        def load_wblk(t, w_dram, nb, stage, wb):
            wv = w_dram.rearrange("(c p) n -> p c n", p=128)
            for c4 in range(4):
                S.dma("sp", stage[:], wv[:, c4 * 4:(c4 + 1) * 4, nb * 512:(nb + 1) * 512], writes=[t + "stage"])
                S.op("pool" if c4 % 2 == 0 else "dve",
                     lambda e, c4=c4: e.tensor_copy(wb[:, c4 * 4:(c4 + 1) * 4, :], stage[:]),
                     reads=[t + "stage"], writes=[(t + "wb", c4)])
            return [(t + "wb", c4) for c4 in range(4)]

        xres = es.enter_context(nc.sbuf_tensor("xres", [128, 8, D], F32))
        for tt in range(8):
            S.dma("sp", xres[:, tt, :], x_win[NPRE + tt * 128:NPRE + (tt + 1) * 128, :], writes=[("xres", tt)])

        def add_proj(tagp, actT, w_dram):
            with ExitStack() as fs:
                stage = [fs.enter_context(nc.sbuf_tensor(f"{tagp}st{i}", [128, 4, 512], F32)) for i in range(2)]
                wblk = [fs.enter_context(nc.sbuf_tensor(f"{tagp}wb{i}", [128, NCH, 512], BF16)) for i in range(2)]
                tmp = [fs.enter_context(nc.sbuf_tensor(f"{tagp}tmp{i}", [128, 512], F32)) for i in range(2)]
                psF = [fs.enter_context(nc.psum_tensor(f"{tagp}ps{i}", [128, 512], F32)) for i in range(2)]
                wv = w_dram.rearrange("(c p) n -> p c n", p=128)
                k = 0
                n = 0
                for nb in range(4):
                    wb = wblk[nb % 2]
                    for c4 in range(4):
                        st = stage[k % 2]
                        S.dma("sp", st[:], wv[:, c4 * 4:(c4 + 1) * 4, nb * 512:(nb + 1) * 512], writes=[(tagp + "st", k % 2)])
                        S.op("pool" if k % 2 == 0 else "dve",
                             lambda e, st=st, wb=wb, c4=c4: e.tensor_copy(wb[:, c4 * 4:(c4 + 1) * 4, :], st[:]),
                             reads=[(tagp + "st", k % 2)], writes=[(tagp + "wb", nb % 2, c4)])
                        k += 1
                    for tt in range(8):
                        ps = psF[n % 2]
                        tm = tmp[n % 2]
                        for c in range(NCH):
                            S.op("pe", lambda e, c=c, ps=ps, wb=wb, tt=tt: e.matmul(
                                ps[:, :], lhsT=actT[:, c, tt * 128:(tt + 1) * 128], rhs=wb[:, c, :],
                                start=(c == 0), stop=(c == NCH - 1)),
                                reads=[(tagp + "wb", nb % 2, c // 4), (tagp + "act", c)], writes=[(tagp + "ps", n % 2)])
                        S.op("act", lambda e, ps=ps, tm=tm: e.copy(out=tm[:], in_=ps[:, :]),
                             reads=[(tagp + "ps", n % 2)], writes=[(tagp + "tmp", n % 2)])
                        S.op("dve", lambda e, tm=tm, tt=tt, nb=nb: e.tensor_tensor(
                            out=xres[:, tt, nb * 512:(nb + 1) * 512], in0=xres[:, tt, nb * 512:(nb + 1) * 512],
                            in1=tm[:], op=ALU.add), reads=[(tagp + "tmp", n % 2), ("xres", tt)], writes=[("xres", tt)])
                        n += 1
                S.barrier()

        add_proj("F", mixT, w_out_d)
        with ExitStack() as gs:
            def sb(name, shape, dt=F32):
                return gs.enter_context(nc.sbuf_tensor("G" + name, shape, dt))

            def pst(name, dt=F32, n=512):
                return gs.enter_context(nc.psum_tensor("Gps" + name, [128, n], dt))

            P = dict(tag="G", xb=sb("xb", [128, D], BF16), ss=sb("ss", [128, 1]),
                     psT=[pst(f"T{i}", BF16, 1024) for i in range(2)])
            gbc = sb("gbc", [128, D])
            mnT = sb("mnT", [128, NCH, 256], BF16); kmT = sb("kmT", [128, NCH, 256], BF16)
            vm = sb("vm", [128, 2, D], BF16)
            hT = moT[:].rearrange("p a b -> p (a b)").rearrange("p (c t) -> p c t", c=NCH)
            qT = aqT[:].rearrange("p a b -> p (a b)").rearrange("p (c t) -> p c t", c=NCH)
            stage_all = sb("stage", [128, 2, 1024])
            stages = [stage_all[:, i, :].rearrange("p (c n) -> p c n", c=4) for i in range(2)]
            wbs = [sb(f"wbk{i}", [128, NCH, 256], BF16) for i in range(2)]
            kc = [0]
            onesb = sb("onesb", [128, 128], BF16)
            PTm = [sb(f"PT{i}", [128, 512], BF16) for i in range(2)]
            rD = sb("rD", [128, 512]); tO = sb("tO", [128, 512])
            psQ = [pst("Q0"), pst("Q1")]; psS = [pst("S0"), pst("S1")]; psO = pst("O"); psD = pst("D")
            S.op("pool", lambda e: e.memset(onesb[:], 1.0), writes=["Gonesb"])
            S.dma("sp", gbc[:], g_mem_d[0:1, :].partition_broadcast(128), writes=["Ggbc"])
            stage_flat = stage_all[:].rearrange("p a b -> p (a b)")
            for mt in range(2):
                S.dma("sp", stage_flat, mem_d[mt * 128:(mt + 1) * 128, :], writes=[("Gstage", 0), ("Gstage", 1)])
                norm_sb(P, stage_flat, [("Gstage", 0), ("Gstage", 1)], gbc, mnT, mt * 128, ("GmnT", mt))
            mnkeys = [("GmnT", 0), ("GmnT", 1)]
            nq_ = 0
            for b2 in range(8):
                wb, wk_ = load_wblk2("G", w_xk_d, b2, stages, wbs, kc)
                for e4 in range(2):
                    ec = b2 * 2 + e4
                    ps = psQ[nq_ % 2]; pk = ("GpsQ", nq_ % 2); nq_ += 1
                    for c in range(NCH):
                        S.op("pe", lambda e, c=c, e4=e4, ps=ps: e.matmul(ps[:, 0:256], lhsT=wb[:, c, e4 * 128:(e4 + 1) * 128],
                                                                         rhs=mnT[:, c, :], start=(c == 0), stop=(c == NCH - 1)),
                             reads=wk_ + mnkeys, writes=[pk])
                    S.op("act", lambda e, ec=ec, ps=ps: e.copy(out=kmT[:, ec, :], in_=ps[:, 0:256]), reads=[pk],
                         writes=[("GkmT", ec)])
            for b2 in range(8):
                wb, wk_ = load_wblk2("G", w_xv_d, b2, stages, wbs, kc)
                for mt in range(2):
                    ps = psQ[nq_ % 2]; pk = ("GpsQ", nq_ % 2); nq_ += 1
                    for c in range(NCH):
                        S.op("pe", lambda e, c=c, mt=mt, ps=ps: e.matmul(ps[:, 0:256], lhsT=mnT[:, c, mt * 128:(mt + 1) * 128],
                                                                         rhs=wb[:, c, :], start=(c == 0), stop=(c == NCH - 1)),
                             reads=wk_ + mnkeys, writes=[pk])
                    S.op("act", lambda e, mt=mt, b2=b2, ps=ps: e.copy(out=vm[:, mt, b2 * 256:(b2 + 1) * 256], in_=ps[:, 0:256]),
                         reads=[pk], writes=[("Gvm", mt, b2)])
            S.dma("sp", gbc[:], g_cross_d[0:1, :].partition_broadcast(128), writes=["Ggbc"])
            SCX = 512.0 ** -0.5
            kmkeys = [("GkmT", ec) for ec in range(NCH)]
            vmkeys = [("Gvm", mt, b2) for mt in range(2) for b2 in range(8)]
            for st in range(2):
                for sub in range(4):
                    tt = st * 4 + sub
                    norm_sb(P, xres[:, tt, :], [("xres", tt)], gbc, hT, sub * 128, "GhT")
                for b2 in range(8):
                    wb, wk_ = load_wblk2("G", w_xq_d, b2, stages, wbs, kc)
                    for e4 in range(2):
                        ec = b2 * 2 + e4
                        ps = psQ[nq_ % 2]; pk = ("GpsQ", nq_ % 2); nq_ += 1
                        for c in range(NCH):
                            S.op("pe", lambda e, c=c, e4=e4, ps=ps: e.matmul(ps[:, :], lhsT=wb[:, c, e4 * 128:(e4 + 1) * 128],
                                                                             rhs=hT[:, c, :], start=(c == 0), stop=(c == NCH - 1)),
                                 reads=wk_ + ["GhT"], writes=[pk])
                        if ec % 2 == 0:
                            S.op("act", lambda e, ec=ec, ps=ps: e.copy(out=qT[:, ec, :], in_=ps[:, :]), reads=[pk],
                                 writes=[("GqT", ec)])
                        else:
                            S.op("dve", lambda e, ec=ec, ps=ps: e.tensor_copy(qT[:, ec, :], ps[:, :]), reads=[pk],
                                 writes=[("GqT", ec)])
                for hd in range(4):
                    ecs = range(hd * 4, hd * 4 + 4)
                    qk = [("GqT", ec) for ec in ecs]
                    for mt in range(2):
                        for i_, ec in enumerate(ecs):
                            S.op("pe", lambda e, mt=mt, ec=ec, i_=i_: e.matmul(
                                psS[mt][:, :], lhsT=kmT[:, ec, mt * 128:(mt + 1) * 128], rhs=qT[:, ec, :],
                                start=(i_ == 0), stop=(i_ == 3)), reads=qk + kmkeys, writes=[("GpsS", mt)])
                        S.op("act", lambda e, mt=mt: e.activation(out=PTm[mt][:], in_=psS[mt][:, :], func=AF.Exp, scale=SCX),
                             reads=[("GpsS", mt)], writes=[("GPT", mt)])
                    ptk = [("GPT", 0), ("GPT", 1)]
                    for mt in range(2):
                        S.op("pe", lambda e, mt=mt: e.matmul(psD[:, :], lhsT=onesb[:], rhs=PTm[mt][:],
                                                             start=(mt == 0), stop=(mt == 1)),
                             reads=ptk + ["Gonesb"], writes=["GpsD"])
                    S.op("act", lambda e: e.copy(out=rD[:], in_=psD[:, :]), reads=["GpsD"], writes=["GrD"])
                    S.op("dve", lambda e: e.reciprocal(rD[:], rD[:]), reads=["GrD"], writes=["GrD"])
                    for ec in ecs:
                        for mt in range(2):
                            S.op("pe", lambda e, mt=mt, ec=ec: e.matmul(psO[:, :], lhsT=vm[:, mt, ec * 128:(ec + 1) * 128],
                                                                        rhs=PTm[mt][:], start=(mt == 0), stop=(mt == 1)),
                                 reads=ptk + vmkeys, writes=["GpsO"])
                        S.op("act", lambda e: e.copy(out=tO[:], in_=psO[:, :]), reads=["GpsO"], writes=["GtO"])
                        S.op("dve", lambda e, ec=ec: e.tensor_tensor(out=mixT[:, ec, st * 512:(st + 1) * 512], in0=tO[:],
                                                                     in1=rD[:], op=ALU.mult),
                             reads=["GtO", "GrD"], writes=[("Gact", ec, st)])
            S.barrier()
        add_proj("G", mixT, w_xo_d)

        if dbg is not None and dbg["name"] == "xresG":
            S.dma("sp", dbg_out.rearrange("(t p) n -> p t n", p=128), xres[:], writes=["dbgo"])
            S.barrier()
            return nc
        ids_all = es.enter_context(nc.sbuf_tensor("ids_all", [128, 8, 128], I32))
        gates_all = es.enter_context(nc.sbuf_tensor("gates_all", [128, 8, 128], F32))
        with ExitStack() as hs:
            def sb(name, shape, dt=F32):
                return hs.enter_context(nc.sbuf_tensor("H" + name, shape, dt))

            def pst(name, dt=F32, n=512):
                return hs.enter_context(nc.psum_tensor("Hps" + name, [128, n], dt))

            P = dict(tag="H", xb=sb("xb", [128, D], BF16), ss=sb("ss", [128, 1]),
                     psT=[pst(f"T{i}", BF16, 1024) for i in range(2)])
            gbc = sb("gbc", [128, D])
            S.dma("sp", gbc[:], g_ffn_d[0:1, :].partition_broadcast(128), writes=["Hgbc"])
            hT = moT[:].rearrange("p a b -> p (a b)").rearrange("p (c t) -> p c t", c=NCH)
            qT = aqT[:].rearrange("p a b -> p (a b)").rearrange("p (c t) -> p c t", c=NCH)
            stage_all = sb("stage", [128, 2, 1024])
            stages = [stage_all[:, i, :].rearrange("p (c n) -> p c n", c=4) for i in range(2)]
            wbs = [sb(f"wbk{i}", [128, NCH, 256], BF16) for i in range(2)]
            kc = [0]
            skT = sb("skT", [128, 16, 128], BF16)
            stage4 = stage_all[:].rearrange("p a b -> p (a b)").rearrange("p (a b) -> p a b", a=4)
            S.dma("sp", stage4, skT_d[:, :, :].rearrange("p (a b) k -> p a (b k)", a=4),
                  writes=[("Hstage", 0), ("Hstage", 1)])
            S.op("dve", lambda e: e.tensor_copy(skT[:].rearrange("p (a b) k -> p a (b k)", a=4), stage4),
                 reads=[("Hstage", 0), ("Hstage", 1)], writes=["HskT"])
            sc = sb("sc", [128, 16, 128]); wk1 = sb("wk1", [128, 128])
            ts = sb("ts", [128, 16, 16]); ti = sb("ti", [128, 16, 16], U32); tif = sb("tif", [128, 16, 16])
            cand = sb("cand", [128, 16, 16]); cid = sb("cid", [128, 16, 16]); wk2 = sb("wk2", [128, 256])
            junk = sb("junk", [128, 256]); bs = sb("bs", [128, 16]); negm = sb("negm", [128, 1])
            ge = sb("ge", [128, 16]); zz = sb("zz", [128, 1]); idf = sb("idf", [128, 128])
            pos = sb("pos", [128, 16], U32); posf = sb("posf", [128, 16]); iota_t = sb("iota", [128, 256])
            S.dma("sp", iota_t[:], iota_d[0:1, :].partition_broadcast(128), writes=["Hiota"])
            psQ = [pst("Q0"), pst("Q1")]; psC = [pst("C0"), pst("C1")]
            nq_ = 0
            for st in range(2):
                for sub in range(4):
                    tt = st * 4 + sub
                    norm_sb(P, xres[:, tt, :], [("xres", tt)], gbc, hT, sub * 128, "HhT")
                for b2 in range(8):
                    wb, wk_ = load_wblk2("H", w_pq_d, b2, stages, wbs, kc)
                    for e4 in range(2):
                        j = b2 * 2 + e4
                        ps = psQ[nq_ % 2]; pk = ("HpsQ", nq_ % 2); nq_ += 1
                        for c in range(NCH):
                            S.op("pe", lambda e, c=c, e4=e4, ps=ps: e.matmul(ps[:, :], lhsT=wb[:, c, e4 * 128:(e4 + 1) * 128],
                                                                             rhs=hT[:, c, :], start=(c == 0), stop=(c == NCH - 1)),
                                 reads=wk_ + ["HhT"], writes=[pk])
                        if j % 2 == 0:
                            S.op("act", lambda e, j=j, ps=ps: e.copy(out=qT[:, j, :], in_=ps[:, :]), reads=[pk],
                                 writes=[("HqT", j)])
                        else:
                            S.op("dve", lambda e, j=j, ps=ps: e.tensor_copy(qT[:, j, :], ps[:, :]), reads=[pk],
                                 writes=[("HqT", j)])
                for sub in range(4):
                    tt = st * 4 + sub
                    for jb in range(4):
                        pc = psC[jb % 2]; pck = ("HpsC", jb % 2)
                        for jj in range(4):
                            j = jb * 4 + jj
                            S.op("pe", lambda e, j=j, jj=jj, pc=pc: e.matmul(
                                pc[:, jj * 128:(jj + 1) * 128], lhsT=qT[:, j, sub * 128:(sub + 1) * 128], rhs=skT[:, j, :],
                                start=True, stop=True), reads=[("HqT", j), "HskT"], writes=[pck])
                        S.op("act", lambda e, jb=jb, pc=pc: e.copy(
                            out=sc[:, jb * 4:(jb + 1) * 4, :], in_=pc[:, :].rearrange("p (a k) -> p a k", a=4)),
                            reads=[pck], writes=[("Hsc", jb)])
                    for j in range(16):
                        sk_ = ("Hsc", j // 4)
                        S.op("dve", lambda e, j=j: e.max(out=ts[:, j, 0:8], in_=sc[:, j, :]), reads=[sk_], writes=[("Hts", j, 0)])
                        S.op("dve", lambda e, j=j: e.match_replace(out=wk1[:], in_to_replace=ts[:, j, 0:8],
                                                                   in_values=sc[:, j, :], imm_value=-1e30),
                             reads=[sk_, ("Hts", j, 0)], writes=["Hwk1"])
                        S.op("dve", lambda e, j=j: e.max(out=ts[:, j, 8:16], in_=wk1[:]), reads=["Hwk1"], writes=[("Hts", j, 1)])
                        S.op("dve", lambda e, j=j: e.max_index(out=ti[:, j, 0:8], in_max=ts[:, j, 0:8], in_values=sc[:, j, :]),
                             reads=[sk_, ("Hts", j, 0)], writes=[("Hti", j, 0)])
                        S.op("dve", lambda e, j=j: e.max_index(out=ti[:, j, 8:16], in_max=ts[:, j, 8:16], in_values=wk1[:]),
                             reads=["Hwk1", ("Hts", j, 1)], writes=[("Hti", j, 1)])
                    S.op("dve", lambda e: e.tensor_copy(tif[:], ti[:]),
                         reads=[("Hti", j, q) for j in range(16) for q in range(2)], writes=["Htif"])
                    for h in range(8):
                        j0, j1 = 2 * h, 2 * h + 1
                        tsk = [("Hts", j0, 0), ("Hts", j0, 1), ("Hts", j1, 0), ("Hts", j1, 1)]
                        S.op("dve", lambda e: e.tensor_tensor(
                            out=cand[:], in0=ts[:, j0, :].unsqueeze(2).to_broadcast([128, 16, 16]),
                            in1=ts[:, j1, :].unsqueeze(1).to_broadcast([128, 16, 16]), op=ALU.add),
                            reads=tsk, writes=["Hcand"])
                        S.op("dve", lambda e: e.scalar_tensor_tensor(
                            out=cid[:], in0=tif[:, j0, :].unsqueeze(2).to_broadcast([128, 16, 16]), scalar=128.0,
                            in1=tif[:, j1, :].unsqueeze(1).to_broadcast([128, 16, 16]), op0=ALU.mult, op1=ALU.add),
                            reads=["Htif"], writes=["Hcid"])
                        candf = cand[:].rearrange("p a b -> p (a b)")
                        cidf = cid[:].rearrange("p a b -> p (a b)")
                        S.op("dve", lambda e: e.max(out=bs[:, 0:8], in_=candf), reads=["Hcand"], writes=["Hbs0"])
                        S.op("dve", lambda e: e.match_replace(out=wk2[:], in_to_replace=bs[:, 0:8], in_values=candf,
                                                              imm_value=-1e30), reads=["Hcand", "Hbs0"], writes=["Hwk2"])
                        S.op("dve", lambda e: e.max(out=bs[:, 8:16], in_=wk2[:]), reads=["Hwk2"], writes=["Hbs1"])
                        S.op("dve", lambda e: e.max_index(out=pos[:, 0:8], in_max=bs[:, 0:8], in_values=candf),
                             reads=["Hcand", "Hbs0"], writes=["Hpos0"])
                        S.op("dve", lambda e: e.max_index(out=pos[:, 8:16], in_max=bs[:, 8:16], in_values=wk2[:]),
                             reads=["Hwk2", "Hbs1"], writes=["Hpos1"])
                        S.op("dve", lambda e: e.tensor_copy(posf[:], pos[:]), reads=["Hpos0", "Hpos1"], writes=["Hposf"])
                        for k in range(16):
                            S.op("dve", lambda e, k=k: e.scalar_tensor_tensor(
                                out=junk[:], in0=iota_t[:], scalar=posf[:, k:k + 1], in1=cidf, op0=ALU.is_equal, op1=ALU.mult,
                                accum_out=idf[:, h * 16 + k:h * 16 + k + 1]),
                                reads=["Hiota", "Hcid", "Hposf"], writes=["Hjunk", ("Hidf", h)])
                        S.op("dve", lambda e: e.tensor_scalar_mul(negm[:], bs[:, 0:1], -1.0), reads=["Hbs0"], writes=["Hnegm"])
                        S.op("act", lambda e: e.activation(out=ge[:], in_=bs[:], func=AF.Exp, bias=negm[:, 0:1], accum_out=zz[:]),
                             reads=["Hbs0", "Hbs1", "Hnegm"], writes=["Hge", "Hzz"])
                        S.op("dve", lambda e: e.reciprocal(zz[:], zz[:]), reads=["Hzz"], writes=["Hzz"])
                        S.op("dve", lambda e: e.tensor_scalar(out=gates_all[:, tt, h * 16:(h + 1) * 16], in0=ge[:],
                                                              scalar1=zz[:, 0:1], scalar2=None, op0=ALU.mult),
                             reads=["Hge", "Hzz"], writes=[("gates", tt, h)])
                    S.op("dve", lambda e: e.tensor_copy(ids_all[:, tt, :], idf[:]),
                         reads=[("Hidf", h) for h in range(8)], writes=[("ids", tt)])
            S.barrier()

        if dbg is not None and dbg["name"] == "peer_ids":
            S.dma("sp", dbg_out[0:128, :].rearrange("p (t k) -> p t k", t=8), ids_all[:].bitcast(F32), writes=["dbgo"])
            S.dma("sp", dbg_out[128:256, :].rearrange("p (t k) -> p t k", t=8), gates_all[:], writes=["dbgo2"])
            S.barrier()
            return nc

        with ExitStack() as hs:
            def sb(name, shape, dt=F32):
                return hs.enter_context(nc.sbuf_tensor("Hb" + name, shape, dt))

            P = dict(tag="Hb", xb=sb("xb", [128, D], BF16), ss=sb("ss", [128, 1]), psT=None)
            gbc = sb("gbc", [128, D])
            S.dma("sp", gbc[:], g_ffn_d[0:1, :].partition_broadcast(128), writes=["Hbgbc"])
            xn2 = [sb(f"xn{i}", [128, D]) for i in range(2)]
            actc = sb("actc", [128, 128]); cf = sb("cf", [128, 128]); tmpv = sb("tmpv", [128, D])
            dg = [sb(f"dg{i}", [128, 128], BF16) for i in range(4)]
            NG = 8
            fence_t = sb("fence", [128, 1])
            gall = mixT[:].rearrange("p a b -> p (a b)")
            gb_ = [gall[:, i * 2 * D:(i + 1) * 2 * D] for i in range(4)]
            gb_ += [moT[:].rearrange("p a b -> p (a b)")[:, i * 2 * D:(i + 1) * 2 * D] for i in range(2)]
            gb_ += [aqT[:].rearrange("p a b -> p (a b)")[:, i * 2 * D:(i + 1) * 2 * D] for i in range(2)]
            psV = [[hs.enter_context(nc.psum_tensor(f"HbpsV{q}_{n}", [128, 512], F32)) for n in range(4)] for q in range(2)]
            ng = 0
            for tt in range(8):
                xn = xn2[tt % 2]
                P["tag"] = f"Hb{tt % 2}"
                S.op("act", lambda e, tt=tt: e.activation(out=P["xb"][:], in_=xres[:, tt, :], func=AF.Square,
                                                          scale=float(D) ** -0.5, accum_out=P["ss"][:]),
                     reads=[("xres", tt)], writes=["Hbxb", "Hbss"])
                S.op("dve", lambda e: e.tensor_scalar_add(P["ss"][:], P["ss"][:], EPS), reads=["Hbss"], writes=["Hbss"])
                S.op("act", lambda e: e.sqrt(P["ss"][:], P["ss"][:]), reads=["Hbss"], writes=["Hbss"])
                S.op("dve", lambda e: e.reciprocal(P["ss"][:], P["ss"][:]), reads=["Hbss"], writes=["Hbss"])
                S.op("dve", lambda e, tt=tt, xn=xn: e.scalar_tensor_tensor(out=xn[:], in0=xres[:, tt, :], scalar=P["ss"][:, 0:1],
                                                                    in1=gbc[:], op0=ALU.mult, op1=ALU.mult),
                     reads=[("xres", tt), "Hbss", "Hbgbc"], writes=[("Hbxn", tt % 2)])
                pv = psV[tt % 2]

                def chain(slot, bq):
                    b_, q_ = bq
                    S.op("act", lambda e: e.activation(out=cf[:, slot:slot + 1], in_=actc[:, slot:slot + 1], func=AF.Gelu),
                         reads=[("Hbact", slot), "Hbfence"], writes=[("Hbcf", slot)])
                    S.op("act", lambda e: e.mul(cf[:, slot:slot + 1], cf[:, slot:slot + 1], gates_all[:, tt, slot:slot + 1]),
                         reads=[("Hbcf", slot)], writes=[("Hbcf", slot)])
                    S.op("act", lambda e: e.activation(out=dg[q_][:], in_=ident[:], func=AF.Copy, scale=cf[:, slot:slot + 1]),
                         reads=[("Hbcf", slot)], writes=[("Hbdg", q_)])
                    for n in range(4):
                        S.op("pe", lambda e, n=n: e.matmul(
                            pv[n][:, :], lhsT=dg[q_][:], rhs=gb_[b_][:, D + n * 512:D + (n + 1) * 512],
                            start=(slot == 0), stop=(slot == 127)),
                            reads=[("Hbdg", q_), ("gbuf", b_)], writes=[("HbpsV", tt % 2, n)])

                prev = None
                for slot in range(128):
                    b_ = ng % NG
                    q_ = ng % 4
                    ng += 1
                    S.dma("pool", None, None, reads=[("ids", tt)], writes=[("gbuf", b_)],
                          fn=lambda e, b_=b_, slot=slot, tt=tt: e.indirect_dma_start(
                              out=gb_[b_], out_offset=None, in_=uv_d[:, :],
                              in_offset=bass.IndirectOffsetOnAxis(ap=ids_all[:, tt, slot:slot + 1], axis=0)))
                    S.op("dve", lambda e, b_=b_, slot=slot, xn=xn: e.scalar_tensor_tensor(
                        out=P["xb"][:], in0=gb_[b_][:, 0:D], scalar=1.0, in1=xn[:], op0=ALU.mult, op1=ALU.mult,
                        accum_out=actc[:, slot:slot + 1]),
                        reads=[("gbuf", b_), ("Hbxn", tt % 2)], writes=["Hbxb", ("Hbact", slot), "Hbfence"])
                    if prev is not None:
                        chain(slot - 1, prev)
                    prev = (b_, q_)
                S.op("dve", lambda e: e.tensor_copy(fence_t[:], actc[:, 0:1]), reads=[("Hbact", 0)], writes=["Hbfence"])
                chain(127, prev)
                if dbg is not None and dbg["name"] == "peer_cf":
                    S.dma("sp", dbg_out[:, tt * 128:(tt + 1) * 128], cf[:], reads=[("Hbcf", s_) for s_ in range(128)], writes=[("dbgo", tt)])
                    S.dma("sp", dbg_out[:, 1024 + tt * 128:1024 + (tt + 1) * 128], actc[:], reads=[("Hbact", s_) for s_ in range(128)], writes=[("dbgo2", tt)])
                S.op("act", lambda e, pv=pv: e.copy(out=tmpv[:, 0:512], in_=pv[0][:, :]),
                     reads=[("HbpsV", tt % 2, 0)], writes=[("Hbtmp", 0)])
                S.op("act", lambda e, pv=pv: e.copy(out=tmpv[:, 512:1024], in_=pv[1][:, :]),
                     reads=[("HbpsV", tt % 2, 1)], writes=[("Hbtmp", 1)])
                S.op("act", lambda e, pv=pv: e.copy(out=tmpv[:, 1024:1536], in_=pv[2][:, :]),
                     reads=[("HbpsV", tt % 2, 2)], writes=[("Hbtmp", 2)])
                S.op("act", lambda e, pv=pv: e.copy(out=tmpv[:, 1536:2048], in_=pv[3][:, :]),
                     reads=[("HbpsV", tt % 2, 3)], writes=[("Hbtmp", 3)])
                S.op("dve", lambda e, tt=tt: e.tensor_tensor(out=xres[:, tt, :], in0=xres[:, tt, :], in1=tmpv[:], op=ALU.add),
                     reads=[("Hbtmp", n) for n in range(4)] + [("xres", tt)], writes=[("xres", tt)])
            S.barrier()

        with ExitStack() as fs:
            gfb = fs.enter_context(nc.sbuf_tensor("gfb", [128, D], F32))
            junk = fs.enter_context(nc.sbuf_tensor("Ijunk", [128, D], BF16))
            ss = fs.enter_context(nc.sbuf_tensor("Iss", [128, 1], F32))
            ot = [fs.enter_context(nc.sbuf_tensor(f"Iot{i}", [128, D], F32)) for i in range(2)]
            S.dma("sp", gfb[:], g_final_d[0:1, :].partition_broadcast(128), writes=["gfb"])
            for tt in range(8):
                o_ = ot[tt % 2]
                S.op("act", lambda e, tt=tt: e.activation(out=junk[:], in_=xres[:, tt, :], func=AF.Square,
                                                          scale=float(D) ** -0.5, accum_out=ss[:]),
                     reads=[("xres", tt)], writes=["Ijunk", "Iss"])
                S.op("dve", lambda e: e.tensor_scalar_add(ss[:], ss[:], EPS), reads=["Iss"], writes=["Iss"])
                S.op("act", lambda e: e.sqrt(ss[:], ss[:]), reads=["Iss"], writes=["Iss"])
                S.op("dve", lambda e: e.reciprocal(ss[:], ss[:]), reads=["Iss"], writes=["Iss"])
                S.op("dve", lambda e, tt=tt, o_=o_: e.scalar_tensor_tensor(out=o_[:], in0=xres[:, tt, :], scalar=ss[:, 0:1],
                                                                    in1=gfb[:], op0=ALU.mult, op1=ALU.mult),
                     reads=[("xres", tt), "Iss", "gfb"], writes=[("Iot", tt % 2)])
                S.dma("sp", out_d[tt * 128:(tt + 1) * 128, :], o_[:], reads=[("Iot", tt % 2)], writes=[("outd", tt)])
            S.barrier()

        S.barrier()
        print("instructions", S.nins, "waits", S.nwaits, S.cnt, S.epoch, S.nsem)
    return nc


def rope_tables(j):
    half = 64
    inv = (10000.0 ** (-np.arange(half, dtype=np.float32) / half)).astype(np.float32)
    pos = (1024 * j - NPRE + np.arange(WIN)).astype(np.float32)
    ang = pos[None, :] * inv[:, None]
    cos = np.cos(ang).astype(np.float32)
    sin = np.sin(ang).astype(np.float32)
    return np.concatenate([cos, cos], 0), np.concatenate([sin, sin], 0)


def make_in_maps(inputs):
    x = np.asarray(inputs["x"], np.float32)
    ident = np.eye(128, dtype=np.float32).astype(ml_dtypes.bfloat16)
    R = np.zeros((128, 128), np.float32)
    for p in range(64):
        R[p, p + 64] = -1.0
        R[p + 64, p] = 1.0
    rotT = np.ascontiguousarray(R.T).astype(ml_dtypes.bfloat16)
    tri = np.triu(np.ones((128, 128), np.float32))
    trl = np.tril(np.ones((128, 128), np.float32))
    f32 = lambda a: np.asarray(a, np.float32)
    skT_h = np.ascontiguousarray(f32(inputs["sub_keys"])[0].reshape(16, 128, 128).transpose(2, 0, 1))
    u_tab = f32(inputs["u_tab"])[0]
    v_tab = f32(inputs["v_tab"])[0]
    maps = []
    for c in range(8):
        b, j = c // 4, c % 4
        xw = np.zeros((WIN, D), np.float32)
        lo = 1024 * j - NPRE
        src0 = max(lo, 0)
        xw[src0 - lo:] = x[b, src0:1024 * j + 1024]
        cos, sin = rope_tables(j)
        pmv = np.where(np.arange(WIN) + lo >= 0, 0.0, -30000.0).astype(np.float32)
        kbA = np.zeros((128, 48), np.float32)
        gi = 0
        for Q in range(2):
            for (d, r, sub) in [(1, 0, s_) for s_ in range(4)] + [(4, r_, 0) for r_ in range(4)] + [(16, r_, 0) for r_ in range(16)]:
                u0 = 2048 + 512 * Q + r + 128 * sub
                u = u0 - 128 * d + d * np.arange(128)
                kbA[:, gi] = np.where(1024 * j - 2048 + u >= 0, 0.0, -30000.0)
                gi += 1
        m = {
            "x_win": xw,
            "w_in": np.ascontiguousarray(np.asarray(inputs["w_in"], np.float32)[0]),
            "g_mix": np.asarray(inputs["g_mix"], np.float32).reshape(1, D),
            "cosT": cos, "sinT": sin, "ident": ident, "rotT": rotT,
            "cw_h": np.ascontiguousarray(f32(inputs["conv_w"])[0].T.reshape(8, 128, 4).transpose(1, 0, 2)),
            "cb_h": np.ascontiguousarray(f32(inputs["conv_b"])[0].reshape(8, 128).T),
            "gm_h": np.ascontiguousarray(f32(inputs["g_mhead"])[0].reshape(8, 128).T),
            "gb_h": np.concatenate([f32(inputs["b_mi"])[0], f32(inputs["b_mf"])[0]]).reshape(1, 8),
            "pm_h": np.ascontiguousarray(pmv.reshape(32, 128).T),
            "tri_f": tri,
            "w_mq": f32(inputs["w_mq"])[0], "w_mk": f32(inputs["w_mk"])[0],
            "w_out": f32(inputs["w_out"])[0], "g_final": f32(inputs["g_final"]).reshape(1, D),
            "mem_b": np.ascontiguousarray(f32(inputs["mem"])[b]),
            "g_mem": f32(inputs["g_mem"]).reshape(1, D), "g_cross": f32(inputs["g_cross"]).reshape(1, D),
            "w_xq": f32(inputs["w_xq"])[0], "w_xk": f32(inputs["w_xk"])[0], "w_xv": f32(inputs["w_xv"])[0],
            "w_xo": f32(inputs["w_xo"])[0],
            "g_ffn": f32(inputs["g_ffn"]).reshape(1, D), "w_pq": f32(inputs["w_pq"])[0],
            "skT_h": skT_h, "iota256": np.arange(256, dtype=np.float32).reshape(1, 256), "u_tab": u_tab, "v_tab": v_tab,
            "kbA": kbA, "ga_h": np.ascontiguousarray(f32(inputs["g_ahead"])[0].T), "trl_f": trl,
        }
        maps.append(m)
    return maps


def kernel(**inputs):
    nc = build()
    maps = make_in_maps(inputs)
    res = run_bass_kernel_spmd(nc, maps, core_ids=list(range(8)))
    out = np.zeros((2, 4096, D), np.float32)
    for c in range(8):
        b, j = c // 4, c % 4
        out[b, 1024 * j:1024 * j + 1024] = res.results[c]["out"]
    return out
```

```python
import numpy as np
import ml_dtypes
from contextlib import ExitStack
import concourse.bass as bass
import concourse.mybir as mybir
from concourse.bass_utils import run_bass_kernel_spmd

F32 = mybir.dt.float32
BF16 = mybir.dt.bfloat16
I32 = mybir.dt.int32
U32 = mybir.dt.uint32
ALU = mybir.AluOpType
AF = mybir.ActivationFunctionType
AX = mybir.AxisListType

D = 2048
NCH = 16
WIN = 4096
OWN = 1024
NPRE = WIN - OWN
EPS = 1e-6
IN_COLS = 6152
NDS = 40


class Sched:
    LIM = 3500
    DLIM = 240

    def __init__(self, nc, es):
        self.nc = nc
        self.es = es
        self.engs = {"pe": nc.tensor, "act": nc.scalar, "dve": nc.vector, "pool": nc.gpsimd, "sp": nc.sync}
        self.nsem = 0
        self.h = {}
        self.epoch = {k: 0 for k in self.engs}
        self.cnt = {k: 0 for k in self.engs}
        for k in self.engs:
            self.h[(k, 0)] = self._new()
        self.seen = {k: {} for k in self.engs}
        self.dver = [0] * NDS
        self.dcnt = [0] * NDS
        for i in range(NDS):
            self.h[("d", i, 0)] = self._new()
        self.dnext = {"sp": 0, "pool": NDS // 2, "act": 0}
        self.lastw = {}
        self.rd = {}
        self.nwaits = 0
        self.nins = 0

    def _new(self):
        self.nsem += 1
        return self.es.enter_context(self.nc.semaphore(f"s{self.nsem}"))

    def _wait(self, eng, sk, val):
        if val <= 0:
            return
        if self.seen[eng].get(sk, 0) >= val:
            return
        self.seen[eng][sk] = val
        self.engs[eng].wait_ge(self.h[sk], val)
        self.nwaits += 1

    def _deps(self, eng, reads, writes):
        need = {}

        def add(t, war=False):
            if t is None:
                return
            sk, val = t
            if sk[0] == eng and eng == "pe":
                return
            if need.get(sk, 0) < val:
                need[sk] = val

        for k in reads:
            add(self.lastw.get(k))
        for k in writes:
            add(self.lastw.get(k))
            for t in self.rd.get(k, ()):
                add(t, war=True)
        for sk, val in need.items():
            self._wait(eng, sk, val)

    def _commit(self, ticket, reads, writes):
        for k in reads:
            self.rd.setdefault(k, []).append(ticket)
        for k in writes:
            self.lastw[k] = ticket
            self.rd[k] = []

    def op(self, eng, fn, reads=(), writes=()):
        self._deps(eng, reads, writes)
        if self.cnt[eng] >= self.LIM:
            self.epoch[eng] += 1
            self.cnt[eng] = 0
            self.h[(eng, self.epoch[eng])] = self._new()
        sk = (eng, self.epoch[eng])
        ins = fn(self.engs[eng])
        ins.then_inc(self.h[sk], 1)
        self.cnt[eng] += 1
        self.nins += 1
        self._commit((sk, self.cnt[eng]), reads, writes)

    def dma(self, q, out, in_, reads=(), writes=(), fn=None):
        i = self.dnext[q]
        half = NDS // 2
        base = half if q == "pool" else 0
        self.dnext[q] = base + (i - base + 1) % half
        sk = ("d", i, self.dver[i])
        self._wait(q, sk, 16 * self.dcnt[i])
        if self.dcnt[i] >= self.DLIM:
            self.dver[i] += 1
            self.dcnt[i] = 0
            sk = ("d", i, self.dver[i])
            self.h[sk] = self._new()
        self._deps(q, reads, writes)
        if fn is None:
            ins = self.engs[q].dma_start(out=out, in_=in_)
        else:
            ins = fn(self.engs[q])
        ins.then_inc(self.h[sk], 16)
        self.dcnt[i] += 1
        self.nins += 1
        self._commit((sk, 16 * self.dcnt[i]), reads, writes)

    def barrier(self):
        for e in self.engs:
            for e2 in self.engs:
                if e2 != e:
                    if self.cnt[e2] > 0:
                        self._wait(e, (e2, self.epoch[e2]), self.cnt[e2])
                    elif self.epoch[e2] > 0:
                        self._wait(e, (e2, self.epoch[e2] - 1), self.LIM)
            for i in range(NDS):
                if self.dcnt[i] > 0:
                    self._wait(e, ("d", i, self.dver[i]), 16 * self.dcnt[i])
                elif self.dver[i] > 0:
                    self._wait(e, ("d", i, self.dver[i] - 1), 16 * self.DLIM)
        self.lastw = {}
        self.rd = {}


def bcast_free(ap_col, n):
    return ap_col.to_broadcast([ap_col.shape[0], n])


class K:
    pass


def load_weight_bf16(S, nc, es, w_dram, col0, ncols, name, stage, stage_key):
    wt = es.enter_context(nc.sbuf_tensor(name, [128, NCH, ncols], BF16))
    wv = w_dram.rearrange("(c p) n -> p c n", p=128)
    step = max(1, 2048 // ncols)
    c = 0
    i = 0
    while c < NCH:
        nn = min(step, NCH - c)
        sl = i % 2
        st = stage[sl]
        sv = st[:, 0:nn * ncols].rearrange("p (c n) -> p c n", n=ncols)
        S.dma("sp", sv, wv[:, c:c + nn, col0:col0 + ncols], writes=[(stage_key, sl)])
        eng = "pool" if i % 2 == 0 else "dve"
        S.op(eng, lambda e, c=c, nn=nn, sv=sv: e.tensor_copy(wt[:, c:c + nn, :], sv),
             reads=[(stage_key, sl)], writes=[(name, c + q) for q in range(nn)])
        c += nn
        i += 1
    return wt


def build(dbg=None):
    nc = bass.Bass("TRN2", target_bir_lowering=False)
    try:
        nc.allow_low_precision("bf16 matmuls with fp32 accumulation")
    except Exception:
        pass

    def din(name, shape, dt=F32):
        return nc.dram_tensor(name, list(shape), dt, kind="ExternalInput").ap()

    def dint(name, shape, dt=BF16):
        return nc.dram_tensor(name, list(shape), dt, kind="Internal").ap()

    x_win = din("x_win", [WIN, D])
    w_in = din("w_in", [D, IN_COLS])
    g_mix = din("g_mix", [1, D])
    cosT = din("cosT", [128, WIN])
    sinT = din("sinT", [128, WIN])
    ident_d = din("ident", [128, 128], BF16)
    rot_d = din("rotT", [128, 128], BF16)

    cw_d = din("cw_h", [128, 8, 4])
    cb_d = din("cb_h", [128, 8])
    gm_d = din("gm_h", [128, 8])
    gb_d = din("gb_h", [1, 8])
    pm_d = din("pm_h", [128, 32])
    tri_d = din("tri_f", [128, 128])
    w_mq_d = din("w_mq", [4, 256, 256])
    w_mk_d = din("w_mk", [4, 256, 256])

    kbA_d = din("kbA", [128, 48])
    ga_d = din("ga_h", [128, 8])
    trl_d = din("trl_f", [128, 128])
    w_out_d = din("w_out", [D, D])
    mem_d = din("mem_b", [256, D])
    g_ffn_d = din("g_ffn", [1, D])
    iota_d = din("iota256", [1, 256])
    w_pq_d = din("w_pq", [D, D])
    skT_d = din("skT_h", [128, 16, 128])
    u_tab_d = din("u_tab", [16384, D])
    v_tab_d = din("v_tab", [16384, D])
    g_mem_d = din("g_mem", [1, D])
    g_cross_d = din("g_cross", [1, D])
    w_xq_d = din("w_xq", [D, D]); w_xk_d = din("w_xk", [D, D]); w_xv_d = din("w_xv", [D, D]); w_xo_d = din("w_xo", [D, D])
    g_final_d = din("g_final", [1, D])
    out_d = nc.dram_tensor("out", [OWN, D], F32, kind="ExternalOutput").ap()
    dbg_out = None
    if dbg is not None:
        dbg_out = nc.dram_tensor("dbg", list(dbg["shape"]), dbg.get("dt", F32), kind="ExternalOutput").ap()

    uv_d = dint("uv_tab", [16384, 2 * D])
    s_minT = dint("s_minT", [1024, WIN])
    s_mv = dint("s_mv", [WIN, 1024])
    s_gates = dint("s_gates", [WIN, 8], F32)
    s_akT = dint("s_akT", [1024, WIN])
    s_av = dint("s_av", [WIN, 1024])

    with ExitStack() as es:
        es.enter_context(nc.allow_low_precision(reason="bf16 operands, fp32 accumulation"))
        S = Sched(nc, es)
        ident = es.enter_context(nc.sbuf_tensor("identb", [128, 128], BF16))
        rotT = es.enter_context(nc.sbuf_tensor("rotTb", [128, 128], BF16))
        S.dma("sp", ident[:], ident_d[:, :], writes=["ident"])
        S.dma("sp", rotT[:], rot_d[:, :], writes=["rotT"])
        es_mix = es
        es_mix2 = es
        moT = es_mix.enter_context(nc.sbuf_tensor("moT", [128, 8, OWN], BF16))
        aqT = es_mix.enter_context(nc.sbuf_tensor("aqT", [128, 8, OWN], BF16))


        def norm_tile(pes, x_src_ap, gbc, hT, col0, xkey, part="both"):
            xt, xb, ss, rstd, psT = pes["xt"], pes["xb"], pes["ss"], pes["rstd"], pes["psT"]
            if part in ("both", "pre"):
                norm_tile_pre(pes, x_src_ap, gbc)
            if part in ("both", "post"):
                norm_tile_post(pes, hT, col0, xkey)

        def norm_tile_pre(pes, x_src_ap, gbc):
            xt, xb, ss, rstd, psT = pes["xt"], pes["xb"], pes["ss"], pes["rstd"], pes["psT"]
            S.dma("sp", xt[:], x_src_ap, writes=["xt"])
            S.op("act", lambda e: e.activation(out=xb[:], in_=xt[:], func=AF.Square, scale=float(D) ** -0.5,
                                               accum_out=ss[:]),
                 reads=["xt"], writes=["xb", "ss"])
            S.op("dve", lambda e: e.tensor_scalar_add(rstd[:], ss[:], EPS), reads=["ss"], writes=["rstd"])
            S.op("act", lambda e: e.sqrt(rstd[:], rstd[:]), reads=["rstd"], writes=["rstd"])
            S.op("dve", lambda e: e.reciprocal(rstd[:], rstd[:]), reads=["rstd"], writes=["rstd"])
            S.op("dve", lambda e: e.scalar_tensor_tensor(out=xb[:], in0=xt[:], scalar=rstd[:, 0:1], in1=gbc[:],
                                                         op0=ALU.mult, op1=ALU.mult),
                 reads=["xt", "rstd", "gbc"], writes=["xb"])

        def norm_tile_post(pes, hT, col0, xkey):
            xt, xb, ss, rstd, psT = pes["xt"], pes["xb"], pes["ss"], pes["rstd"], pes["psT"]
            for half in range(2):
                for j in range(8):
                    c = half * 8 + j
                    S.op("pe", lambda e, c=c, j=j, half=half: e.transpose(
                        psT[half][:, j * 128:(j + 1) * 128], xb[:, c * 128:(c + 1) * 128], ident[:]),
                        reads=["xb", "ident"], writes=[("psT", half)])
                eng = "act" if half == 0 else "dve"
                if eng == "act":
                    S.op("act", lambda e, half=half: e.copy(
                        out=hT[:, half * 8:(half + 1) * 8, col0:col0 + 128],
                        in_=psT[half][:].rearrange("p (c t) -> p c t", c=8)),
                        reads=[("psT", half)], writes=[xkey])
                else:
                    S.op("dve", lambda e, half=half: e.tensor_copy(
                        hT[:, half * 8:(half + 1) * 8, col0:col0 + 128],
                        psT[half][:].rearrange("p (c t) -> p c t", c=8)),
                        reads=[("psT", half)], writes=[xkey])

        def phase_proj(name, st_list, specs, gain_d):
            with ExitStack() as pes_:
                pes = {}
                pes["xt"] = pes_.enter_context(nc.sbuf_tensor(name + "xt", [128, D], F32))
                pes["xb"] = pes_.enter_context(nc.sbuf_tensor(name + "xb", [128, D], BF16))
                pes["ss"] = pes_.enter_context(nc.sbuf_tensor(name + "ss", [128, 1], F32))
                pes["rstd"] = pes_.enter_context(nc.sbuf_tensor(name + "rstd", [128, 1], F32))
                pes["psT"] = [pes_.enter_context(nc.psum_tensor(name + f"psT{i}", [128, 1024], BF16)) for i in range(2)]
                gbc = pes_.enter_context(nc.sbuf_tensor(name + "gbc", [128, D], F32))
                S.dma("sp", gbc[:], gain_d.partition_broadcast(128), writes=["gbc"])
                hT = [pes_.enter_context(nc.sbuf_tensor(name + f"hT{i}", [128, NCH, 512], BF16)) for i in range(2)]
                stage = [pes_.enter_context(nc.sbuf_tensor(name + f"wst{i}", [128, 2048], F32)) for i in range(2)]
                psM = [pes_.enter_context(nc.psum_tensor(name + f"psM{i}", [128, 512], F32)) for i in range(4)]
                for sub in range(4):
                    t0_ = st_list[0] * 512 + sub * 128
                    norm_tile(pes, x_win[t0_:t0_ + 128, :], gbc, hT[0], sub * 128, ("hT", 0))
                ws = []
                for si, sp in enumerate(specs):
                    ws.append(load_weight_bf16(S, nc, pes_, w_in, sp["col0"], sp["ncols"], f"{name}w{si}", stage,
                                               name + "wst"))
                env = dict(pes_=pes_, psM=psM)
                for sp in specs:
                    if "setup" in sp:
                        sp["setup"](env)
                pmc = [0]

                def emit_norm(sti, sub, part="both"):
                    t0 = st_list[sti] * 512 + sub * 128
                    norm_tile(pes, x_win[t0:t0 + 128, :], gbc, hT[sti % 2], sub * 128, ("hT", sti % 2), part=part)

                def grp_f(st, h, hkey, sp, w, wkeys, cc):
                    ps = psM[pmc[0] % 4]
                    pk = ("psM", pmc[0] % 4)
                    pmc[0] += 1
                    for c in range(NCH):
                        S.op("pe", lambda e, c=c: e.matmul(
                            ps[:, :], lhsT=w[:, c, cc * 128:(cc + 1) * 128], rhs=h[:, c, :],
                            start=(c == 0), stop=(c == NCH - 1)), reads=[hkey] + wkeys, writes=[pk])
                    sp["evac"](env, st, cc, ps, pk)

                def grp_t(st, h, hkey, sp, w, wkeys, sub, nb):
                    n0 = nb * 512
                    nn = min(512, sp["ncols"] - n0)
                    ps = psM[pmc[0] % 4]
                    pk = ("psM", pmc[0] % 4)
                    pmc[0] += 1
                    for c in range(NCH):
                        S.op("pe", lambda e, c=c: e.matmul(
                            ps[:, 0:nn], lhsT=h[:, c, sub * 128:(sub + 1) * 128],
                            rhs=w[:, c, n0:n0 + nn], start=(c == 0), stop=(c == NCH - 1)),
                            reads=[hkey] + wkeys, writes=[pk])
                    sp["evac"](env, st, sub, nb, ps, pk, nn)

                for sti, st in enumerate(st_list):
                    h = hT[sti % 2]
                    hkey = ("hT", sti % 2)
                    groups = []
                    for si, sp in enumerate(specs):
                        w = ws[si]
                        wkeys = [(f"{name}w{si}", c) for c in range(NCH)]
                        if sp["kind"] == "f":
                            for cc in range(sp["ncols"] // 128):
                                groups.append(lambda st=st, h=h, hkey=hkey, sp=sp, w=w, wkeys=wkeys, cc=cc:
                                              grp_f(st, h, hkey, sp, w, wkeys, cc))
                        else:
                            for sub in range(4):
                                for nb in range((sp["ncols"] + 511) // 512):
                                    groups.append(lambda st=st, h=h, hkey=hkey, sp=sp, w=w, wkeys=wkeys, sub=sub, nb=nb:
                                                  grp_t(st, h, hkey, sp, w, wkeys, sub, nb))
                    per = (len(groups) + 3) // 4
                    for k in range(4):
                        if sti + 1 < len(st_list):
                            emit_norm(sti + 1, k, "pre")
                        for g in groups[k * per:(k + 1) * per]:
                            g()
                        if sti + 1 < len(st_list):
                            emit_norm(sti + 1, k, "post")
                    for sp in specs:
                        if "flush" in sp:
                            sp["flush"](env, st)
                S.barrier()

        def mk_f_to_dram(dst, nchunks, tag, rope=False, sb_dst=None, st0=0, func=None):
            st_ = {}

            def setup(env):
                if sb_dst is None:
                    st_["stg"] = [env["pes_"].enter_context(nc.sbuf_tensor(f"{tag}stg{i}", [128, nchunks, 512], BF16))
                                  for i in range(2)]
                st_["n"] = 0
                if func == "sigexp":
                    st_["sg"] = env["pes_"].enter_context(nc.sbuf_tensor(f"{tag}sg", [128, 512], F32))
                if rope:
                    st_["kb"] = env["pes_"].enter_context(nc.sbuf_tensor(f"{tag}kb", [128, 512], BF16))
                    st_["t1"] = env["pes_"].enter_context(nc.sbuf_tensor(f"{tag}t1", [128, 512], F32))
                    st_["t2"] = env["pes_"].enter_context(nc.sbuf_tensor(f"{tag}t2", [128, 512], F32))
                    st_["psR"] = env["pes_"].enter_context(nc.psum_tensor(f"{tag}psR", [128, 512], F32))
                    st_["cos"] = env["pes_"].enter_context(nc.sbuf_tensor(f"{tag}cos", [128, 512], F32))
                    st_["sin"] = env["pes_"].enter_context(nc.sbuf_tensor(f"{tag}sin", [128, 512], F32))

            def evac(env, st, cc, ps, pk):
                if sb_dst is None:
                    sl = st_["n"] % 2
                    oap = st_["stg"][sl][:, cc, :]
                    okey = (tag + "stg", sl)
                else:
                    oap = sb_dst[:, cc, (st - st0) * 512:(st - st0 + 1) * 512]
                    okey = (tag + "sb", cc, st)
                if not rope:
                    if func == "sigexp":
                        sg = st_["sg"]
                        S.op("act", lambda e: e.activation(out=sg[:], in_=ps[:, :], func=AF.Exp, scale=-1.0),
                             reads=[pk], writes=[tag + "sg"])
                        S.op("dve", lambda e: e.tensor_scalar_add(sg[:], sg[:], 1.0), reads=[tag + "sg"], writes=[tag + "sg"])
                        S.op("dve", lambda e: e.reciprocal(oap, sg[:]), reads=[tag + "sg"], writes=[okey])
                    elif func is not None:
                        S.op("act", lambda e: e.activation(out=oap, in_=ps[:, :], func=func), reads=[pk], writes=[okey])
                    elif cc % 2 == 0:
                        S.op("act", lambda e: e.copy(out=oap, in_=ps[:, :]), reads=[pk], writes=[okey])
                    else:
                        S.op("dve", lambda e: e.tensor_copy(oap, ps[:, :]), reads=[pk], writes=[okey])
                else:
                    kb, t1, t2, psR = st_["kb"], st_["t1"], st_["t2"], st_["psR"]
                    p0 = 0
                    if cc == 0:
                        S.dma("sp", st_["cos"][:], cosT[:, st * 512:(st + 1) * 512], writes=[tag + "cos"])
                        S.dma("sp", st_["sin"][:], sinT[:, st * 512:(st + 1) * 512], writes=[tag + "sin"])
                    S.op("act", lambda e: e.copy(out=kb[:], in_=ps[:, :]), reads=[pk], writes=[tag + "kb"])
                    S.op("pe", lambda e: e.matmul(psR[:, :], lhsT=rotT[:], rhs=kb[:], start=True, stop=True),
                         reads=[tag + "kb", "rotT"], writes=[tag + "psR"])
                    S.op("dve", lambda e: e.tensor_tensor(out=t1[:], in0=kb[:], in1=st_["cos"][:, 0:512], op=ALU.mult),
                         reads=[tag + "kb", tag + "cos"], writes=[tag + "t1"])
                    S.op("act", lambda e: e.copy(out=t2[:], in_=psR[:, :]), reads=[tag + "psR"], writes=[tag + "t2"])
                    S.op("dve", lambda e: e.tensor_tensor(out=t2[:], in0=t2[:], in1=st_["sin"][:, 0:512], op=ALU.mult),
                         reads=[tag + "t2", tag + "sin"], writes=[tag + "t2"])
                    S.op("dve", lambda e: e.tensor_tensor(out=oap, in0=t1[:], in1=t2[:], op=ALU.add),
                         reads=[tag + "t1", tag + "t2"], writes=[okey])

            def flush(env, st):
                if sb_dst is None:
                    sl = st_["n"] % 2
                    stg = st_["stg"][sl]
                    S.dma("pool", dst.rearrange("(c p) t -> p c t", p=128)[:, :, st * 512:(st + 1) * 512], stg[:],
                          reads=[(tag + "stg", sl)], writes=[(tag + "dram", st)])
                st_["n"] += 1

            return dict(setup=setup, evac=evac, flush=flush)

        def mk_t_to_dram(dst, ncols, tag, dt=BF16):
            st_ = {}

            def setup(env):
                st_["stg"] = [env["pes_"].enter_context(nc.sbuf_tensor(f"{tag}stg{i}", [128, 4, ncols], dt))
                              for i in range(2)]
                st_["n"] = 0

            def evac(env, st, sub, nb, ps, pk, nn):
                sl = st_["n"] % 2
                stg = st_["stg"][sl]
                skey = (tag + "stg", sl)
                if (sub + nb) % 2 == 0:
                    S.op("act", lambda e: e.copy(out=stg[:, sub, nb * 512:nb * 512 + nn], in_=ps[:, 0:nn]),
                         reads=[pk], writes=[skey])
                else:
                    S.op("dve", lambda e: e.tensor_copy(stg[:, sub, nb * 512:nb * 512 + nn], ps[:, 0:nn]),
                         reads=[pk], writes=[skey])

            def flush(env, st):
                sl = st_["n"] % 2
                stg = st_["stg"][sl]
                S.dma("pool", dst[st * 512:(st + 1) * 512, :].rearrange("(s p) n -> p s n", p=128), stg[:],
                      reads=[(tag + "stg", sl)], writes=[(tag + "dram", st)])
                st_["n"] += 1

            return dict(setup=setup, evac=evac, flush=flush)

        spA = [dict(kind="f", col0=0, ncols=1024, **mk_f_to_dram(s_minT, 8, "Amin")),
               dict(kind="t", col0=1024, ncols=1024, **mk_t_to_dram(s_mv, 1024, "Amv")),
               dict(kind="t", col0=3072, ncols=8, **mk_t_to_dram(s_gates, 8, "Ag", F32))]
        phase_proj("A", list(range(8)), spA, g_mix[0:1, :])

        if dbg is not None and dbg["name"] == "stopA":
            S.barrier()
            return nc
        spB = [dict(kind="f", col0=4104, ncols=1024, **mk_f_to_dram(s_akT, 8, "Bak", rope=True)),
               dict(kind="t", col0=5128, ncols=1024, **mk_t_to_dram(s_av, 1024, "Bav"))]
        phase_proj("B", list(range(2, 8)), spB, g_mix[0:1, :])
        if dbg is not None and dbg["name"] == "stopB":
            S.barrier()
            return nc
        spC = [dict(kind="f", col0=2048, ncols=1024, **mk_f_to_dram(None, 8, "Cmo", sb_dst=moT, st0=6, func="sigexp")),
               dict(kind="f", col0=3080, ncols=1024, **mk_f_to_dram(None, 8, "Caq", rope=True, sb_dst=aqT, st0=6))]
        phase_proj("C", [6, 7], spC, g_mix[0:1, :])

        mixT = es_mix2.enter_context(nc.sbuf_tensor("mixT", [128, NCH, OWN], BF16))
        print("after C", S.cnt, S.epoch, S.nsem)
        if dbg is not None and dbg["name"] == "aqT":
            S.dma("sp", dbg_out[0:1024, :].rearrange("(c p) t -> p c t", p=128), aqT[:], writes=["dbgo"])
            S.dma("sp", dbg_out[1024:2048, :].rearrange("(c p) t -> p c t", p=128), moT[:], writes=["dbgo2"])
            S.barrier()
            return nc
        with ExitStack() as ds:
            def sb(name, shape, dt=F32):
                return ds.enter_context(nc.sbuf_tensor("D" + name, shape, dt))

            def pst(name):
                return ds.enter_context(nc.psum_tensor("Dps" + name, [128, 512], F32))

            cw = sb("cw", [128, 8, 4]); cbias = sb("cbias", [128, 8]); gm = sb("gm", [128, 8]); gb = sb("gb", [128, 8])
            pm = sb("pm", [128, 32]); tri_f = sb("trif", [128, 128]); ones_f = sb("onesf", [128, 128])
            S.dma("sp", cw[:], cw_d[:, :, :], writes=["cw"])
            S.dma("sp", cbias[:], cb_d[:, :], writes=["cbias"])
            S.dma("sp", gm[:], gm_d[:, :], writes=["gm"])
            S.dma("sp", gb[:], gb_d[0:1, :].partition_broadcast(128), writes=["gb"])
            S.dma("sp", pm[:], pm_d[:, :], writes=["pm"])
            S.dma("sp", tri_f[:], tri_d[:, :], writes=["tri_f"])
            S.op("pool", lambda e: e.memset(ones_f[:], 1.0), writes=["ones_f"])
            wstg = sb("wstg", [128, 4, 2, 256])
            wq = sb("wq", [128, 4, 2, 256], BF16); wk = sb("wk", [128, 4, 2, 256], BF16)
            S.dma("sp", wstg[:], w_mq_d.rearrange("h (c p) e -> p h c e", p=128), writes=["wstg"])
            S.op("dve", lambda e: e.tensor_copy(wq[:], wstg[:]), reads=["wstg"], writes=["wq"])
            S.dma("sp", wstg[:], w_mk_d.rearrange("h (c p) e -> p h c e", p=128), reads=[], writes=["wstg"])
            S.op("dve", lambda e: e.tensor_copy(wk[:], wstg[:]), reads=["wstg"], writes=["wk"])
            Sst = [sb(f"Sst{h}", [128, 2, 384]) for h in range(4)]
            Sbf = [sb(f"Sbf{h}", [128, 2, 384], BF16) for h in range(4)]
            for h in range(4):
                S.op("pool", lambda e, h=h: e.memset(Sst[h][:], 0.0), writes=[("Sst", h, 0), ("Sst", h, 1)])
                S.op("pool", lambda e, h=h: e.memset(Sbf[h][:], 0.0), writes=[("Sbf", h)])
            xin = [sb(f"xin{i}", [128, 8, 131], BF16) for i in range(2)]
            vaug = [sb(f"vaug{i}", [128, 4, 384], BF16) for i in range(2)]
            gt = [sb(f"gt{i}", [128, 8]) for i in range(2)]
            cT = [sb(f"cT{i}", [128, 8, 128], BF16) for i in range(2)]
            for i in range(2):
                S.op("pool", lambda e, i=i: e.memset(vaug[i][:, :, 256:384], 1.0), writes=[("vaug", i)])
            acc = [sb(f"acc{i}", [128, 128]) for i in range(2)]
            g2 = sb("g2", [128, 8]); sp4 = sb("sp4", [128, 4]); w4 = sb("w4", [128, 4]); spb = sb("spb", [128, 4, 128])
            two = lambda nm, shape, dt=F32: [sb(f"{nm}{i}", shape, dt) for i in range(2)]
            kp = two("kp", [128, 256], BF16); kTb = two("kTb", [128, 2, 128], BF16); qTb = two("qTb", [128, 2, 128], BF16)
            clampT = two("clampT", [128, 128]); PT = two("PT", [128, 128], BF16); dd = two("dd", [128, 128])
            hn = two("hn", [128, 2, 128]); sq = two("sq", [128, 2, 128]); rr = two("rr", [128, 128]); tmpo = two("tmpo", [128, 128])
            dec = two("dec", [128, 1]); dSs = two("dSs", [128, 2, 384])
            psk = pst("k"); pskT = pst("kT"); psqT = pst("qT"); psS = pst("S"); psO = pst("O"); psMisc = pst("M")
            psdS = [pst("dS0"), pst("dS1")]
            minT_v = s_minT.rearrange("(c p) t -> p c t", p=128)

            def conv_tile(et):
                rs = slice(et * 128, (et + 1) * 128)
                S.dma("pool", uv_d[rs, 0:D], u_tab_d[rs, :], writes=[("uvd", et, 0)])
                S.dma("pool", uv_d[rs, D:2 * D], v_tab_d[rs, :], writes=[("uvd", et, 1)])

            for ck in range(32):
                own = ck >= 24
                par = ck % 2
                t0 = ck * 128
                xin_, vaug_, gt_, cT_ = xin[par], vaug[par], gt[par], cT[par]
                if ck == 0:
                    S.op("pool", lambda e: e.memset(xin_[:, :, 0:3], 0.0), writes=[("xin", par)])
                    S.dma("sp", xin_[:, :, 3:131], minT_v[:, :, 0:128], writes=[("xin", par)])
                else:
                    S.dma("sp", xin_[:, :, 0:131], minT_v[:, :, t0 - 3:t0 + 128], writes=[("xin", par)])
                S.dma("sp", vaug_[:, :, 0:256], s_mv[t0:t0 + 128, :].rearrange("p (h e) -> p h e", h=4),
                      writes=[("vaug", par)])
                S.dma("sp", gt_[:], s_gates[t0:t0 + 128, :], writes=[("gt", par)])
                for et in range(ck * 4, ck * 4 + 4):
                    conv_tile(et)
                S.op("dve", lambda e: e.tensor_tensor(out=g2[:], in0=gt_[:], in1=gb[:], op=ALU.add),
                     reads=[("gt", par), "gb"], writes=["g2"])
                S.op("dve", lambda e: e.tensor_scalar(out=g2[:, 0:4], in0=g2[:, 0:4], scalar1=pm[:, ck:ck + 1],
                                                      scalar2=None, op0=ALU.add), reads=["g2", "pm"], writes=["g2"])
                S.op("act", lambda e: e.activation(out=sp4[:], in_=g2[:, 4:8], func=AF.Exp, scale=-1.0),
                     reads=["g2"], writes=["sp4"])
                S.op("dve", lambda e: e.tensor_scalar_add(sp4[:], sp4[:], 1.0), reads=["sp4"], writes=["sp4"])
                S.op("act", lambda e: e.activation(out=sp4[:], in_=sp4[:], func=AF.Ln), reads=["sp4"], writes=["sp4"])
                S.op("pe", lambda e: e.matmul(psMisc[:, 256:260], lhsT=tri_f[:], rhs=sp4[:], start=True, stop=True),
                     reads=["tri_f", "sp4"], writes=["ps_csp"])
                S.op("act", lambda e: e.copy(out=w4[:], in_=psMisc[:, 256:260]), reads=["ps_csp"], writes=["w4"])
                S.op("dve", lambda e: e.tensor_tensor(out=w4[:], in0=g2[:, 0:4], in1=w4[:], op=ALU.add),
                     reads=["g2", "w4"], writes=["w4"])
                S.op("act", lambda e: e.activation(out=w4[:], in_=w4[:], func=AF.Exp), reads=["w4"], writes=["w4"])
                S.op("dve", lambda e: e.tensor_scalar_mul(w4[:], w4[:], 0.0625), reads=["w4"], writes=["w4"])
                S.op("dve", lambda e: e.tensor_copy(spb[:], sp4[:].unsqueeze(2).to_broadcast([128, 4, 128])),
                     reads=["sp4"], writes=["spb"])
                for c8 in range(8):
                    a_ = acc[c8 % 2]
                    ak = ("acc", c8 % 2)
                    S.op("dve", lambda e, c8=c8, a_=a_: e.tensor_scalar(out=a_[:], in0=xin_[:, c8, 0:128],
                                                                        scalar1=cw[:, c8, 0:1], scalar2=None,
                                                                        op0=ALU.mult),
                         reads=[("xin", par), "cw"], writes=[ak])
                    for jj in range(1, 4):
                        S.op("dve", lambda e, c8=c8, a_=a_, jj=jj: e.scalar_tensor_tensor(
                            out=a_[:], in0=xin_[:, c8, jj:jj + 128], scalar=cw[:, c8, jj:jj + 1], in1=a_[:],
                            op0=ALU.mult, op1=ALU.add), reads=[("xin", par), "cw", ak], writes=[ak])
                    S.op("act", lambda e, c8=c8, a_=a_: e.activation(out=cT_[:, c8, :], in_=a_[:], func=AF.Silu,
                                                                     bias=cbias[:, c8:c8 + 1]),
                         reads=[ak, "cbias"], writes=[("cT", par, c8)])
                def head_gen(h):
                    hp = h % 2
                    kp_, kTb_, qTb_, clampT_, PT_, dd_ = kp[hp], kTb[hp], qTb[hp], clampT[hp], PT[hp], dd[hp]
                    hn_, sq_, rr_, tmpo_, dec_, dSs_ = hn[hp], sq[hp], rr[hp], tmpo[hp], dec[hp], dSs[hp]
                    ckeys = [("cT", par, 2 * h), ("cT", par, 2 * h + 1)]
                    for dc in range(2):
                        S.op("pe", lambda e, dc=dc: e.matmul(psk[:, 0:256], lhsT=cT_[:, 2 * h + dc, :], rhs=wk[:, h, dc, :],
                                                             start=(dc == 0), stop=(dc == 1)),
                             reads=ckeys + ["wk"], writes=["psk"])
                    S.op("dve", lambda e: e.tensor_scalar(out=kp_[:], in0=psk[:, 0:256], scalar1=w4[:, h:h + 1],
                                                          scalar2=None, op0=ALU.mult), reads=["psk", "w4"], writes=[("kp", hp)])
                    yield
                    S.op("pe", lambda e: e.matmul(psMisc[:, 0:128], lhsT=spb[:, h, :], rhs=tri_f[:], start=True, stop=True),
                         reads=["spb", "tri_f"], writes=["ps_cb"])
                    S.op("act", lambda e: e.activation(out=dec_[:], in_=psMisc[:, 127:128], func=AF.Exp, scale=-1.0),
                         reads=["ps_cb"], writes=[("dec", hp)])
                    if own:
                        o0 = (ck - 24) * 128
                        S.op("act", lambda e: e.activation(out=clampT_[:], in_=psMisc[:, 0:128], func=AF.Exp),
                             reads=["ps_cb"], writes=[("clampT", hp)])
                        yield
                        for ec in range(2):
                            for dc in range(2):
                                S.op("pe", lambda e, ec=ec, dc=dc: e.matmul(
                                    pskT[:, ec * 128:(ec + 1) * 128], lhsT=wk[:, h, dc, ec * 128:(ec + 1) * 128],
                                    rhs=cT_[:, 2 * h + dc, :], start=(dc == 0), stop=(dc == 1)),
                                    reads=ckeys + ["wk"], writes=["pskT"])
                        for ec in range(2):
                            for dc in range(2):
                                S.op("pe", lambda e, ec=ec, dc=dc: e.matmul(
                                    psqT[:, ec * 128:(ec + 1) * 128], lhsT=wq[:, h, dc, ec * 128:(ec + 1) * 128],
                                    rhs=cT_[:, 2 * h + dc, :], start=(dc == 0), stop=(dc == 1)),
                                    reads=ckeys + ["wq"], writes=["psqT"])
                        S.op("act", lambda e: e.copy(out=kTb_[:], in_=pskT[:, 0:256].rearrange("p (c t) -> p c t", c=2)),
                             reads=["pskT"], writes=[("kTb", hp)])
                        S.op("dve", lambda e: e.tensor_copy(qTb_[:], psqT[:, 0:256].rearrange("p (c t) -> p c t", c=2)),
                             reads=["psqT"], writes=[("qTb", hp)])
                        yield
                        for ec in range(2):
                            S.op("pe", lambda e, ec=ec: e.matmul(psS[:, 0:128], lhsT=kTb_[:, ec, :], rhs=qTb_[:, ec, :],
                                                                 start=(ec == 0), stop=(ec == 1)),
                                 reads=[("kTb", hp), ("qTb", hp)], writes=["psS"])
                        S.op("act", lambda e: e.copy(out=dd_[:], in_=psS[:, 0:128]), reads=["psS"], writes=[("dd", hp)])
                        S.op("dve", lambda e: e.scalar_tensor_tensor(out=PT_[:], in0=dd_[:], scalar=w4[:, h:h + 1],
                                                                     in1=tri_f[:], op0=ALU.mult, op1=ALU.mult),
                             reads=[("dd", hp), "w4", "tri_f"], writes=[("PT", hp)])
                        yield
                        for j in range(3):
                            S.op("pe", lambda e, j=j: e.matmul(psO[:, j * 128:(j + 1) * 128],
                                                               lhsT=vaug_[:, h, j * 128:(j + 1) * 128], rhs=PT_[:],
                                                               start=True, stop=False),
                                 reads=[("vaug", par), ("PT", hp)], writes=["psO"])
                            for dc in range(2):
                                S.op("pe", lambda e, j=j, dc=dc: e.matmul(
                                    psO[:, j * 128:(j + 1) * 128], lhsT=Sbf[h][:, dc, j * 128:(j + 1) * 128],
                                    rhs=qTb_[:, dc, :], start=False, stop=(dc == 1)),
                                    reads=[("Sbf", h), ("qTb", hp)], writes=["psO"])
                        S.op("act", lambda e: e.activation(out=dd_[:], in_=psO[:, 256:384], func=AF.Abs),
                             reads=["psO"], writes=[("dd", hp)])
                        S.op("act", lambda e: e.copy(out=hn_[:], in_=psO[:, 0:256].rearrange("p (c t) -> p c t", c=2)),
                             reads=["psO"], writes=[("hn", hp)])
                        yield
                        S.op("dve", lambda e: e.tensor_tensor(out=dd_[:], in0=dd_[:], in1=clampT_[:], op=ALU.max),
                             reads=[("dd", hp), ("clampT", hp)], writes=[("dd", hp)])
                        S.op("dve", lambda e: e.reciprocal(dd_[:], dd_[:]), reads=[("dd", hp)], writes=[("dd", hp)])
                        S.op("dve", lambda e: e.tensor_tensor(
                            out=hn_[:], in0=hn_[:], in1=dd_[:].unsqueeze(1).to_broadcast([128, 2, 128]), op=ALU.mult),
                            reads=[("hn", hp), ("dd", hp)], writes=[("hn", hp)])
                        S.op("act", lambda e: e.activation(out=sq_[:], in_=hn_[:], func=AF.Square), reads=[("hn", hp)], writes=[("sq", hp)])
                        for j in range(2):
                            S.op("pe", lambda e, j=j: e.matmul(psMisc[:, 128:256], lhsT=ones_f[:], rhs=sq_[:, j, :],
                                                               start=(j == 0), stop=(j == 1)),
                                 reads=[("sq", hp), "ones_f"], writes=["ps_n"])
                        S.op("dve", lambda e: e.tensor_scalar(out=rr_[:], in0=psMisc[:, 128:256], scalar1=1.0 / 256,
                                                              scalar2=EPS, op0=ALU.mult, op1=ALU.add),
                             reads=["ps_n"], writes=[("rr", hp)])
                        yield
                        S.op("act", lambda e: e.sqrt(rr_[:], rr_[:]), reads=[("rr", hp)], writes=[("rr", hp)])
                        S.op("dve", lambda e: e.reciprocal(rr_[:], rr_[:]), reads=[("rr", hp)], writes=[("rr", hp)])
                        for j in range(2):
                            S.op("dve", lambda e, j=j: e.scalar_tensor_tensor(
                                out=tmpo_[:], in0=hn_[:, j, :], scalar=gm[:, 2 * h + j:2 * h + j + 1], in1=rr_[:],
                                op0=ALU.mult, op1=ALU.mult), reads=[("hn", hp), "gm", ("rr", hp)], writes=[("tmpo", hp)])
                            S.op("pool", lambda e, j=j: e.tensor_tensor(
                                out=mixT[:, 2 * h + j, o0:o0 + 128], in0=tmpo_[:], in1=moT[:, 2 * h + j, o0:o0 + 128],
                                op=ALU.mult), reads=[("tmpo", hp)], writes=[("mixT", 2 * h + j, ck)])
                    yield
                    for dc in range(2):
                        S.op("pe", lambda e, dc=dc: e.matmul(psdS[dc][:, 0:384], lhsT=kp_[:, dc * 128:(dc + 1) * 128],
                                                             rhs=vaug_[:, h, :], start=True, stop=True),
                             reads=[("kp", hp), ("vaug", par)], writes=[("psdS", dc)])
                    for dc in range(2):
                        S.op("dve", lambda e, dc=dc: e.tensor_scalar(out=Sst[h][:, dc, :], in0=Sst[h][:, dc, :],
                                                                     scalar1=dec_[:, 0:1], scalar2=None, op0=ALU.mult),
                             reads=[("Sst", h, dc), ("dec", hp)], writes=[("Sst", h, dc)])
                        S.op("act", lambda e, dc=dc: e.copy(out=dSs_[:, dc, :], in_=psdS[dc][:, 0:384]),
                             reads=[("psdS", dc)], writes=[(("dSs", hp), dc)])
                        S.op("dve", lambda e, dc=dc: e.scalar_tensor_tensor(
                            out=Sst[h][:, dc, :], in0=dSs_[:, dc, :], scalar=dec_[:, 0:1], in1=Sst[h][:, dc, :],
                            op0=ALU.mult, op1=ALU.add), reads=[(("dSs", hp), dc), ("dec", hp), ("Sst", h, dc)],
                            writes=[("Sst", h, dc)])
                    S.op("act", lambda e: e.copy(out=Sbf[h][:], in_=Sst[h][:]),
                         reads=[("Sst", h, 0), ("Sst", h, 1)], writes=[("Sbf", h)])
                for pair in ((0, 1), (2, 3)):
                    gens = [head_gen(h) for h in pair]
                    while gens:
                        for g in list(gens):
                            try:
                                next(g)
                            except StopIteration:
                                gens.remove(g)
            S.barrier()

        with ExitStack() as ds:
            def sb(name, shape, dt=F32):
                return ds.enter_context(nc.sbuf_tensor("E" + name, shape, dt))

            def pst(name):
                return ds.enter_context(nc.psum_tensor("Eps" + name, [128, 512], F32))

            kT = sb("kT", [128, 8, NPRE], BF16)
            S.dma("sp", kT[:], s_akT.rearrange("(c p) t -> p c t", p=128)[:, :, 1024:WIN], writes=["kT"])
            accN = sb("accN", [128, 8, OWN]); accD = sb("accD", [128, 8, OWN])
            VA = [sb(f"VA{i}", [128, 1024], BF16) for i in range(2)]
            VB = [sb(f"VB{i}", [128, 1024], BF16) for i in range(2)]
            two = lambda nm, shape, dt=F32: [sb(f"{nm}{i}", shape, dt) for i in range(2)]
            PA = two("PA", [128, 128], BF16); PB = two("PB", [128, 128], BF16)
            eA = two("eA", [128, 128]); eB = two("eB", [128, 128]); tO = two("tO", [128, 128]); tD = two("tD", [128, 128])
            kbA = sb("kbA", [128, 48]); ga = sb("ga", [128, 8])
            trl = sb("trl", [128, 128]); tru = sb("tru", [128, 128]); ones_b = sb("onesb", [128, 128], BF16)
            ones_f2 = sb("onesf2", [128, 128])
            S.dma("sp", kbA[:], kbA_d[:, :], writes=["kbA"])
            S.dma("sp", ga[:], ga_d[:, :], writes=["ga"])
            S.dma("sp", trl[:], trl_d[:, :], writes=["trl"])
            S.dma("sp", tru[:], tri_d[:, :], writes=["tru"])
            S.op("pool", lambda e: e.memset(ones_b[:], 1.0), writes=["ones_b"])
            S.op("pool", lambda e: e.memset(ones_f2[:], 1.0), writes=["ones_f2"])
            psA = [pst("A0"), pst("A1")]; psB = [pst("B0"), pst("B1")]
            psO = [pst("O0"), pst("O1")]; psD = [pst("D0"), pst("D1")]
            psN = psA[0]
            SC = 128.0 ** -0.5
            gi = 0
            for Q in range(2):
                glist = [(1, 0, sub) for sub in range(4)] + [(4, r, 0) for r in range(4)] + [(16, r, 0) for r in range(16)]
                for (d, r, sub) in glist:
                    nq = 128 if d < 16 else 32
                    u0 = 2048 + 512 * Q + r + 128 * sub
                    i0 = 512 * Q + r + 128 * sub
                    uA = u0 - 128 * d
                    par = gi % 2
                    va, vb = VA[par], VB[par]
                    S.dma("sp", va[:], bass.AP(tensor=s_av.tensor, offset=(1024 + uA) * 1024, ap=[[d * 1024, 128], [1, 1024]]),
                          writes=[("VA", par)])
                    S.dma("sp", vb[0:nq, :], bass.AP(tensor=s_av.tensor, offset=(1024 + u0) * 1024, ap=[[d * 1024, nq], [1, 1024]]),
                          writes=[("VB", par)])
                    qsl = slice(i0, i0 + (nq - 1) * d + 1, d)
                    def st1(h):
                        p2 = h % 2
                        q_ap = aqT[:, h, qsl]
                        S.op("pe", lambda e: e.matmul(psA[p2][:, 0:nq], lhsT=kT[:, h, uA:uA + 127 * d + 1:d], rhs=q_ap,
                                                      start=True, stop=True), reads=["kT"], writes=[("psA", p2)])
                        S.op("pe", lambda e: e.matmul(psB[p2][0:nq, 0:nq], lhsT=kT[:, h, u0:u0 + (nq - 1) * d + 1:d], rhs=q_ap,
                                                      start=True, stop=True), reads=["kT"], writes=[("psB", p2)])

                    def st2(h):
                        p2 = h % 2
                        S.op("act", lambda e: e.activation(out=eA[p2][:, 0:nq], in_=psA[p2][:, 0:nq], func=AF.Exp, scale=SC,
                                                           bias=kbA[:, gi:gi + 1]), reads=[("psA", p2), "kbA"], writes=[("eA", p2)])
                        S.op("act", lambda e: e.activation(out=eB[p2][0:nq, 0:nq], in_=psB[p2][0:nq, 0:nq], func=AF.Exp, scale=SC),
                             reads=[("psB", p2)], writes=[("eB", p2)])
                        S.op("dve", lambda e: e.tensor_tensor(out=PA[p2][:, 0:nq], in0=eA[p2][:, 0:nq], in1=trl[:, 0:nq], op=ALU.mult),
                             reads=[("eA", p2), "trl"], writes=[("PA", p2)])
                        S.op("dve", lambda e: e.tensor_tensor(out=PB[p2][0:nq, 0:nq], in0=eB[p2][0:nq, 0:nq], in1=tru[0:nq, 0:nq],
                                                              op=ALU.mult), reads=[("eB", p2), "tru"], writes=[("PB", p2)])

                    def st3(h):
                        p2 = h % 2
                        S.op("pe", lambda e: e.matmul(psO[p2][:, 0:nq], lhsT=va[:, h * 128:(h + 1) * 128], rhs=PA[p2][:, 0:nq],
                                                      start=True, stop=False), reads=[("VA", par), ("PA", p2)], writes=[("psO", p2)])
                        S.op("pe", lambda e: e.matmul(psO[p2][:, 0:nq], lhsT=vb[0:nq, h * 128:(h + 1) * 128], rhs=PB[p2][0:nq, 0:nq],
                                                      start=False, stop=True), reads=[("VB", par), ("PB", p2)], writes=[("psO", p2)])
                        S.op("pe", lambda e: e.matmul(psD[p2][:, 0:nq], lhsT=ones_b[:, :], rhs=PA[p2][:, 0:nq],
                                                      start=True, stop=False), reads=["ones_b", ("PA", p2)], writes=[("psD", p2)])
                        S.op("pe", lambda e: e.matmul(psD[p2][:, 0:nq], lhsT=ones_b[0:nq, :], rhs=PB[p2][0:nq, 0:nq],
                                                      start=False, stop=True), reads=["ones_b", ("PB", p2)], writes=[("psD", p2)])

                    def st4(h):
                        p2 = h % 2
                        akey = ("acc", h, Q)
                        if d == 1:
                            S.op("act", lambda e: e.copy(out=accN[:, h, qsl], in_=psO[p2][:, 0:nq]), reads=[("psO", p2)], writes=[akey])
                            S.op("act", lambda e: e.copy(out=accD[:, h, qsl], in_=psD[p2][:, 0:nq]), reads=[("psD", p2)], writes=[akey])
                        else:
                            S.op("act", lambda e: e.copy(out=tO[p2][:, 0:nq], in_=psO[p2][:, 0:nq]), reads=[("psO", p2)], writes=[("tO", p2)])
                            S.op("act", lambda e: e.copy(out=tD[p2][:, 0:nq], in_=psD[p2][:, 0:nq]), reads=[("psD", p2)], writes=[("tD", p2)])
                            S.op("dve", lambda e: e.tensor_tensor(out=accN[:, h, qsl], in0=accN[:, h, qsl], in1=tO[p2][:, 0:nq],
                                                                  op=ALU.add), reads=[("tO", p2), akey], writes=[akey])
                            S.op("dve", lambda e: e.tensor_tensor(out=accD[:, h, qsl], in0=accD[:, h, qsl], in1=tD[p2][:, 0:nq],
                                                                  op=ALU.add), reads=[("tD", p2), akey], writes=[akey])

                    for i_ in range(9):
                        if i_ < 8:
                            st1(i_)
                            st2(i_)
                        if i_ >= 1:
                            st3(i_ - 1)
                            st4(i_ - 1)
                    gi += 1
            o5 = sb("o5", [128, 512]); sq5 = sb("sq5", [128, 512]); r5 = sb("r5", [128, 512])
            for h in range(8):
                for Q in range(2):
                    cs = slice(Q * 512, (Q + 1) * 512)
                    akey = ("acc", h, Q)
                    S.op("dve", lambda e: e.reciprocal(r5[:], accD[:, h, cs]), reads=[akey], writes=["r5"])
                    S.op("dve", lambda e: e.tensor_tensor(out=o5[:], in0=accN[:, h, cs], in1=r5[:], op=ALU.mult),
                         reads=[akey, "r5"], writes=["o5"])
                    S.op("act", lambda e: e.activation(out=sq5[:], in_=o5[:], func=AF.Square), reads=["o5"], writes=["sq5"])
                    S.op("pe", lambda e: e.matmul(psN[:, :], lhsT=ones_f2[:], rhs=sq5[:], start=True, stop=True),
                         reads=["ones_f2", "sq5"], writes=[("psA", 0)])
                    S.op("dve", lambda e: e.tensor_scalar(out=r5[:], in0=psN[:, :], scalar1=1.0 / 128, scalar2=EPS,
                                                          op0=ALU.mult, op1=ALU.add), reads=[("psA", 0)], writes=["r5"])
                    S.op("act", lambda e: e.sqrt(r5[:], r5[:]), reads=["r5"], writes=["r5"])
                    S.op("dve", lambda e: e.reciprocal(r5[:], r5[:]), reads=["r5"], writes=["r5"])
                    S.op("dve", lambda e: e.scalar_tensor_tensor(out=mixT[:, 8 + h, cs], in0=o5[:], scalar=ga[:, h:h + 1],
                                                                 in1=r5[:], op0=ALU.mult, op1=ALU.mult),
                         reads=["o5", "ga", "r5"], writes=[("mixT", 8 + h, Q)])
            S.barrier()

        if dbg is not None and dbg["name"] == "mixT":
            S.dma("sp", dbg_out.rearrange("(c p) t -> p c t", p=128), mixT[:], writes=["dbgo"])
            S.barrier()

        def norm_sb(P, x_ap, xkeys, gbc, hT, col0, hkey, xn=None):
            t = P["tag"]
            xb, ss, psT = P["xb"], P["ss"], P["psT"]
            S.op("act", lambda e: e.activation(out=xb[:], in_=x_ap, func=AF.Square, scale=float(D) ** -0.5,
                                               accum_out=ss[:]), reads=xkeys, writes=[t + "xb", t + "ss"])
            S.op("dve", lambda e: e.tensor_scalar_add(ss[:], ss[:], EPS), reads=[t + "ss"], writes=[t + "ss"])
            S.op("act", lambda e: e.sqrt(ss[:], ss[:]), reads=[t + "ss"], writes=[t + "ss"])
            S.op("dve", lambda e: e.reciprocal(ss[:], ss[:]), reads=[t + "ss"], writes=[t + "ss"])
            if xn is not None:
                S.op("dve", lambda e: e.scalar_tensor_tensor(out=xn[:], in0=x_ap, scalar=ss[:, 0:1], in1=gbc[:],
                                                             op0=ALU.mult, op1=ALU.mult),
                     reads=list(xkeys) + [t + "ss", t + "gbc"], writes=[t + "xn"])
                if hT is not None:
                    S.op("pool", lambda e: e.tensor_copy(xb[:], xn[:]), reads=[t + "xn"], writes=[t + "xb"])
            else:
                S.op("dve", lambda e: e.scalar_tensor_tensor(out=xb[:], in0=x_ap, scalar=ss[:, 0:1], in1=gbc[:],
                                                             op0=ALU.mult, op1=ALU.mult),
                     reads=list(xkeys) + [t + "ss", t + "gbc"], writes=[t + "xb"])
            if hT is None:
                return
            for half in range(2):
                for j in range(8):
                    c = half * 8 + j
                    S.op("pe", lambda e, c=c, j=j, half=half: e.transpose(
                        psT[half][:, j * 128:(j + 1) * 128], xb[:, c * 128:(c + 1) * 128], ident[:]),
                        reads=[t + "xb"], writes=[(t + "psT", half)])
                if half == 0:
                    S.op("act", lambda e, half=half: e.copy(
                        out=hT[:, half * 8:(half + 1) * 8, col0:col0 + 128],
                        in_=psT[half][:].rearrange("p (c t) -> p c t", c=8)),
                        reads=[(t + "psT", half)], writes=[hkey])
                else:
                    S.op("dve", lambda e, half=half: e.tensor_copy(
                        hT[:, half * 8:(half + 1) * 8, col0:col0 + 128],
                        psT[half][:].rearrange("p (c t) -> p c t", c=8)),
                        reads=[(t + "psT", half)], writes=[hkey])

        def load_wblk2(t, w_dram, b2, stages, wbs, kc):
            wv = w_dram.rearrange("(c p) n -> p c n", p=128)
            wb = wbs[b2 % 2]
            for c4 in range(4):
                S.dma("pool", wb[:, c4 * 4:(c4 + 1) * 4, :], wv[:, c4 * 4:(c4 + 1) * 4, b2 * 256:(b2 + 1) * 256],
                      writes=[(t + "wb", b2 % 2, c4)])
            return wb, [(t + "wb", b2 % 2, c4) for c4 in range(4)]

        def load_wblk(t, w_dram, nb, stage, wb):
            wv = w_dram.rearrange("(c p) n -> p c n", p=128)
            for c4 in range(4):
                S.dma("sp", stage[:], wv[:, c4 * 4:(c4 + 1) * 4, nb * 512:(nb + 1) * 512], writes=[t + "stage"])
                S.op("pool" if c4 % 2 == 0 else "dve",
                     lambda e, c4=c4: e.tensor_copy(wb[:, c4 * 4:(c4 + 1) * 4, :], stage[:]),
                     reads=[t + "stage"], writes=[(t + "wb", c4)])
            return [(t + "wb", c4) for c4 in range(4)]

        xres = es.enter_context(nc.sbuf_tensor("xres", [128, 8, D], F32))
        for tt in range(8):
            S.dma("sp", xres[:, tt, :], x_win[NPRE + tt * 128:NPRE + (tt + 1) * 128, :], writes=[("xres", tt)])

        def add_proj(tagp, actT, w_dram):
            with ExitStack() as fs:
                stage = [fs.enter_context(nc.sbuf_tensor(f"{tagp}st{i}", [128, 4, 512], F32)) for i in range(2)]
                wblk = [fs.enter_context(nc.sbuf_tensor(f"{tagp}wb{i}", [128, NCH, 512], BF16)) for i in range(2)]
                tmp = [fs.enter_context(nc.sbuf_tensor(f"{tagp}tmp{i}", [128, 512], F32)) for i in range(2)]
                psF = [fs.enter_context(nc.psum_tensor(f"{tagp}ps{i}", [128, 512], F32)) for i in range(2)]
                wv = w_dram.rearrange("(c p) n -> p c n", p=128)
                k = 0
                n = 0
                for nb in range(4):
                    wb = wblk[nb % 2]
                    for c4 in range(4):
                        S.dma("pool", wb[:, c4 * 4:(c4 + 1) * 4, :], wv[:, c4 * 4:(c4 + 1) * 4, nb * 512:(nb + 1) * 512],
                              writes=[(tagp + "wb", nb % 2, c4)])
                        k += 1
                    for tt in range(8):
                        ps = psF[n % 2]
                        tm = tmp[n % 2]
                        for c in range(NCH):
                            S.op("pe", lambda e, c=c, ps=ps, wb=wb, tt=tt: e.matmul(
                                ps[:, :], lhsT=actT[:, c, tt * 128:(tt + 1) * 128], rhs=wb[:, c, :],
                                start=(c == 0), stop=(c == NCH - 1)),
                                reads=[(tagp + "wb", nb % 2, c // 4), (tagp + "act", c)], writes=[(tagp + "ps", n % 2)])
                        S.op("act", lambda e, ps=ps, tm=tm: e.copy(out=tm[:], in_=ps[:, :]),
                             reads=[(tagp + "ps", n % 2)], writes=[(tagp + "tmp", n % 2)])
                        S.op("dve", lambda e, tm=tm, tt=tt, nb=nb: e.tensor_tensor(
                            out=xres[:, tt, nb * 512:(nb + 1) * 512], in0=xres[:, tt, nb * 512:(nb + 1) * 512],
                            in1=tm[:], op=ALU.add), reads=[(tagp + "tmp", n % 2), ("xres", tt)], writes=[("xres", tt)])
                        n += 1
                S.barrier()

        add_proj("F", mixT, w_out_d)
        with ExitStack() as gs:
            def sb(name, shape, dt=F32):
                return gs.enter_context(nc.sbuf_tensor("G" + name, shape, dt))

            def pst(name, dt=F32, n=512):
                return gs.enter_context(nc.psum_tensor("Gps" + name, [128, n], dt))

            P = dict(tag="G", xb=sb("xb", [128, D], BF16), ss=sb("ss", [128, 1]),
                     psT=[pst(f"T{i}", BF16, 1024) for i in range(2)])
            gbc = sb("gbc", [128, D])
            mnT = sb("mnT", [128, NCH, 256], BF16); kmT = sb("kmT", [128, NCH, 256], BF16)
            vm = sb("vm", [128, 2, D], BF16)
            hT = moT[:].rearrange("p a b -> p (a b)").rearrange("p (c t) -> p c t", c=NCH)
            qT = aqT[:].rearrange("p a b -> p (a b)").rearrange("p (c t) -> p c t", c=NCH)
            stage_all = sb("stage", [128, 2, 1024])
            stages = [stage_all[:, i, :].rearrange("p (c n) -> p c n", c=4) for i in range(2)]
            wbs = [sb(f"wbk{i}", [128, NCH, 256], BF16) for i in range(2)]
            kc = [0]
            onesb = sb("onesb", [128, 128], BF16)
            PTm = [sb(f"PT{i}", [128, 512], BF16) for i in range(2)]
            rD = sb("rD", [128, 512]); tO = sb("tO", [128, 512])
            psQ = [pst("Q0"), pst("Q1")]; psS = [pst("S0"), pst("S1")]; psO = pst("O"); psD = pst("D")
            S.op("pool", lambda e: e.memset(onesb[:], 1.0), writes=["Gonesb"])
            S.dma("sp", gbc[:], g_mem_d[0:1, :].partition_broadcast(128), writes=["Ggbc"])
            stage_flat = stage_all[:].rearrange("p a b -> p (a b)")
            for mt in range(2):
                S.dma("sp", stage_flat, mem_d[mt * 128:(mt + 1) * 128, :], writes=[("Gstage", 0), ("Gstage", 1)])
                norm_sb(P, stage_flat, [("Gstage", 0), ("Gstage", 1)], gbc, mnT, mt * 128, ("GmnT", mt))
            mnkeys = [("GmnT", 0), ("GmnT", 1)]
            nq_ = 0
            for b2 in range(8):
                wb, wk_ = load_wblk2("G", w_xk_d, b2, stages, wbs, kc)
                for e4 in range(2):
                    ec = b2 * 2 + e4
                    ps = psQ[nq_ % 2]; pk = ("GpsQ", nq_ % 2); nq_ += 1
                    for c in range(NCH):
                        S.op("pe", lambda e, c=c, e4=e4, ps=ps: e.matmul(ps[:, 0:256], lhsT=wb[:, c, e4 * 128:(e4 + 1) * 128],
                                                                         rhs=mnT[:, c, :], start=(c == 0), stop=(c == NCH - 1)),
                             reads=wk_ + mnkeys, writes=[pk])
                    S.op("act", lambda e, ec=ec, ps=ps: e.copy(out=kmT[:, ec, :], in_=ps[:, 0:256]), reads=[pk],
                         writes=[("GkmT", ec)])
            for b2 in range(8):
                wb, wk_ = load_wblk2("G", w_xv_d, b2, stages, wbs, kc)
                for mt in range(2):
                    ps = psQ[nq_ % 2]; pk = ("GpsQ", nq_ % 2); nq_ += 1
                    for c in range(NCH):
                        S.op("pe", lambda e, c=c, mt=mt, ps=ps: e.matmul(ps[:, 0:256], lhsT=mnT[:, c, mt * 128:(mt + 1) * 128],
                                                                         rhs=wb[:, c, :], start=(c == 0), stop=(c == NCH - 1)),
                             reads=wk_ + mnkeys, writes=[pk])
                    S.op("act", lambda e, mt=mt, b2=b2, ps=ps: e.copy(out=vm[:, mt, b2 * 256:(b2 + 1) * 256], in_=ps[:, 0:256]),
                         reads=[pk], writes=[("Gvm", mt, b2)])
            S.dma("sp", gbc[:], g_cross_d[0:1, :].partition_broadcast(128), writes=["Ggbc"])
            SCX = 512.0 ** -0.5
            kmkeys = [("GkmT", ec) for ec in range(NCH)]
            vmkeys = [("Gvm", mt, b2) for mt in range(2) for b2 in range(8)]
            for st in range(2):
                for sub in range(4):
                    tt = st * 4 + sub
                    norm_sb(P, xres[:, tt, :], [("xres", tt)], gbc, hT, sub * 128, "GhT")
                for b2 in range(8):
                    wb, wk_ = load_wblk2("G", w_xq_d, b2, stages, wbs, kc)
                    for e4 in range(2):
                        ec = b2 * 2 + e4
                        ps = psQ[nq_ % 2]; pk = ("GpsQ", nq_ % 2); nq_ += 1
                        for c in range(NCH):
                            S.op("pe", lambda e, c=c, e4=e4, ps=ps: e.matmul(ps[:, :], lhsT=wb[:, c, e4 * 128:(e4 + 1) * 128],
                                                                             rhs=hT[:, c, :], start=(c == 0), stop=(c == NCH - 1)),
                                 reads=wk_ + ["GhT"], writes=[pk])
                        if ec % 2 == 0:
                            S.op("act", lambda e, ec=ec, ps=ps: e.copy(out=qT[:, ec, :], in_=ps[:, :]), reads=[pk],
                                 writes=[("GqT", ec)])
                        else:
                            S.op("dve", lambda e, ec=ec, ps=ps: e.tensor_copy(qT[:, ec, :], ps[:, :]), reads=[pk],
                                 writes=[("GqT", ec)])
                for hd in range(4):
                    ecs = range(hd * 4, hd * 4 + 4)
                    qk = [("GqT", ec) for ec in ecs]
                    for mt in range(2):
                        for i_, ec in enumerate(ecs):
                            S.op("pe", lambda e, mt=mt, ec=ec, i_=i_: e.matmul(
                                psS[mt][:, :], lhsT=kmT[:, ec, mt * 128:(mt + 1) * 128], rhs=qT[:, ec, :],
                                start=(i_ == 0), stop=(i_ == 3)), reads=qk + kmkeys, writes=[("GpsS", mt)])
                        S.op("act", lambda e, mt=mt: e.activation(out=PTm[mt][:], in_=psS[mt][:, :], func=AF.Exp, scale=SCX),
                             reads=[("GpsS", mt)], writes=[("GPT", mt)])
                    ptk = [("GPT", 0), ("GPT", 1)]
                    for mt in range(2):
                        S.op("pe", lambda e, mt=mt: e.matmul(psD[:, :], lhsT=onesb[:], rhs=PTm[mt][:],
                                                             start=(mt == 0), stop=(mt == 1)),
                             reads=ptk + ["Gonesb"], writes=["GpsD"])
                    S.op("act", lambda e: e.copy(out=rD[:], in_=psD[:, :]), reads=["GpsD"], writes=["GrD"])
                    S.op("dve", lambda e: e.reciprocal(rD[:], rD[:]), reads=["GrD"], writes=["GrD"])
                    for ec in ecs:
                        for mt in range(2):
                            S.op("pe", lambda e, mt=mt, ec=ec: e.matmul(psO[:, :], lhsT=vm[:, mt, ec * 128:(ec + 1) * 128],
                                                                        rhs=PTm[mt][:], start=(mt == 0), stop=(mt == 1)),
                                 reads=ptk + vmkeys, writes=["GpsO"])
                        S.op("act", lambda e: e.copy(out=tO[:], in_=psO[:, :]), reads=["GpsO"], writes=["GtO"])
                        S.op("dve", lambda e, ec=ec: e.tensor_tensor(out=mixT[:, ec, st * 512:(st + 1) * 512], in0=tO[:],
                                                                     in1=rD[:], op=ALU.mult),
                             reads=["GtO", "GrD"], writes=[("Gact", ec, st)])
            S.barrier()
        add_proj("G", mixT, w_xo_d)

        if dbg is not None and dbg["name"] == "xresG":
            S.dma("sp", dbg_out.rearrange("(t p) n -> p t n", p=128), xres[:], writes=["dbgo"])
            S.barrier()
            return nc
        ids_all = es.enter_context(nc.sbuf_tensor("ids_all", [128, 8, 128], I32))
        gates_all = es.enter_context(nc.sbuf_tensor("gates_all", [128, 8, 128], F32))
        with ExitStack() as hs:
            def sb(name, shape, dt=F32):
                return hs.enter_context(nc.sbuf_tensor("H" + name, shape, dt))

            def pst(name, dt=F32, n=512):
                return hs.enter_context(nc.psum_tensor("Hps" + name, [128, n], dt))

            P = dict(tag="H", xb=sb("xb", [128, D], BF16), ss=sb("ss", [128, 1]),
                     psT=[pst(f"T{i}", BF16, 1024) for i in range(2)])
            gbc = sb("gbc", [128, D])
            S.dma("sp", gbc[:], g_ffn_d[0:1, :].partition_broadcast(128), writes=["Hgbc"])
            hT = moT[:].rearrange("p a b -> p (a b)").rearrange("p (c t) -> p c t", c=NCH)
            qT = aqT[:].rearrange("p a b -> p (a b)").rearrange("p (c t) -> p c t", c=NCH)
            stage_all = sb("stage", [128, 2, 1024])
            stages = [stage_all[:, i, :].rearrange("p (c n) -> p c n", c=4) for i in range(2)]
            wbs = [sb(f"wbk{i}", [128, NCH, 256], BF16) for i in range(2)]
            kc = [0]
            skT = sb("skT", [128, 16, 128], BF16)
            stage4 = stage_all[:].rearrange("p a b -> p (a b)").rearrange("p (a b) -> p a b", a=4)
            S.dma("sp", stage4, skT_d[:, :, :].rearrange("p (a b) k -> p a (b k)", a=4),
                  writes=[("Hstage", 0), ("Hstage", 1)])
            S.op("dve", lambda e: e.tensor_copy(skT[:].rearrange("p (a b) k -> p a (b k)", a=4), stage4),
                 reads=[("Hstage", 0), ("Hstage", 1)], writes=["HskT"])
            sc = sb("sc", [128, 16, 128]); wk1 = sb("wk1", [128, 128])
            ts = sb("ts", [128, 16, 16]); ti = sb("ti", [128, 16, 16], U32); tif = sb("tif", [128, 16, 16])
            cand = sb("cand", [128, 16, 16]); cid = sb("cid", [128, 16, 16]); wk2 = sb("wk2", [128, 256])
            junk = sb("junk", [128, 256]); bs = sb("bs", [128, 16]); negm = sb("negm", [128, 1])
            ge = sb("ge", [128, 16]); zz = sb("zz", [128, 1]); idf = sb("idf", [128, 128])
            pos = sb("pos", [128, 16], U32); posf = sb("posf", [128, 16]); iota_t = sb("iota", [128, 256])
            S.dma("sp", iota_t[:], iota_d[0:1, :].partition_broadcast(128), writes=["Hiota"])
            psQ = [pst("Q0"), pst("Q1")]; psC = [pst("C0"), pst("C1")]
            nq_ = 0
            for st in range(2):
                for sub in range(4):
                    tt = st * 4 + sub
                    norm_sb(P, xres[:, tt, :], [("xres", tt)], gbc, hT, sub * 128, "HhT")
                for b2 in range(8):
                    wb, wk_ = load_wblk2("H", w_pq_d, b2, stages, wbs, kc)
                    for e4 in range(2):
                        j = b2 * 2 + e4
                        ps = psQ[nq_ % 2]; pk = ("HpsQ", nq_ % 2); nq_ += 1
                        for c in range(NCH):
                            S.op("pe", lambda e, c=c, e4=e4, ps=ps: e.matmul(ps[:, :], lhsT=wb[:, c, e4 * 128:(e4 + 1) * 128],
                                                                             rhs=hT[:, c, :], start=(c == 0), stop=(c == NCH - 1)),
                                 reads=wk_ + ["HhT"], writes=[pk])
                        if j % 2 == 0:
                            S.op("act", lambda e, j=j, ps=ps: e.copy(out=qT[:, j, :], in_=ps[:, :]), reads=[pk],
                                 writes=[("HqT", j)])
                        else:
                            S.op("dve", lambda e, j=j, ps=ps: e.tensor_copy(qT[:, j, :], ps[:, :]), reads=[pk],
                                 writes=[("HqT", j)])
                for sub in range(4):
                    tt = st * 4 + sub
                    for jb in range(4):
                        pc = psC[jb % 2]; pck = ("HpsC", jb % 2)
                        for jj in range(4):
                            j = jb * 4 + jj
                            S.op("pe", lambda e, j=j, jj=jj, pc=pc: e.matmul(
                                pc[:, jj * 128:(jj + 1) * 128], lhsT=qT[:, j, sub * 128:(sub + 1) * 128], rhs=skT[:, j, :],
                                start=True, stop=True), reads=[("HqT", j), "HskT"], writes=[pck])
                        S.op("act", lambda e, jb=jb, pc=pc: e.copy(
                            out=sc[:, jb * 4:(jb + 1) * 4, :], in_=pc[:, :].rearrange("p (a k) -> p a k", a=4)),
                            reads=[pck], writes=[("Hsc", jb)])
                    for j in range(16):
                        sk_ = ("Hsc", j // 4)
                        S.op("dve", lambda e, j=j: e.max(out=ts[:, j, 0:8], in_=sc[:, j, :]), reads=[sk_], writes=[("Hts", j, 0)])
                        S.op("dve", lambda e, j=j: e.match_replace(out=wk1[:], in_to_replace=ts[:, j, 0:8],
                                                                   in_values=sc[:, j, :], imm_value=-1e30),
                             reads=[sk_, ("Hts", j, 0)], writes=["Hwk1"])
                        S.op("dve", lambda e, j=j: e.max(out=ts[:, j, 8:16], in_=wk1[:]), reads=["Hwk1"], writes=[("Hts", j, 1)])
                        S.op("dve", lambda e, j=j: e.max_index(out=ti[:, j, 0:8], in_max=ts[:, j, 0:8], in_values=sc[:, j, :]),
                             reads=[sk_, ("Hts", j, 0)], writes=[("Hti", j, 0)])
                        S.op("dve", lambda e, j=j: e.max_index(out=ti[:, j, 8:16], in_max=ts[:, j, 8:16], in_values=wk1[:]),
                             reads=["Hwk1", ("Hts", j, 1)], writes=[("Hti", j, 1)])
                    S.op("dve", lambda e: e.tensor_copy(tif[:], ti[:]),
                         reads=[("Hti", j, q) for j in range(16) for q in range(2)], writes=["Htif"])
                    for h in range(8):
                        j0, j1 = 2 * h, 2 * h + 1
                        tsk = [("Hts", j0, 0), ("Hts", j0, 1), ("Hts", j1, 0), ("Hts", j1, 1)]
                        S.op("dve", lambda e: e.tensor_tensor(
                            out=cand[:], in0=ts[:, j0, :].unsqueeze(2).to_broadcast([128, 16, 16]),
                            in1=ts[:, j1, :].unsqueeze(1).to_broadcast([128, 16, 16]), op=ALU.add),
                            reads=tsk, writes=["Hcand"])
                        S.op("dve", lambda e: e.scalar_tensor_tensor(
                            out=cid[:], in0=tif[:, j0, :].unsqueeze(2).to_broadcast([128, 16, 16]), scalar=128.0,
                            in1=tif[:, j1, :].unsqueeze(1).to_broadcast([128, 16, 16]), op0=ALU.mult, op1=ALU.add),
                            reads=["Htif"], writes=["Hcid"])
                        candf = cand[:].rearrange("p a b -> p (a b)")
                        cidf = cid[:].rearrange("p a b -> p (a b)")
                        S.op("dve", lambda e: e.max(out=bs[:, 0:8], in_=candf), reads=["Hcand"], writes=["Hbs0"])
                        S.op("dve", lambda e: e.match_replace(out=wk2[:], in_to_replace=bs[:, 0:8], in_values=candf,
                                                              imm_value=-1e30), reads=["Hcand", "Hbs0"], writes=["Hwk2"])
                        S.op("dve", lambda e: e.max(out=bs[:, 8:16], in_=wk2[:]), reads=["Hwk2"], writes=["Hbs1"])
                        S.op("dve", lambda e: e.max_index(out=pos[:, 0:8], in_max=bs[:, 0:8], in_values=candf),
                             reads=["Hcand", "Hbs0"], writes=["Hpos0"])
                        S.op("dve", lambda e: e.max_index(out=pos[:, 8:16], in_max=bs[:, 8:16], in_values=wk2[:]),
                             reads=["Hwk2", "Hbs1"], writes=["Hpos1"])
                        S.op("dve", lambda e: e.tensor_copy(posf[:], pos[:]), reads=["Hpos0", "Hpos1"], writes=["Hposf"])
                        for k in range(16):
                            S.op("dve", lambda e, k=k: e.scalar_tensor_tensor(
                                out=junk[:], in0=iota_t[:], scalar=posf[:, k:k + 1], in1=cidf, op0=ALU.is_equal, op1=ALU.mult,
                                accum_out=idf[:, h * 16 + k:h * 16 + k + 1]),
                                reads=["Hiota", "Hcid", "Hposf"], writes=["Hjunk", ("Hidf", h)])
                        S.op("dve", lambda e: e.tensor_scalar_mul(negm[:], bs[:, 0:1], -1.0), reads=["Hbs0"], writes=["Hnegm"])
                        S.op("act", lambda e: e.activation(out=ge[:], in_=bs[:], func=AF.Exp, bias=negm[:, 0:1], accum_out=zz[:]),
                             reads=["Hbs0", "Hbs1", "Hnegm"], writes=["Hge", "Hzz"])
                        S.op("dve", lambda e: e.reciprocal(zz[:], zz[:]), reads=["Hzz"], writes=["Hzz"])
                        S.op("dve", lambda e: e.tensor_scalar(out=gates_all[:, tt, h * 16:(h + 1) * 16], in0=ge[:],
                                                              scalar1=zz[:, 0:1], scalar2=None, op0=ALU.mult),
                             reads=["Hge", "Hzz"], writes=[("gates", tt, h)])
                    S.op("dve", lambda e: e.tensor_copy(ids_all[:, tt, :], idf[:]),
                         reads=[("Hidf", h) for h in range(8)], writes=[("ids", tt)])
            S.barrier()

        if dbg is not None and dbg["name"] == "peer_ids":
            S.dma("sp", dbg_out[0:128, :].rearrange("p (t k) -> p t k", t=8), ids_all[:].bitcast(F32), writes=["dbgo"])
            S.dma("sp", dbg_out[128:256, :].rearrange("p (t k) -> p t k", t=8), gates_all[:], writes=["dbgo2"])
            S.barrier()
            return nc

        with ExitStack() as hs:
            def sb(name, shape, dt=F32):
                return hs.enter_context(nc.sbuf_tensor("Hb" + name, shape, dt))

            P = dict(tag="Hb", xb=sb("xb", [128, D], BF16), ss=sb("ss", [128, 1]), psT=None)
            gbc = sb("gbc", [128, D])
            S.dma("sp", gbc[:], g_ffn_d[0:1, :].partition_broadcast(128), writes=["Hbgbc"])
            xn2 = [sb(f"xn{i}", [128, D]) for i in range(2)]
            actc = sb("actc", [128, 128]); cf = sb("cf", [128, 128]); tmpv = sb("tmpv", [128, D])
            dg = [sb(f"dg{i}", [128, 128], BF16) for i in range(4)]
            NG = 8
            fence_t = sb("fence", [128, 1])
            gall = mixT[:].rearrange("p a b -> p (a b)")
            gb_ = [gall[:, i * 2 * D:(i + 1) * 2 * D] for i in range(4)]
            gb_ += [moT[:].rearrange("p a b -> p (a b)")[:, i * 2 * D:(i + 1) * 2 * D] for i in range(2)]
            gb_ += [aqT[:].rearrange("p a b -> p (a b)")[:, i * 2 * D:(i + 1) * 2 * D] for i in range(2)]
            psV = [[hs.enter_context(nc.psum_tensor(f"HbpsV{q}_{n}", [128, 512], F32)) for n in range(4)] for q in range(2)]
            ng = 0
            for tt in range(8):
                xn = xn2[tt % 2]
                P["tag"] = f"Hb{tt % 2}"
                S.op("act", lambda e, tt=tt: e.activation(out=P["xb"][:], in_=xres[:, tt, :], func=AF.Square,
                                                          scale=float(D) ** -0.5, accum_out=P["ss"][:]),
                     reads=[("xres", tt)], writes=["Hbxb", "Hbss"])
                S.op("dve", lambda e: e.tensor_scalar_add(P["ss"][:], P["ss"][:], EPS), reads=["Hbss"], writes=["Hbss"])
                S.op("act", lambda e: e.sqrt(P["ss"][:], P["ss"][:]), reads=["Hbss"], writes=["Hbss"])
                S.op("dve", lambda e: e.reciprocal(P["ss"][:], P["ss"][:]), reads=["Hbss"], writes=["Hbss"])
                S.op("dve", lambda e, tt=tt, xn=xn: e.scalar_tensor_tensor(out=xn[:], in0=xres[:, tt, :], scalar=P["ss"][:, 0:1],
                                                                    in1=gbc[:], op0=ALU.mult, op1=ALU.mult),
                     reads=[("xres", tt), "Hbss", "Hbgbc"], writes=[("Hbxn", tt % 2)])
                pv = psV[tt % 2]

                def chain(slot, bq):
                    b_, q_ = bq
                    S.op("act", lambda e: e.activation(out=cf[:, slot:slot + 1], in_=actc[:, slot:slot + 1], func=AF.Gelu),
                         reads=[("Hbact", slot), "Hbfence"], writes=[("Hbcf", slot)])
                    S.op("act", lambda e: e.mul(cf[:, slot:slot + 1], cf[:, slot:slot + 1], gates_all[:, tt, slot:slot + 1]),
                         reads=[("Hbcf", slot)], writes=[("Hbcf", slot)])
                    S.op("act", lambda e: e.activation(out=dg[q_][:], in_=ident[:], func=AF.Copy, scale=cf[:, slot:slot + 1]),
                         reads=[("Hbcf", slot)], writes=[("Hbdg", q_)])
                    for n in range(4):
                        S.op("pe", lambda e, n=n: e.matmul(
                            pv[n][:, :], lhsT=dg[q_][:], rhs=gb_[b_][:, D + n * 512:D + (n + 1) * 512],
                            start=(slot == 0), stop=(slot == 127)),
                            reads=[("Hbdg", q_), ("gbuf", b_)], writes=[("HbpsV", tt % 2, n)])

                prev = None
                for slot in range(128):
                    b_ = ng % NG
                    q_ = ng % 4
                    ng += 1
                    S.dma("pool", None, None, reads=[("ids", tt)], writes=[("gbuf", b_)],
                          fn=lambda e, b_=b_, slot=slot, tt=tt: e.indirect_dma_start(
                              out=gb_[b_], out_offset=None, in_=uv_d[:, :],
                              in_offset=bass.IndirectOffsetOnAxis(ap=ids_all[:, tt, slot:slot + 1], axis=0)))
                    S.op("dve", lambda e, b_=b_, slot=slot, xn=xn: e.scalar_tensor_tensor(
                        out=P["xb"][:], in0=gb_[b_][:, 0:D], scalar=1.0, in1=xn[:], op0=ALU.mult, op1=ALU.mult,
                        accum_out=actc[:, slot:slot + 1]),
                        reads=[("gbuf", b_), ("Hbxn", tt % 2)], writes=["Hbxb", ("Hbact", slot), "Hbfence"])
                    if prev is not None:
                        chain(slot - 1, prev)
                    prev = (b_, q_)
                S.op("dve", lambda e: e.tensor_copy(fence_t[:], actc[:, 0:1]), reads=[("Hbact", 0)], writes=["Hbfence"])
                chain(127, prev)
                if dbg is not None and dbg["name"] == "peer_cf":
                    S.dma("sp", dbg_out[:, tt * 128:(tt + 1) * 128], cf[:], reads=[("Hbcf", s_) for s_ in range(128)], writes=[("dbgo", tt)])
                    S.dma("sp", dbg_out[:, 1024 + tt * 128:1024 + (tt + 1) * 128], actc[:], reads=[("Hbact", s_) for s_ in range(128)], writes=[("dbgo2", tt)])
                S.op("act", lambda e, pv=pv: e.copy(out=tmpv[:, 0:512], in_=pv[0][:, :]),
                     reads=[("HbpsV", tt % 2, 0)], writes=[("Hbtmp", 0)])
                S.op("act", lambda e, pv=pv: e.copy(out=tmpv[:, 512:1024], in_=pv[1][:, :]),
                     reads=[("HbpsV", tt % 2, 1)], writes=[("Hbtmp", 1)])
                S.op("act", lambda e, pv=pv: e.copy(out=tmpv[:, 1024:1536], in_=pv[2][:, :]),
                     reads=[("HbpsV", tt % 2, 2)], writes=[("Hbtmp", 2)])
                S.op("act", lambda e, pv=pv: e.copy(out=tmpv[:, 1536:2048], in_=pv[3][:, :]),
                     reads=[("HbpsV", tt % 2, 3)], writes=[("Hbtmp", 3)])
                S.op("dve", lambda e, tt=tt: e.tensor_tensor(out=xres[:, tt, :], in0=xres[:, tt, :], in1=tmpv[:], op=ALU.add),
                     reads=[("Hbtmp", n) for n in range(4)] + [("xres", tt)], writes=[("xres", tt)])
            S.barrier()

        with ExitStack() as fs:
            gfb = fs.enter_context(nc.sbuf_tensor("gfb", [128, D], F32))
            junk = fs.enter_context(nc.sbuf_tensor("Ijunk", [128, D], BF16))
            ss = fs.enter_context(nc.sbuf_tensor("Iss", [128, 1], F32))
            ot = [fs.enter_context(nc.sbuf_tensor(f"Iot{i}", [128, D], F32)) for i in range(2)]
            S.dma("sp", gfb[:], g_final_d[0:1, :].partition_broadcast(128), writes=["gfb"])
            for tt in range(8):
                o_ = ot[tt % 2]
                S.op("act", lambda e, tt=tt: e.activation(out=junk[:], in_=xres[:, tt, :], func=AF.Square,
                                                          scale=float(D) ** -0.5, accum_out=ss[:]),
                     reads=[("xres", tt)], writes=["Ijunk", "Iss"])
                S.op("dve", lambda e: e.tensor_scalar_add(ss[:], ss[:], EPS), reads=["Iss"], writes=["Iss"])
                S.op("act", lambda e: e.sqrt(ss[:], ss[:]), reads=["Iss"], writes=["Iss"])
                S.op("dve", lambda e: e.reciprocal(ss[:], ss[:]), reads=["Iss"], writes=["Iss"])
                S.op("dve", lambda e, tt=tt, o_=o_: e.scalar_tensor_tensor(out=o_[:], in0=xres[:, tt, :], scalar=ss[:, 0:1],
                                                                    in1=gfb[:], op0=ALU.mult, op1=ALU.mult),
                     reads=[("xres", tt), "Iss", "gfb"], writes=[("Iot", tt % 2)])
                S.dma("sp", out_d[tt * 128:(tt + 1) * 128, :], o_[:], reads=[("Iot", tt % 2)], writes=[("outd", tt)])
            S.barrier()

        S.barrier()
        print("instructions", S.nins, "waits", S.nwaits, S.cnt, S.epoch, S.nsem)
    return nc


def rope_tables(j):
    half = 64
    inv = (10000.0 ** (-np.arange(half, dtype=np.float32) / half)).astype(np.float32)
    pos = (1024 * j - NPRE + np.arange(WIN)).astype(np.float32)
    ang = pos[None, :] * inv[:, None]
    cos = np.cos(ang).astype(np.float32)
    sin = np.sin(ang).astype(np.float32)
    return np.concatenate([cos, cos], 0), np.concatenate([sin, sin], 0)


def make_in_maps(inputs):
    x = np.asarray(inputs["x"], np.float32)
    ident = np.eye(128, dtype=np.float32).astype(ml_dtypes.bfloat16)
    R = np.zeros((128, 128), np.float32)
    for p in range(64):
        R[p, p + 64] = -1.0
        R[p + 64, p] = 1.0
    rotT = np.ascontiguousarray(R.T).astype(ml_dtypes.bfloat16)
    tri = np.triu(np.ones((128, 128), np.float32))
    trl = np.tril(np.ones((128, 128), np.float32))
    f32 = lambda a: np.asarray(a, np.float32)
    skT_h = np.ascontiguousarray(f32(inputs["sub_keys"])[0].reshape(16, 128, 128).transpose(2, 0, 1))
    u_tab = f32(inputs["u_tab"])[0]
    v_tab = f32(inputs["v_tab"])[0]
    maps = []
    for c in range(8):
        b, j = c // 4, c % 4
        xw = np.zeros((WIN, D), np.float32)
        lo = 1024 * j - NPRE
        src0 = max(lo, 0)
        xw[src0 - lo:] = x[b, src0:1024 * j + 1024]
        cos, sin = rope_tables(j)
        pmv = np.where(np.arange(WIN) + lo >= 0, 0.0, -30000.0).astype(np.float32)
        kbA = np.zeros((128, 48), np.float32)
        gi = 0
        for Q in range(2):
            for (d, r, sub) in [(1, 0, s_) for s_ in range(4)] + [(4, r_, 0) for r_ in range(4)] + [(16, r_, 0) for r_ in range(16)]:
                u0 = 2048 + 512 * Q + r + 128 * sub
                u = u0 - 128 * d + d * np.arange(128)
                kbA[:, gi] = np.where(1024 * j - 2048 + u >= 0, 0.0, -30000.0)
                gi += 1
        m = {
            "x_win": xw,
            "w_in": np.ascontiguousarray(np.asarray(inputs["w_in"], np.float32)[0]),
            "g_mix": np.asarray(inputs["g_mix"], np.float32).reshape(1, D),
            "cosT": cos, "sinT": sin, "ident": ident, "rotT": rotT,
            "cw_h": np.ascontiguousarray(f32(inputs["conv_w"])[0].T.reshape(8, 128, 4).transpose(1, 0, 2)),
            "cb_h": np.ascontiguousarray(f32(inputs["conv_b"])[0].reshape(8, 128).T),
            "gm_h": np.ascontiguousarray(f32(inputs["g_mhead"])[0].reshape(8, 128).T),
            "gb_h": np.concatenate([f32(inputs["b_mi"])[0], f32(inputs["b_mf"])[0]]).reshape(1, 8),
            "pm_h": np.ascontiguousarray(pmv.reshape(32, 128).T),
            "tri_f": tri,
            "w_mq": f32(inputs["w_mq"])[0], "w_mk": f32(inputs["w_mk"])[0],
            "w_out": f32(inputs["w_out"])[0], "g_final": f32(inputs["g_final"]).reshape(1, D),
            "mem_b": np.ascontiguousarray(f32(inputs["mem"])[b]),
            "g_mem": f32(inputs["g_mem"]).reshape(1, D), "g_cross": f32(inputs["g_cross"]).reshape(1, D),
            "w_xq": f32(inputs["w_xq"])[0], "w_xk": f32(inputs["w_xk"])[0], "w_xv": f32(inputs["w_xv"])[0],
            "w_xo": f32(inputs["w_xo"])[0],
            "g_ffn": f32(inputs["g_ffn"]).reshape(1, D), "w_pq": f32(inputs["w_pq"])[0],
            "skT_h": skT_h, "iota256": np.arange(256, dtype=np.float32).reshape(1, 256), "u_tab": u_tab, "v_tab": v_tab,
            "kbA": kbA, "ga_h": np.ascontiguousarray(f32(inputs["g_ahead"])[0].T), "trl_f": trl,
        }
        maps.append(m)
    return maps


def kernel(**inputs):
    nc = build()
    maps = make_in_maps(inputs)
    res = run_bass_kernel_spmd(nc, maps, core_ids=list(range(8)))
    out = np.zeros((2, 4096, D), np.float32)
    for c in range(8):
        b, j = c // 4, c % 4
        out[b, 1024 * j:1024 * j + 1024] = res.results[c]["out"]
    return out
```
